# Optimizing a Trainium2 kernel written in Bass

```python
import math
import jax, jax.numpy as jnp
from jax import lax
import numpy as np

D_MODEL = 4096
BATCH = 4
SEQ = 2048
DEPTH = 1

HEAD_DIM = 128
NSA_HEADS = 16
NSA_KV_HEADS = 4
NSA_REP = NSA_HEADS // NSA_KV_HEADS
NSA_WIDTH = NSA_HEADS * HEAD_DIM
NSA_KV_WIDTH = NSA_KV_HEADS * HEAD_DIM
CMP_BLOCK = 32
CMP_STRIDE = 16
CMP_HIDDEN = 256
SLC_BLOCK = 64
SLC_TOPK = 16
WINDOW = 512
SLC_Q_CHUNK = 32
DIFF_HEADS = 8
DIFF_DIM = 128
DIFF_VDIM = 2 * DIFF_DIM
DIFF_WIDTH = DIFF_HEADS * DIFF_VDIM
MIX_WIDTH = NSA_WIDTH + DIFF_WIDTH
COL_SIZES = (NSA_WIDTH,
             6 * NSA_KV_WIDTH,
             3 * NSA_HEADS,
             2 * DIFF_HEADS * DIFF_DIM,
             2 * DIFF_HEADS * DIFF_DIM,
             DIFF_WIDTH)
IN_COLS = sum(COL_SIZES)
SPLIT_POINTS = tuple(int(c) for c in np.cumsum(COL_SIZES)[:-1])
D_FF = 4 * D_MODEL
Q_BLOCK = 128
LN_EPS = 1e-5
RMS_EPS = 1e-6
NEG = -1e30
DEEPNORM_ALPHA = (2.0 * DEPTH) ** 0.25
DEEPNORM_BETA = (8.0 * DEPTH) ** -0.25

kernel_name = "nsa_diffattn_parallel_heads_deepnorm"


def alibi_slopes(n_heads):
    return np.array([2.0 ** (-8.0 * (h + 1) / n_heads) for h in range(n_heads)], np.float32)


def layer_norm(x, g, b):
    xf = x.astype(jnp.float32)
    mu = jnp.mean(xf, -1, keepdims=True)
    var = jnp.mean(jnp.square(xf - mu), -1, keepdims=True)
    return ((xf - mu) * lax.rsqrt(var + LN_EPS) * g + b).astype(x.dtype)


def rms_norm(x, g):
    xf = x.astype(jnp.float32)
    return (xf * lax.rsqrt(jnp.mean(xf * xf, -1, keepdims=True) + RMS_EPS) * g).astype(x.dtype)


def masked_softmax(s, mask):
    p = jax.nn.softmax(jnp.where(mask, s, NEG), axis=-1)
    return jnp.where(mask, p, 0.0)


def nsa_compressed_branch(q, k, v, pe_k, w1_k, w2_k, pe_v, w1_v, w2_v):
    S = k.shape[2]
    n_cmp = (S - CMP_BLOCK) // CMP_STRIDE + 1
    idx = np.arange(n_cmp)[:, None] * CMP_STRIDE + np.arange(CMP_BLOCK)[None, :]

    def compress(t, pe, w1, w2):
        blk = t[:, :, idx] + pe
        blk = blk.reshape(blk.shape[:3] + (CMP_BLOCK * HEAD_DIM,))
        return jax.nn.gelu(blk @ w1) @ w2

    kc = compress(k, pe_k, w1_k, w2_k)
    vc = compress(v, pe_v, w1_v, w2_v)
    s = jnp.einsum('bgrtd,bgnd->bgrtn', q, kc).astype(jnp.float32) * (HEAD_DIM ** -0.5)
    mask = idx[None, :, -1] <= np.arange(S)[:, None]
    p = masked_softmax(s, mask)
    o = jnp.einsum('bgrtn,bgnd->bgrtd', p.astype(vc.dtype), vc)
    return o, p


def nsa_select_blocks(p_cmp):
    S = p_cmp.shape[3]
    n_cmp = p_cmp.shape[4]
    n_slc = S // SLC_BLOCK
    cs = np.arange(n_cmp) * CMP_STRIDE
    ss = np.arange(n_slc) * SLC_BLOCK
    overlap = np.clip(np.minimum(cs[:, None] + CMP_BLOCK, ss[None, :] + SLC_BLOCK)
                      - np.maximum(cs[:, None], ss[None, :]), 0, None)
    overlap = (overlap / CMP_BLOCK).astype(np.float32)
    imp = jnp.einsum('bgtn,nj->bgtj', jnp.sum(p_cmp, axis=2), overlap)
    t = np.arange(S)[:, None]
    j = np.arange(n_slc)[None, :]
    causal = j * SLC_BLOCK <= t
    tb = t // SLC_BLOCK
    forced = (j == 0) | (j == tb) | (j == tb - 1)
    score = jnp.where(causal, jnp.where(forced, 1e6, imp), NEG)
    vals, sel = lax.top_k(score, min(SLC_TOPK, n_slc))
    return sel, vals > 0.5 * NEG


def nsa_selected_branch(q, k, v, sel, valid, slopes):
    B, G, R, S, Dh = q.shape
    n_slc = S // SLC_BLOCK
    n_top = sel.shape[-1]
    nc = S // SLC_Q_CHUNK
    kb = k.reshape(B, G, n_slc, SLC_BLOCK, Dh)
    vb = v.reshape(B, G, n_slc, SLC_BLOCK, Dh)
    q_c = q.reshape(B, G, R, nc, SLC_Q_CHUNK, Dh).transpose(3, 0, 1, 2, 4, 5)
    sel_c = sel.reshape(B, G, nc, SLC_Q_CHUNK, n_top).transpose(2, 0, 1, 3, 4)
    val_c = valid.reshape(B, G, nc, SLC_Q_CHUNK, n_top).transpose(2, 0, 1, 3, 4)
    t_c = jnp.arange(S, dtype=jnp.int32).reshape(nc, SLC_Q_CHUNK)
    bi = jnp.arange(B)[:, None, None, None]
    gi = jnp.arange(G)[None, :, None, None]

    def one_chunk(args):
        qc, selc, valc, tc = args
        kg = kb[bi, gi, selc]
        vg = vb[bi, gi, selc]
        s = jnp.einsum('bgrqd,bgqkld->bgrqkl', qc, kg).astype(jnp.float32) * (Dh ** -0.5)
        pos = selc[..., None] * SLC_BLOCK + jnp.arange(SLC_BLOCK)
        dist = tc[None, None, :, None, None] - pos
        mask = valc[..., None] & (dist >= 0)
        s = s - slopes[None, :, :, None, None, None] * dist[:, :, None].astype(jnp.float32)
        n_keys = n_top * SLC_BLOCK
        p = masked_softmax(s.reshape(B, G, R, SLC_Q_CHUNK, n_keys),
                           mask.reshape(B, G, 1, SLC_Q_CHUNK, n_keys))
        return jnp.einsum('bgrqn,bgqnd->bgrqd', p.astype(vg.dtype),
                          vg.reshape(B, G, SLC_Q_CHUNK, n_keys, Dh))

    o = lax.map(one_chunk, (q_c, sel_c, val_c, t_c))
    return o.transpose(1, 2, 3, 0, 4, 5).reshape(B, G, R, S, Dh)


def nsa_window_branch(q, k, v, slopes):
    B, G, R, S, Dh = q.shape
    nb = S // Q_BLOCK
    span = WINDOW + Q_BLOCK
    kp = jnp.pad(k, ((0, 0), (0, 0), (WINDOW, 0), (0, 0)))
    vp = jnp.pad(v, ((0, 0), (0, 0), (WINDOW, 0), (0, 0)))
    q_b = q.reshape(B, G, R, nb, Q_BLOCK, Dh).transpose(3, 0, 1, 2, 4, 5)

    def one_block(args):
        qb, b = args
        start = b * Q_BLOCK
        kw = lax.dynamic_slice_in_dim(kp, start, span, axis=2)
        vw = lax.dynamic_slice_in_dim(vp, start, span, axis=2)
        t = start + jnp.arange(Q_BLOCK)
        s_pos = start - WINDOW + jnp.arange(span)
        dist = t[:, None] - s_pos[None, :]
        mask = (dist >= 0) & (dist < WINDOW) & (s_pos[None, :] >= 0)
        s = jnp.einsum('bgrqd,bgkd->bgrqk', qb, kw).astype(jnp.float32) * (Dh ** -0.5)
        s = s - slopes[:, :, None, None] * dist.astype(jnp.float32)
        p = masked_softmax(s, mask)
        return jnp.einsum('bgrqk,bgkd->bgrqd', p.astype(vw.dtype), vw)

    o = lax.map(one_block, (q_b, jnp.arange(nb)))
    return o.transpose(1, 2, 3, 0, 4, 5).reshape(B, G, R, S, Dh)


def differential_attention(q12, k12, v, lam, slopes):
    B, H, _, S, d = q12.shape
    nb = S // Q_BLOCK
    q_b = q12.reshape(B, H, 2, nb, Q_BLOCK, d).transpose(3, 0, 1, 2, 4, 5)
    s_pos = jnp.arange(S)

    def one_block(args):
        qb, b = args
        t = b * Q_BLOCK + jnp.arange(Q_BLOCK)
        dist = t[:, None] - s_pos[None, :]
        mask = dist >= 0
        s = jnp.einsum('bhmqd,bhmkd->bhmqk', qb, k12).astype(jnp.float32) * (d ** -0.5)
        s = s - slopes[:, None, None, None] * dist.astype(jnp.float32)
        p = masked_softmax(s, mask)
        a = p[:, :, 0] - lam * p[:, :, 1]
        return jnp.einsum('bhqk,bhkd->bhqd', a.astype(v.dtype), v)

    o = lax.map(one_block, (q_b, jnp.arange(nb)))
    return o.transpose(1, 2, 0, 3, 4).reshape(B, H, S, v.shape[-1])


def hybrid_layer(x, w_in, cmp_pe_k, cmp_w1_k, cmp_w2_k, cmp_pe_v, cmp_w1_v, cmp_w2_v,
                 nsa_out_g, lambda_q1, lambda_k1, lambda_q2, lambda_k2, diff_subln_g,
                 w_out, ln1_g, ln1_b, w_ff1, w_ff2, ln2_g, ln2_b, lambda_init):
    B, S, _ = x.shape
    proj = jnp.einsum('bsd,dc->bsc', x, w_in)
    q_nsa, kv_nsa, g_nsa, q_diff, k_diff, v_diff = jnp.split(proj, SPLIT_POINTS, axis=-1)

    q = q_nsa.reshape(B, S, NSA_KV_HEADS, NSA_REP, HEAD_DIM).transpose(0, 2, 3, 1, 4)
    kv = kv_nsa.reshape(B, S, 6, NSA_KV_HEADS, HEAD_DIM).transpose(2, 0, 3, 1, 4)
    gates = jax.nn.sigmoid(g_nsa.astype(jnp.float32))
    gates = gates.reshape(B, S, NSA_KV_HEADS, NSA_REP, 3).transpose(0, 2, 3, 1, 4)
    slopes_nsa = jnp.asarray(alibi_slopes(NSA_HEADS).reshape(NSA_KV_HEADS, NSA_REP))
    o_cmp, p_cmp = nsa_compressed_branch(q, kv[0], kv[1], cmp_pe_k, cmp_w1_k, cmp_w2_k,
                                         cmp_pe_v, cmp_w1_v, cmp_w2_v)
    sel, valid = nsa_select_blocks(p_cmp)
    o_slc = nsa_selected_branch(q, kv[2], kv[3], sel, valid, slopes_nsa)
    o_win = nsa_window_branch(q, kv[4], kv[5], slopes_nsa)
    o_nsa = (gates[..., 0:1] * o_cmp + gates[..., 1:2] * o_slc
             + gates[..., 2:3] * o_win).astype(x.dtype)
    o_nsa = o_nsa.transpose(0, 3, 1, 2, 4).reshape(B, S, NSA_HEADS, HEAD_DIM)
    o_nsa = rms_norm(o_nsa, nsa_out_g).reshape(B, S, NSA_WIDTH)

    q12 = q_diff.reshape(B, S, DIFF_HEADS, 2, DIFF_DIM).transpose(0, 2, 3, 1, 4)
    k12 = k_diff.reshape(B, S, DIFF_HEADS, 2, DIFF_DIM).transpose(0, 2, 3, 1, 4)
    v = v_diff.reshape(B, S, DIFF_HEADS, DIFF_VDIM).transpose(0, 2, 1, 3)
    lam = (jnp.exp(jnp.sum(lambda_q1.astype(jnp.float32) * lambda_k1.astype(jnp.float32)))
           - jnp.exp(jnp.sum(lambda_q2.astype(jnp.float32) * lambda_k2.astype(jnp.float32)))
           + lambda_init)
    slopes_diff = jnp.asarray(alibi_slopes(DIFF_HEADS))
    o_diff = differential_attention(q12, k12, v, lam, slopes_diff)
    o_diff = rms_norm(o_diff, diff_subln_g) * (1.0 - lambda_init)
    o_diff = o_diff.transpose(0, 2, 1, 3).reshape(B, S, DIFF_WIDTH)

    mix = jnp.einsum('bsc,cd->bsd', jnp.concatenate([o_nsa, o_diff], axis=-1), w_out)
    x = layer_norm(DEEPNORM_ALPHA * x + mix, ln1_g, ln1_b)

    h = jnp.square(jax.nn.relu(jnp.einsum('bsd,df->bsf', x, w_ff1)))
    h = jnp.einsum('bsf,fd->bsd', h, w_ff2)
    return layer_norm(DEEPNORM_ALPHA * x + h, ln2_g, ln2_b)


def setup_inputs(seed: int = 0) -> dict:
    key = jax.random.key(seed)
    ks = jax.random.split(key, 24)
    f32 = jnp.float32
    L = DEPTH

    def nrm(k, shape, scale):
        return jax.random.normal(k, shape, f32) * scale

    def gain(k, shape):
        return 1.0 + 0.01 * jax.random.normal(k, shape, f32)

    return {
        "x": nrm(ks[0], (BATCH, SEQ, D_MODEL), 1.0),
        "w_in": nrm(ks[1], (L, D_MODEL, IN_COLS), D_MODEL ** -0.5),
        "cmp_pe_k": nrm(ks[2], (L, CMP_BLOCK, HEAD_DIM), 0.02),
        "cmp_w1_k": nrm(ks[3], (L, CMP_BLOCK * HEAD_DIM, CMP_HIDDEN), (CMP_BLOCK * HEAD_DIM) ** -0.5),
        "cmp_w2_k": nrm(ks[4], (L, CMP_HIDDEN, HEAD_DIM), CMP_HIDDEN ** -0.5),
        "cmp_pe_v": nrm(ks[5], (L, CMP_BLOCK, HEAD_DIM), 0.02),
        "cmp_w1_v": nrm(ks[6], (L, CMP_BLOCK * HEAD_DIM, CMP_HIDDEN), (CMP_BLOCK * HEAD_DIM) ** -0.5),
        "cmp_w2_v": nrm(ks[7], (L, CMP_HIDDEN, HEAD_DIM), CMP_HIDDEN ** -0.5),
        "nsa_out_g": gain(ks[8], (L, NSA_HEADS, HEAD_DIM)),
        "lambda_q1": nrm(ks[9], (L, DIFF_DIM), 0.1),
        "lambda_k1": nrm(ks[10], (L, DIFF_DIM), 0.1),
        "lambda_q2": nrm(ks[11], (L, DIFF_DIM), 0.1),
        "lambda_k2": nrm(ks[12], (L, DIFF_DIM), 0.1),
        "diff_subln_g": gain(ks[13], (L, DIFF_VDIM)),
        "w_out": nrm(ks[14], (L, MIX_WIDTH, D_MODEL), MIX_WIDTH ** -0.5 * DEEPNORM_BETA),
        "ln1_g": gain(ks[15], (L, D_MODEL)),
        "ln1_b": nrm(ks[16], (L, D_MODEL), 0.01),
        "w_ff1": nrm(ks[17], (L, D_MODEL, D_FF), D_MODEL ** -0.5),
        "w_ff2": nrm(ks[18], (L, D_FF, D_MODEL), D_FF ** -0.5 * DEEPNORM_BETA),
        "ln2_g": gain(ks[19], (L, D_MODEL)),
        "ln2_b": nrm(ks[20], (L, D_MODEL), 0.01),
    }


def reference(x, w_in, cmp_pe_k, cmp_w1_k, cmp_w2_k, cmp_pe_v, cmp_w1_v, cmp_w2_v,
              nsa_out_g, lambda_q1, lambda_k1, lambda_q2, lambda_k2, diff_subln_g,
              w_out, ln1_g, ln1_b, w_ff1, w_ff2, ln2_g, ln2_b):
    for l in range(DEPTH):
        lambda_init = 0.8 - 0.6 * math.exp(-0.3 * l)
        x = hybrid_layer(x, w_in[l], cmp_pe_k[l], cmp_w1_k[l], cmp_w2_k[l],
                         cmp_pe_v[l], cmp_w1_v[l], cmp_w2_v[l], nsa_out_g[l],
                         lambda_q1[l], lambda_k1[l], lambda_q2[l], lambda_k2[l],
                         diff_subln_g[l], w_out[l], ln1_g[l], ln1_b[l],
                         w_ff1[l], w_ff2[l], ln2_g[l], ln2_b[l], lambda_init)
    return x
```

```python
import os
import numpy as np
import concourse.bass as bass
import concourse.mybir as mybir
from concourse.bass_utils import run_bass_kernel_spmd
from contextlib import ExitStack

F32 = mybir.dt.float32
BF16 = mybir.dt.bfloat16
AF = mybir.ActivationFunctionType
ALU = mybir.AluOpType
AX = mybir.AxisListType

ALPHA = 2.0 ** 0.25
QSCALE = 128.0 ** -0.5
LN_EPS = 1e-5
RMS_EPS = 1e-6
NEGB = -30000.0
NFQ = 8
FPR = 128 // NFQ
DOFF = 896
DW = 3200
ENGS = ['pe', 'act', 'dve', 'pool', 'sp']


class Buf:
    def __init__(self, name):
        self.name = name
        self.w = {}
        self.r = {}
        self.dsem = None


class DSem:
    def __init__(self, h):
        self.h = h
        self.cnt = 0


class Trk:
    def __init__(self, nc):
        self.nc = nc
        self.q = {e: [] for e in ENGS}
        self.sem = {e: nc.alloc_semaphore(name='c_' + e) for e in ENGS if e != 'sp'}
        self.cnt = {e: 0 for e in ENGS}
        self.seen = {e: {} for e in ENGS}
        self.same = True
        self.dsems = []
        self.sems = {}
        for e in self.sem:
            self.sems[id(self.sem[e])] = self.sem[e]
        self.ninst = 0

    def newsem(self, name):
        name = f"{name}_{len(self.dsems)}"
        d = DSem(self.nc.alloc_semaphore(name=name))
        self.dsems.append(d)
        self.sems[id(d.h)] = d.h
        return d

    def _wait(self, eng, deps):
        for sid, val in deps.items():
            sem = self.sems[sid]
            own = (eng != 'sp' and sem is self.sem[eng])
            if own:
                if eng == 'pe' or not self.same or val > self.cnt[eng]:
                    continue
            if self.seen[eng].get(sid, 0) >= val:
                continue
            self.seen[eng][sid] = val
            self.ninst += 1
            self.q[eng].append(lambda h, sem=sem, val=val: h.wait_ge(sem, val))

    def _deps(self, reads, writes, acc):
        deps = {}

        def add(d):
            for s, v in d.items():
                if deps.get(s, 0) < v:
                    deps[s] = v
        for b in reads:
            add(b.w)
        for b in writes:
            add(b.r)
            if not acc:
                add(b.w)
        return deps

    def _record(self, tok, reads, writes, acc):
        s, v = tok
        for b in reads:
            b.r[s] = max(b.r.get(s, 0), v)
        for b in writes:
            if acc:
                b.w[s] = max(b.w.get(s, 0), v)
            else:
                b.w = {s: v}
                b.r = {}

    def op(self, eng, fn, reads=(), writes=(), inc=True, acc=False):
        self._wait(eng, self._deps(reads, writes, acc))
        sem = self.sem[eng]
        self.ninst += 1
        if inc:
            self.cnt[eng] += 1
            tok = (id(sem), self.cnt[eng])
            self.q[eng].append(lambda h, fn=fn, sem=sem: fn(h).then_inc(sem, 1))
        else:
            tok = (id(sem), self.cnt[eng] + 1)
            self.q[eng].append(lambda h, fn=fn: fn(h))
        self._record(tok, reads, writes, acc)

    def dma(self, pairs, reads=(), writes=(), dsem=None, acc=False, qeng='sp'):
        if dsem is None:
            b = writes[0]
            if b.dsem is None:
                b.dsem = self.newsem('d_' + b.name)
            dsem = b.dsem
        self._wait(qeng, self._deps(reads, writes, acc))
        for (o, i) in pairs:
            dsem.cnt += 16
            self.ninst += 1
            self.q[qeng].append(lambda h, o=o, i=i, s=dsem.h: h.dma_start(out=o, in_=i).then_inc(s, 16))
        self._record((id(dsem.h), dsem.cnt), reads, writes, acc)

    def barrier(self):
        deps = {}
        for e in self.sem:
            if self.cnt[e] > 0:
                deps[id(self.sem[e])] = self.cnt[e]
        for d in self.dsems:
            if d.cnt > 0:
                deps[id(d.h)] = d.cnt
        for e in ENGS:
            self._wait(e, deps)

    def emit(self):
        q = self.q
        with self.nc.Block() as block:
            @block.tensor
            def _(h):
                for f in q['pe']:
                    f(h)

            @block.scalar
            def _(h):
                for f in q['act']:
                    f(h)

            @block.vector
            def _(h):
                for f in q['dve']:
                    f(h)

            @block.gpsimd
            def _(h):
                for f in q['pool']:
                    f(h)

            @block.sync
            def _(h):
                for f in q['sp']:
                    f(h)
        self.q = {e: [] for e in ENGS}


def nsa_slope(h):
    return float(2.0 ** (-8.0 * (h + 1) / 16))


def diff_slope(h):
    return float(2.0 ** (-8.0 * (h + 1) / 8))


def build(debug=False):
    nc = bass.Bass("TRN2", target_bir_lowering=False)

    def din(name, shape, dt=F32):
        return nc.dram_tensor(name, shape, dt, kind="ExternalInput").ap()

    def dscr(name, shape, dt):
        return nc.dram_tensor(name, shape, dt, kind=("ExternalOutput" if debug else "Internal")).ap()

    xT = din("xT", [4096, 2048])
    w_in = din("w_in", [89, 128, 4096])
    w_out = din("w_out", [32, 128, 4096])
    w_ff1 = din("w_ff1", [128, 128, 4096])
    w_ff2 = din("w_ff2", [16, NFQ, 128, 4096])
    w1c = din("w1c", [2, 2, 128, 4096])
    w2c = din("w2c", [2, 128, 256])
    pec = din("pec", [2, 128, 32])
    gnsa = din("gnsa", [128, 2048])
    gdif = din("gdif", [128, 256])
    lamv = din("lamv", [128, 512])
    lnp = din("lnp", [128, 128])
    c_ident = din("c_ident", [128, 128])
    c_tri = din("c_tri", [128, 128])
    c_expand = din("c_expand", [32, 2048])
    c_dclamp = din("c_dclamp", [128, DW])
    p_ctxb = din("p_ctxb", [128, 1])
    p_cmpmask = din("p_cmpmask", [128, 1024])
    p_selA = din("p_selA", [128, 256])
    p_selB = din("p_selB", [128, 256])
    outT = nc.dram_tensor("outT", [4096, 1024], F32, kind="ExternalOutput").ap()
    S_in = dscr("S_in", [88, 128, 2048], BF16)
    Ys = dscr("Ys", [32, 128, 1024], F32)
    X1s = dscr("X1s", [32, 128, 1024], F32)
    Y2s = dscr("Y2s", [32, 128, 1024], F32)
    dbg_cat = dscr("dbg_cat", [128, 32 * 1024], BF16) if debug else None

    T = Trk(nc)
    B_out = Buf("outT")
    B_Sin = [Buf(f"Sin{i}") for i in range(88)]
    B_Ys = [Buf(f"Ys{i}") for i in range(32)]
    B_X1s = [Buf(f"X1s{i}") for i in range(32)]
    B_Y2s = [Buf(f"Y2s{i}") for i in range(16)]

    def TT(eng, out, in0, in1, op, reads, writes, **kw):
        T.op(eng, lambda h: h.tensor_tensor(out=out, in0=in0, in1=in1, op=op), reads, writes, **kw)

    def STT(eng, out, in0, scalar, in1, op0, op1, reads, writes, **kw):
        T.op(eng, lambda h: h.scalar_tensor_tensor(out=out, in0=in0, scalar=scalar, in1=in1, op0=op0, op1=op1),
             reads, writes, **kw)

    def TS(eng, out, in0, s1, s2, op0, op1, reads, writes, **kw):
        if op1 is None:
            T.op(eng, lambda h: h.tensor_scalar(out=out, in0=in0, scalar1=s1, scalar2=None, op0=op0), reads, writes, **kw)
        else:
            T.op(eng, lambda h: h.tensor_scalar(out=out, in0=in0, scalar1=s1, scalar2=s2, op0=op0, op1=op1),
                 reads, writes, **kw)

    def ACT(out, in_, func, reads, writes, bias=None, scale=None, accum_out=None, **kw):
        kws = {}
        if bias is not None:
            kws['bias'] = bias
        if scale is not None:
            kws['scale'] = scale
        if accum_out is not None:
            kws['accum_out'] = accum_out
        T.op('act', lambda h: h.activation(out=out, in_=in_, func=func, **kws), reads, writes, **kw)

    def CP(eng, out, in_, reads, writes, **kw):
        if eng == 'act':
            ACT(out, in_, AF.Copy, reads, writes, **kw)
        else:
            T.op(eng, lambda h: h.tensor_copy(out=out, in_=in_), reads, writes, **kw)

    def MS(eng, ap, val, writes):
        T.op(eng, lambda h: h.memset(ap, val), (), writes)

    def MM(out, lhsT, rhs, start, stop, reads, writes, inc=True, acc=False):
        T.op('pe', lambda h: h.matmul(out, lhsT, rhs, start=start, stop=stop), reads, writes, inc=inc, acc=acc)

    def DMA(out, in_, reads, writes, dsem=None, acc=False):
        T.dma([(out, in_)], reads, writes, dsem=dsem, acc=acc)

    def REDUCE(eng, out, in_, op, reads, writes):
        T.op(eng, lambda h: h.tensor_reduce(out=out, in_=in_, axis=AX.X, op=op), reads, writes)

    def RECIP(eng, out, in_, reads, writes):
        T.op(eng, lambda h: h.reciprocal(out=out, in_=in_), reads, writes)

    with ExitStack() as top:
        acnt = {'n': 0}

        def alloc(es, name, shape, dt):
            acnt['n'] += 1
            return es.enter_context(nc.sbuf_tensor(f"{name}_{acnt['n']}", shape, dt))

        banks = [top.enter_context(nc.psum_tensor(f"bank{i}", [128, 512], F32)) for i in range(8)]
        BB = [Buf(f"bank{i}") for i in range(8)]

        gates = alloc(top, "gates", [128, 8, 48], F32)
        B_gates = Buf("gates")
        lnp_t = alloc(top, "lnp_t", [128, 128], F32)
        B_lnp = Buf("lnp")
        identf = alloc(top, "identf", [128, 128], F32)
        identb = alloc(top, "identb", [128, 128], BF16)
        B_ident = Buf("ident")
        zcol = alloc(top, "zcol", [128, 1], F32)
        B_zcol = Buf("zcol")
        DMA(lnp_t[:, :], lnp[:, :], [], [B_lnp])
        DMA(identf[:, :], c_ident[:, :], [], [B_ident])
        CP('dve', identb[:, :], identf[:, :], [B_ident], [B_ident])
        MS('dve', zcol[:, :], 0.0, [B_zcol])

        class Pipe:
            pass

        def make_pipe(es, nwb=3):
            P = Pipe()
            P.stg = [alloc(es, f"stg{i}", [128, 4096], F32) for i in range(2)]
            P.B_stg = [Buf(f"stg{i}") for i in range(2)]
            P.wb = [alloc(es, f"wb{i}", [128, 4096], BF16) for i in range(nwb)]
            P.B_wb = [Buf(f"wb{i}") for i in range(nwb)]
            P.nwb = nwb
            P.jc = 0
            return P

        def gemm(P, jobs):
            n = len(jobs)
            base = P.jc

            def issue_dma(j):
                s = (base + j) % 2
                DMA(P.stg[s][:, :], jobs[j]['src'], [], [P.B_stg[s]])

            def issue_cast(j):
                s = (base + j) % 2
                w = (base + j) % P.nwb
                eng = 'dve' if (base + j) % 2 == 0 else 'pool'
                CP(eng, P.wb[w][:, :], P.stg[s][:, :], [P.B_stg[s]], [P.B_wb[w]])
            issue_dma(0)
            if n > 1:
                issue_dma(1)
            issue_cast(0)
            for j in range(n):
                job = jobs[j]
                if j + 1 < n:
                    issue_cast(j + 1)
                if j + 2 < n:
                    issue_dma(j + 2)
                w = (base + j) % P.nwb
                bset = 4 * ((base + j) % 2)
                if job.get('pre') is not None:
                    job['pre']()
                if job.get('custom') is not None:
                    job['custom'](P.wb[w], P.B_wb[w], bset)
                    continue
                KC, MC = job['KC'], job['MC']
                wv = P.wb[w][:, :].rearrange("p (k c) -> p k c", c=MC * 128)
                for k in range(KC):
                    for mc in range(MC):
                        for tg in range(2):
                            bi = bset + mc * 2 + tg
                            rap, rb = job['rhs'](k, tg)
                            last = (k == KC - 1 and mc == MC - 1 and tg == 1)
                            MM(banks[bi][:, :], wv[:, k, mc * 128:(mc + 1) * 128], rap,
                               start=(k == 0), stop=(k == KC - 1), reads=[P.B_wb[w]] + rb, writes=[BB[bi]],
                               inc=last, acc=(k > 0))
                job['evac'](bset)
            P.jc += n

        with ExitStack() as es:
            P = make_pipe(es)
            actT = alloc(es, "actT", [128, 32, 1024], BF16)
            B_act = [Buf(f"act{i}") for i in range(16)]
            xst = [alloc(es, f"xst{i}", [128, 2, 1024], F32) for i in range(2)]
            B_xst = [Buf(f"xst{i}") for i in range(2)]
            ost = [alloc(es, f"ost{i}", [128, 1024], BF16) for i in range(2)]
            B_ost = [Buf(f"ost{i}") for i in range(2)]
            for b in B_ost:
                b.dsem = T.newsem("st_" + b.name)
            xTv = xT.rearrange("(k p) n -> p k n", p=128)
            st = {'oc': 0}

            def load_x(tok0):
                for j in range(16):
                    s = j % 2
                    DMA(xst[s][:, :, :], xTv[:, 2 * j:2 * j + 2, tok0:tok0 + 1024], [], [B_xst[s]])
                    CP('dve' if j % 2 == 0 else 'pool', actT[:, 2 * j:2 * j + 2, :], xst[s][:, :, :], [B_xst[s]], [B_act[j]])

            def rhs_act(k, tg):
                return actT[:, k, tg * 512:(tg + 1) * 512], [B_act[k // 2]]

            def mk_evac_in(blk, tok0, scale):
                def ev(bset):
                    s = st['oc'] % 2
                    st['oc'] += 1
                    for tg in range(2):
                        ACT(ost[s][:, tg * 512:(tg + 1) * 512], banks[bset + tg][:, :], AF.Copy,
                            [BB[bset + tg]], [B_ost[s]], scale=scale, acc=(tg == 1))
                    DMA(S_in[blk][:, tok0:tok0 + 1024], ost[s][:, :], [B_ost[s]], [B_Sin[blk]], dsem=B_ost[s].dsem, acc=True)
                return ev

            def gate_custom(wbt, B_wbt, bset):
                wv = wbt[:, :].rearrange("p (k c) -> p k c", c=128)
                for i in range(8):
                    bi = bset + (i % 4)
                    for k in range(32):
                        MM(banks[bi][:, 0:128], actT[:, k, i * 128:(i + 1) * 128], wv[:, k, :],
                           start=(k == 0), stop=(k == 31), reads=[B_wbt, B_act[k // 2]], writes=[BB[bi]],
                           inc=(k == 31), acc=(k > 0))
                    ACT(gates[:, i, :], banks[bi][:, 0:48], AF.Sigmoid, [BB[bi]], [B_gates], acc=True)

            kv_blocks = list(range(16, 40)) + list(range(56, 88))
            q_blocks = list(range(0, 16)) + list(range(40, 56))
            load_x(0)
            jobs = [dict(src=w_in[b], KC=32, MC=1, rhs=rhs_act, evac=mk_evac_in(b, 0, 1.0)) for b in kv_blocks]
            gemm(P, jobs)
            load_x(1024)
            jobs = [dict(src=w_in[88], custom=gate_custom)]
            for b in range(88):
                jobs.append(dict(src=w_in[b], KC=32, MC=1, rhs=rhs_act,
                                 evac=mk_evac_in(b, 1024, QSCALE if b in q_blocks else 1.0)))
            gemm(P, jobs)
            T.barrier()
            T.emit()

        esBC = ExitStack()
        esBC.__enter__()
        catT = alloc(esBC, "catT", [128, 32, 1024], BF16)
        B_cat = [Buf(f"cat{i}") for i in range(32)]
        kcT_all = alloc(esBC, "kcT_all", [128, 4, 128], BF16)
        vc_all = alloc(esBC, "vc_all", [128, 4, 128], BF16)
        B_kc = Buf("kc")
        B_vc = Buf("vc")

        with ExitStack() as es:
            stg = alloc(es, "cstg", [128, 4096], F32)
            B_stg = Buf("cstg")
            w1b = [alloc(es, f"w1b{kv}", [128, 32, 256], BF16) for kv in range(2)]
            B_w1b = [Buf(f"w1b{kv}") for kv in range(2)]
            w2f = alloc(es, "w2f", [128, 256], F32)
            w2b = [alloc(es, f"w2b{kv}", [128, 2, 128], BF16) for kv in range(2)]
            B_w2 = [Buf(f"w2_{kv}") for kv in range(2)]
            B_w2f = Buf("w2f")
            pef = alloc(es, "pef", [128, 32], F32)
            peb = [alloc(es, f"peb{kv}", [128, 32], BF16) for kv in range(2)]
            B_pe = [Buf(f"pe{kv}") for kv in range(2)]
            B_pef = Buf("pef")
            cT = [alloc(es, f"cT{i}", [128, 2048], BF16) for i in range(2)]
            B_cT = [Buf(f"cT{i}") for i in range(2)]
            b1 = alloc(es, "b1", [128, 1], F32)
            B_b1 = Buf("b1")
            gx = alloc(es, "gx", [128, 128], F32)
            gw = alloc(es, "gw", [128, 128], F32)
            B_gx = Buf("gx")
            B_gw = Buf("gw")
            hid = [alloc(es, f"hid{hc}", [128, 128], BF16) for hc in range(2)]
            B_hid = [Buf(f"hid{hc}") for hc in range(2)]
            for kv in range(2):
                for half in range(2):
                    DMA(stg[:, :], w1c[kv, half], [], [B_stg])
                    CP('dve', w1b[kv][:, half * 16:(half + 1) * 16, :],
                       stg[:, :].rearrange("p (l h) -> p l h", h=256), [B_stg], [B_w1b[kv]], acc=(half == 1))
                DMA(w2f[:, :], w2c[kv], [], [B_w2f])
                CP('dve', w2b[kv][:, :, :], w2f[:, :].rearrange("p (c d) -> p c d", d=128), [B_w2f], [B_w2[kv]])
                DMA(pef[:, :], pec[kv], [], [B_pef])
                CP('dve', peb[kv][:, :], pef[:, :], [B_pef], [B_pe[kv]])
            MS('dve', kcT_all[:, :, :], 0.0, [B_kc])
            MS('dve', vc_all[:, :, :], 0.0, [B_vc])
            for hc in range(2):
                MS('pool', hid[hc][:, :], 0.0, [B_hid[hc]])
            cnt = 0
            for g in range(4):
                for kv in range(2):
                    blk = 16 + kv * 4 + g
                    s = cnt % 2
                    cnt += 1
                    DMA(cT[s][:, :], S_in[blk], [B_Sin[blk]], [B_cT[s]])
                    for hc in range(2):
                        bi = 6 + (hc % 2)
                        for l in range(32):
                            MM(banks[bi][:, 0:127], w1b[kv][:, l, hc * 128:(hc + 1) * 128],
                               cT[s][:, l:l + 16 * 126 + 1:16], start=(l == 0), stop=(l == 31),
                               reads=[B_w1b[kv], B_cT[s]], writes=[BB[bi]], inc=False, acc=(l > 0))
                        for l in range(32):
                            MM(banks[bi][:, 128:129], w1b[kv][:, l, hc * 128:(hc + 1) * 128],
                               peb[kv][:, l:l + 1], start=(l == 0), stop=(l == 31),
                               reads=[B_w1b[kv], B_pe[kv]], writes=[BB[bi]], inc=(l == 31), acc=True)
                        CP('dve', b1[:, :], banks[bi][:, 128:129], [BB[bi]], [B_b1])
                        ACT(gx[:, 0:127], banks[bi][:, 0:127], AF.Identity, [BB[bi], B_b1], [B_gx], bias=b1[:, 0:1])
                        TT('dve', gw[:, 0:127], gx[:, 0:127], gx[:, 0:127], ALU.mult, [B_gx], [B_gw])
                        TS('dve', gw[:, 0:127], gw[:, 0:127], 0.044715, 1.0, ALU.mult, ALU.add, [B_gw], [B_gw])
                        TT('dve', gw[:, 0:127], gw[:, 0:127], gx[:, 0:127], ALU.mult, [B_gw, B_gx], [B_gw])
                        ACT(gw[:, 0:127], gw[:, 0:127], AF.Tanh, [B_gw], [B_gw], scale=0.7978845608028654)
                        STT('dve', gw[:, 0:127], gw[:, 0:127], 1.0, gx[:, 0:127], ALU.add, ALU.mult, [B_gw, B_gx], [B_gw])
                        TS('dve', hid[hc][:, 0:127], gw[:, 0:127], 0.5, None, ALU.mult, None, [B_gw], [B_hid[hc]])
                    if kv == 0:
                        for hc in range(2):
                            MM(banks[6][:, 0:127], w2b[0][:, hc, :], hid[hc][:, 0:127], start=(hc == 0), stop=(hc == 1),
                               reads=[B_w2[0], B_hid[hc]], writes=[BB[6]], inc=(hc == 1), acc=(hc == 1))
                        CP('act', kcT_all[:, g, 0:127], banks[6][:, 0:127], [BB[6]], [B_kc], acc=True)
                    else:
                        for hc in range(2):
                            MM(banks[6][0:127, 0:128], hid[hc][:, 0:127], w2b[1][:, hc, :], start=(hc == 0), stop=(hc == 1),
                               reads=[B_w2[1], B_hid[hc]], writes=[BB[6]], inc=(hc == 1), acc=(hc == 1))
                        CP('act', vc_all[0:127, g, :], banks[6][0:127, 0:128], [BB[6]], [B_vc], acc=True)
            T.barrier()
            T.emit()

        with ExitStack() as es:
            dcl = alloc(es, "dcl", [128, DW], F32)
            B_dcl = Buf("dcl")
            DMA(dcl[:, :], c_dclamp[:, :], [], [B_dcl])
            tmpf = [alloc(es, f"tmpf{i}", [128, 512], F32) for i in range(2)]
            B_tmpf = [Buf(f"tmpf{i}") for i in range(2)]
            tri = alloc(es, "tri", [128, 128], BF16)
            tri2 = alloc(es, "tri2", [128, 128], BF16)
            B_tri = Buf("tri")
            DMA(tmpf[1][:, 0:128], c_tri[:, :], [], [B_tmpf[1]])
            CP('dve', tri[:, :], tmpf[1][:, 0:128], [B_tmpf[1]], [B_tri])
            TS('dve', tri2[:, :], tmpf[1][:, 0:128], -1.0, 1.0, ALU.mult, ALU.add, [B_tmpf[1]], [B_tri], acc=True)
            expb = alloc(es, "expb", [32, 2048], BF16)
            B_exp = Buf("exp")
            for q4 in range(4):
                DMA(tmpf[0][0:32, :], c_expand[:, q4 * 512:(q4 + 1) * 512], [], [B_tmpf[0]])
                CP('dve', expb[:, q4 * 512:(q4 + 1) * 512], tmpf[0][0:32, :], [B_tmpf[0]], [B_exp], acc=True)
            ctxb = alloc(es, "ctxb", [128, 1], F32)
            B_ctxb = Buf("ctxb")
            DMA(ctxb[:, :], p_ctxb[:, :], [], [B_ctxb])
            cmpmask = alloc(es, "cmpmask", [128, 8, 128], F32)
            B_cm = Buf("cmpmask")
            DMA(cmpmask[:, :, :], p_cmpmask.rearrange("p (i n) -> p i n", n=128), [], [B_cm])
            selA = alloc(es, "selA", [128, 8, 32], F32)
            selB = alloc(es, "selB", [128, 8, 32], F32)
            B_sAB = Buf("selAB")
            DMA(selA[:, :, :], p_selA.rearrange("p (i n) -> p i n", n=32), [], [B_sAB])
            DMA(selB[:, :, :], p_selB.rearrange("p (i n) -> p i n", n=32), [], [B_sAB], acc=True,
                dsem=T.newsem("selB"))
            gn = alloc(es, "gn", [128, 512], F32)
            gd = alloc(es, "gd", [128, 256], F32)
            B_g = Buf("gn")
            B_gd = Buf("gd")
            DMA(gd[:, :], gdif[:, :], [], [B_gd])
            lam_t = alloc(es, "lam_t", [128, 512], F32)
            B_lam = Buf("lam")
            lsc = alloc(es, "lsc", [128, 8], F32)
            DMA(lam_t[:, :], lamv[:, :], [], [B_lam])
            TT('dve', lam_t[:, 0:128], lam_t[:, 0:128], lam_t[:, 128:256], ALU.mult, [B_lam], [B_lam])
            TT('dve', lam_t[:, 256:384], lam_t[:, 256:384], lam_t[:, 384:512], ALU.mult, [B_lam], [B_lam])
            REDUCE('dve', lsc[:, 0:1], lam_t[:, 0:128], ALU.add, [B_lam], [B_lam])
            REDUCE('dve', lsc[:, 1:2], lam_t[:, 256:384], ALU.add, [B_lam], [B_lam])
            ACT(lsc[:, 2:4], lsc[:, 0:2], AF.Exp, [B_lam], [B_lam])
            TT('dve', lsc[:, 4:5], lsc[:, 3:4], lsc[:, 2:3], ALU.subtract, [B_lam], [B_lam])
            TS('dve', lsc[:, 5:6], lsc[:, 4:5], -0.2, None, ALU.add, None, [B_lam], [B_lam])
            neglam = lsc[:, 5:6]

            PT = [alloc(es, f"PT{i}", [128, 512], BF16) for i in range(3)]
            B_PT = [Buf(f"PT{i}") for i in range(3)]
            rg = {'t': 0, 'p': 0, 's': 0, 'm': 0}
            sm = alloc(es, "sm", [128, 64], F32)
            B_sm = Buf("sm")
            sqs = alloc(es, "sqs", [128, 256], F32)
            B_sqs = Buf("sqs")
            ob = alloc(es, "ob", [128, 256], BF16)
            B_ob = Buf("ob")
            of = alloc(es, "of", [128, 256], F32)
            B_of = Buf("of")

            def rms_and_store(src_ap, src_bufs, width, gain_ap, gain_bufs, extra, chunk0, i):
                TT('pool', sqs[:, 0:width], src_ap, src_ap, ALU.mult, src_bufs, [B_sqs])
                REDUCE('dve', sm[:, 0:1], sqs[:, 0:width], ALU.add, [B_sqs], [B_sm])
                TS('dve', sm[:, 0:1], sm[:, 0:1], 1.0 / width, RMS_EPS, ALU.mult, ALU.add, [B_sm], [B_sm])
                ACT(sm[:, 1:2], sm[:, 0:1], AF.Ln, [B_sm], [B_sm])
                ACT(sm[:, 2:3], sm[:, 1:2], AF.Exp, [B_sm], [B_sm], scale=-0.5)
                if extra != 1.0:
                    TS('dve', sm[:, 2:3], sm[:, 2:3], extra, None, ALU.mult, None, [B_sm], [B_sm])
                STT('dve', ob[:, 0:width], src_ap, sm[:, 2:3], gain_ap, ALU.mult, ALU.mult,
                    src_bufs + [B_sm] + gain_bufs, [B_ob])
                for hf in range(width // 128):
                    bi = 6 + (rg['m'] % 2)
                    rg['m'] += 1
                    MM(banks[bi][:, 0:128], ob[:, hf * 128:(hf + 1) * 128], identb[:, :], True, True,
                       [B_ob, B_ident], [BB[bi]])
                    CP('act', catT[:, chunk0 + hf, i * 128:(i + 1) * 128], banks[bi][:, 0:128], [BB[bi]],
                       [B_cat[chunk0 + hf]], acc=True)

            def load_V(VT, B_VT, Vtok, B_Vtok, ncol):
                for kt in range(16):
                    bi = 6 + (rg['m'] % 2)
                    rg['m'] += 1
                    for c in range(ncol):
                        MM(banks[bi][:, c * 128:(c + 1) * 128], VT[c][:, kt * 128:(kt + 1) * 128], identb[:, :], True, True,
                           [B_VT[c], B_ident], [BB[bi]], inc=(c == ncol - 1), acc=(c > 0))
                    CP('act' if kt % 2 == 0 else 'dve', Vtok[:, kt, 0:ncol * 128], banks[bi][:, 0:ncol * 128],
                       [BB[bi]], [B_Vtok], acc=True)

            def dense_attn(nm, QT, B_QT, KT, B_KT, Vtok, B_Vtok, dv, slope, CN, chunks, kt_lo_fn, pair_fn, MT, B_MT,
                           finalize):
                ntile = CN // 128
                for c in chunks:
                    qt0 = 8 + c * ntile
                    kts = list(range(kt_lo_fn(qt0), qt0 + ntile))
                    obank = {}
                    for m in range(nm):
                        for ip in range(ntile):
                            obank[(m, ip)] = 2 + m * ntile + ip
                    first = {}
                    for kt in kts:
                        sbk = rg['s'] % 2
                        rg['s'] += 1
                        for m in range(nm):
                            MM(banks[sbk][:, m * CN:(m + 1) * CN], KT[m][:, kt * 128:(kt + 1) * 128],
                               QT[m][:, c * CN:(c + 1) * CN], True, True, [B_KT[m], B_QT[m]], [BB[sbk]],
                               inc=(m == nm - 1), acc=(m > 0))
                        u0 = 1024 + c * CN - kt * 128 + DOFF
                        tf = rg['t'] % 2
                        rg['t'] += 1
                        if nm == 1:
                            STT('dve', tmpf[tf][:, 0:CN], dcl[:, u0:u0 + CN], -slope, banks[sbk][:, 0:CN],
                                ALU.mult, ALU.add, [B_dcl, BB[sbk]], [B_tmpf[tf]])
                        else:
                            STT('dve', tmpf[tf][:, 0:nm * CN].rearrange("p (m n) -> p m n", m=nm),
                                dcl[:, u0:u0 + CN].unsqueeze(1).broadcast_to([128, nm, CN]), -slope,
                                banks[sbk][:, 0:nm * CN].rearrange("p (m n) -> p m n", m=nm),
                                ALU.mult, ALU.add, [B_dcl, BB[sbk]], [B_tmpf[tf]])
                        pi = rg['p'] % 3
                        rg['p'] += 1
                        ACT(PT[pi][:, 0:nm * CN], tmpf[tf][:, 0:nm * CN], AF.Exp, [B_tmpf[tf], B_ctxb, B_zcol],
                            [B_PT[pi]], bias=(ctxb[:, 0:1] if kt < 8 else zcol[:, 0:1]))
                        if MT is not None:
                            TT('pool', PT[pi][:, 0:CN], PT[pi][:, 0:CN], MT[:, kt, :], ALU.mult,
                               [B_PT[pi], B_MT], [B_PT[pi]])
                        for ip in range(ntile):
                            qt = qt0 + ip
                            pm = pair_fn(kt, qt)
                            if pm is None or pm == 'full':
                                continue
                            msk = tri if pm == 'tri' else tri2
                            if nm == 1:
                                TT('pool', PT[pi][:, ip * 128:(ip + 1) * 128], PT[pi][:, ip * 128:(ip + 1) * 128],
                                   msk[:, :], ALU.mult, [B_PT[pi], B_tri], [B_PT[pi]])
                            else:
                                v = PT[pi][:, 0:nm * CN].rearrange("p (m n) -> p m n", m=nm)[:, :, ip * 128:(ip + 1) * 128]
                                TT('pool', v, v, msk[:, :].unsqueeze(1).broadcast_to([128, nm, 128]), ALU.mult,
                                   [B_PT[pi], B_tri], [B_PT[pi]])
                        avs = []
                        for m in range(nm):
                            for ip in range(ntile):
                                qt = qt0 + ip
                                if pair_fn(kt, qt) is None:
                                    continue
                                avs.append((m, ip))
                        for idx, (m, ip) in enumerate(avs):
                            qt = qt0 + ip
                            ob_ = obank[(m, ip)]
                            st_ = (m, ip) not in first
                            first[(m, ip)] = True
                            MM(banks[ob_][:, 0:dv + 1], PT[pi][:, m * CN + ip * 128:m * CN + (ip + 1) * 128],
                               Vtok[:, kt, 0:dv + 1], st_, (kt == qt), [B_PT[pi], B_Vtok], [BB[ob_]],
                               inc=(idx == len(avs) - 1), acc=(not st_))
                    for ip in range(ntile):
                        finalize(c * ntile + ip, [obank[(m, ip)] for m in range(nm)])

            with ExitStack() as es2:
                QT = [alloc(es2, f"QT{r}", [128, 1024], BF16) for r in range(4)]
                B_QT = [Buf(f"QT{r}") for r in range(4)]
                sK = alloc(es2, "sK", [128, 2048], BF16)
                wK = alloc(es2, "wK", [128, 2048], BF16)
                B_sK = Buf("sK")
                B_wK = Buf("wK")
                VTs = [alloc(es2, "VTs0", [128, 2048], BF16)] * 2
                B_VTs = [Buf("VTs0")] * 2
                sV = alloc(es2, "sV", [128, 16, 132], BF16)
                wV = alloc(es2, "wV", [128, 16, 132], BF16)
                B_sV = Buf("sV")
                B_wV = Buf("wV")
                MS('pool', sV[:, :, 128:129], 1.0, [B_sV])
                MS('pool', wV[:, :, 128:129], 1.0, [B_wV])
                acc = alloc(es2, "acc", [128, 8, 4, 128], F32)
                B_acc = [[Buf(f"acc{i}_{r}") for r in range(4)] for i in range(8)]
                MT = alloc(es2, "MT", [128, 16, 512], BF16)
                B_MT = Buf("MT")
                Ppad = alloc(es2, "Ppad", [128, 132], F32)
                B_Pp = Buf("Ppad")
                MS('dve', Ppad[:, :], 0.0, [B_Pp])
                Ef = alloc(es2, "Ef", [128, 128], F32)
                B_Ef = Buf("Ef")
                pbt = alloc(es2, "pbt", [128, 128], BF16)
                B_pbt = Buf("pbt")
                pTb = alloc(es2, "pTb", [128, 128], BF16)
                B_pTb = Buf("pTb")
                impt = alloc(es2, "impt", [128, 3, 32], F32)
                B_imp = Buf("imp")
                c3 = alloc(es2, "c3", [128, 32, 32], F32)
                B_c3 = Buf("c3")
                selb = alloc(es2, "selb", [128, 32], BF16)
                B_selb = Buf("selb")
                selT = alloc(es2, "selT", [32, 1024], BF16)
                B_selT = Buf("selT")
                for g in range(4):
                    DMA(gn[:, :], gnsa[:, g * 512:(g + 1) * 512], [], [B_g])
                    for r in range(4):
                        DMA(QT[r][:, :], S_in[g * 4 + r][:, 1024:2048], [B_Sin[g * 4 + r]], [B_QT[r]])
                    DMA(sK[:, :], S_in[16 + 2 * 4 + g], [B_Sin[16 + 8 + g]], [B_sK])
                    DMA(wK[:, :], S_in[16 + 4 * 4 + g], [B_Sin[16 + 16 + g]], [B_wK])
                    DMA(VTs[0][:, :], S_in[16 + 3 * 4 + g], [B_Sin[16 + 12 + g]], [B_VTs[0]])
                    load_V([VTs[0]], [B_VTs[0]], sV, B_sV, 1)
                    DMA(VTs[1][:, :], S_in[16 + 5 * 4 + g], [B_Sin[16 + 20 + g]], [B_VTs[1]])
                    load_V([VTs[1]], [B_VTs[1]], wV, B_wV, 1)
                    for i in range(8):
                        for r in range(4):
                            h = g * 4 + r
                            bi = 6 + (rg['m'] % 2)
                            rg['m'] += 1
                            MM(banks[bi][:, 0:128], QT[r][:, i * 128:(i + 1) * 128], kcT_all[:, g, :], True, True,
                               [B_QT[r], B_kc], [BB[bi]])
                            TT('dve', Ef[:, :], banks[bi][:, 0:128], cmpmask[:, i, :], ALU.add, [BB[bi], B_cm], [B_Ef])
                            ACT(Ef[:, :], Ef[:, :], AF.Exp, [B_Ef], [B_Ef])
                            REDUCE('dve', sm[:, 8:9], Ef[:, :], ALU.add, [B_Ef], [B_sm])
                            TS('dve', sm[:, 9:10], sm[:, 8:9], 1e-30, None, ALU.add, None, [B_sm], [B_sm])
                            RECIP('dve', sm[:, 10:11], sm[:, 9:10], [B_sm], [B_sm])
                            TS('dve', Ef[:, :], Ef[:, :], sm[:, 10:11], None, ALU.mult, None, [B_Ef, B_sm], [B_Ef])
                            if r == 0:
                                CP('dve', Ppad[:, 1:129], Ef[:, :], [B_Ef], [B_Pp])
                            else:
                                TT('dve', Ppad[:, 1:129], Ppad[:, 1:129], Ef[:, :], ALU.add, [B_Ef, B_Pp], [B_Pp])
                            CP('pool', pbt[:, :], Ef[:, :], [B_Ef], [B_pbt])
                            bi2 = 6 + (rg['m'] % 2)
                            rg['m'] += 1
                            MM(banks[bi2][:, 0:128], pbt[:, :], identb[:, :], True, True, [B_pbt, B_ident], [BB[bi2]])
                            CP('act', pTb[:, :], banks[bi2][:, 0:128], [BB[bi2]], [B_pTb])
                            bi3 = 6 + (rg['m'] % 2)
                            rg['m'] += 1
                            MM(banks[bi3][:, 0:128], pTb[:, :], vc_all[:, g, :], True, True, [B_pTb, B_vc], [BB[bi3]])
                            TS('dve', acc[:, i, r, :], banks[bi3][:, 0:128], gates[:, i, h * 3:h * 3 + 1], None,
                               ALU.mult, None, [BB[bi3], B_gates], [B_acc[i][r]])
                        Av = Ppad[:, 1:129].rearrange("p (j f) -> p j f", f=4)
                        Bv = Ppad[:, 0:128].rearrange("p (j f) -> p j f", f=4)
                        TT('dve', impt[:, 0, :], Av[:, :, 0], Av[:, :, 1], ALU.add, [B_Pp], [B_imp])
                        TT('dve', impt[:, 0, :], impt[:, 0, :], Av[:, :, 2], ALU.add, [B_Pp, B_imp], [B_imp])
                        TT('dve', impt[:, 1, :], Av[:, :, 3], Bv[:, :, 0], ALU.add, [B_Pp, B_imp], [B_imp])
                        STT('dve', impt[:, 0, :], impt[:, 1, :], 0.5, impt[:, 0, :], ALU.mult, ALU.add, [B_imp], [B_imp])
                        TT('dve', impt[:, 0, :], impt[:, 0, :], selA[:, i, :], ALU.mult, [B_imp, B_sAB], [B_imp])
                        TT('dve', impt[:, 2, :], impt[:, 0, :], selB[:, i, :], ALU.add, [B_imp, B_sAB], [B_imp])
                        sc = impt[:, 2, :]
                        TT('dve', c3[:, :, :], sc.unsqueeze(1).broadcast_to([128, 32, 32]),
                           sc.unsqueeze(2).broadcast_to([128, 32, 32]), ALU.is_gt, [B_imp], [B_c3])
                        REDUCE('dve', impt[:, 1, :], c3[:, :, :], ALU.add, [B_c3], [B_imp])
                        TS('dve', selb[:, :], impt[:, 1, :], 15.5, None, ALU.is_lt, None, [B_imp], [B_selb])
                        bi = 6 + (rg['m'] % 2)
                        rg['m'] += 1
                        MM(banks[bi][0:32, 0:128], selb[:, :], identb[:, :], True, True, [B_selb, B_ident], [BB[bi]])
                        CP('act', selT[:, i * 128:(i + 1) * 128], banks[bi][0:32, 0:128], [BB[bi]], [B_selT], acc=True)
                    for c in range(2):
                        qt0 = 8 + 4 * c
                        for kt in range(0, qt0 + 4):
                            bi = 6 + (rg['m'] % 2)
                            rg['m'] += 1
                            MM(banks[bi][:, :], expb[:, kt * 128:(kt + 1) * 128], selT[:, c * 512:(c + 1) * 512],
                               True, True, [B_exp, B_selT], [BB[bi]])
                            CP('act', MT[:, kt, :], banks[bi][:, :], [BB[bi]], [B_MT], acc=True)
                            if kt >= qt0:
                                ip = kt - qt0
                                TT('pool', MT[:, kt, ip * 128:(ip + 1) * 128], MT[:, kt, ip * 128:(ip + 1) * 128],
                                   tri[:, :], ALU.mult, [B_MT, B_tri], [B_MT])
                        for r in range(4):
                            h = g * 4 + r

                            def fin_slc(i, obs, r=r, h=h):
                                o = banks[obs[0]]
                                TS('dve', sm[:, 16:17], o[:, 128:129], 1e-30, None, ALU.add, None, [BB[obs[0]]], [B_sm])
                                RECIP('dve', sm[:, 17:18], sm[:, 16:17], [B_sm], [B_sm])
                                TT('dve', sm[:, 18:19], sm[:, 17:18], gates[:, i, h * 3 + 1:h * 3 + 2], ALU.mult,
                                   [B_sm, B_gates], [B_sm])
                                STT('dve', acc[:, i, r, :], o[:, 0:128], sm[:, 18:19], acc[:, i, r, :], ALU.mult, ALU.add,
                                    [BB[obs[0]], B_sm, B_acc[i][r]], [B_acc[i][r]])
                            dense_attn(1, [QT[r]], [B_QT[r]], [sK], [B_sK], sV, B_sV, 128, nsa_slope(h), 512, [c],
                                       lambda qt0_: 0, lambda kt, qt: ('full' if kt <= qt else None), MT, B_MT, fin_slc)
                    for r in range(4):
                        h = g * 4 + r

                        def fin_win(i, obs, r=r, h=h):
                            o = banks[obs[0]]
                            TS('dve', sm[:, 16:17], o[:, 128:129], 1e-30, None, ALU.add, None, [BB[obs[0]]], [B_sm])
                            RECIP('dve', sm[:, 17:18], sm[:, 16:17], [B_sm], [B_sm])
                            TT('dve', sm[:, 18:19], sm[:, 17:18], gates[:, i, h * 3 + 2:h * 3 + 3], ALU.mult,
                               [B_sm, B_gates], [B_sm])
                            STT('dve', acc[:, i, r, :], o[:, 0:128], sm[:, 18:19], acc[:, i, r, :], ALU.mult, ALU.add,
                                [BB[obs[0]], B_sm, B_acc[i][r]], [B_acc[i][r]])
                            rms_and_store(acc[:, i, r, :], [B_acc[i][r]], 128, gn[:, r * 128:(r + 1) * 128], [B_g], 1.0, h, i)

                        def pair_win(kt, qt):
                            if kt == qt:
                                return 'tri'
                            if kt == qt - 4:
                                return 'tri2'
                            if qt - 4 < kt < qt:
                                return 'full'
                            return None
                        dense_attn(1, [QT[r]], [B_QT[r]], [wK], [B_wK], wV, B_wV, 128, nsa_slope(h), 512, [0, 1],
                                   lambda qt0_: qt0_ - 4, pair_win, None, None, fin_win)
                T.barrier()
                T.emit()

            with ExitStack() as es2:
                QT = [alloc(es2, f"dQT{m}", [128, 1024], BF16) for m in range(2)]
                B_QT = [Buf(f"dQT{m}") for m in range(2)]
                KT = [alloc(es2, f"dKT{m}", [128, 2048], BF16) for m in range(2)]
                B_KT = [Buf(f"dKT{m}") for m in range(2)]
                VT = [alloc(es2, f"dVT{m}", [128, 2048], BF16) for m in range(2)]
                B_VT = [Buf(f"dVT{m}") for m in range(2)]
                dV = alloc(es2, "dV", [128, 16, 260], BF16)
                B_dV = Buf("dV")
                MS('pool', dV[:, :, 256:257], 1.0, [B_dV])
                for h in range(8):
                    for m in range(2):
                        DMA(QT[m][:, :], S_in[40 + 2 * h + m][:, 1024:2048], [B_Sin[40 + 2 * h + m]], [B_QT[m]])
                        DMA(KT[m][:, :], S_in[56 + 2 * h + m], [B_Sin[56 + 2 * h + m]], [B_KT[m]])
                        DMA(VT[m][:, :], S_in[72 + 2 * h + m], [B_Sin[72 + 2 * h + m]], [B_VT[m]])
                    load_V(VT, B_VT, dV, B_dV, 2)

                    def fin_diff(i, obs, h=h):
                        o1 = banks[obs[0]]
                        o2 = banks[obs[1]]
                        TS('dve', sm[:, 24:25], o1[:, 256:257], 1e-30, None, ALU.add, None, [BB[obs[0]]], [B_sm])
                        RECIP('dve', sm[:, 25:26], sm[:, 24:25], [B_sm], [B_sm])
                        TS('dve', sm[:, 26:27], o2[:, 256:257], 1e-30, None, ALU.add, None, [BB[obs[1]]], [B_sm])
                        RECIP('dve', sm[:, 27:28], sm[:, 26:27], [B_sm], [B_sm])
                        TT('dve', sm[:, 28:29], sm[:, 27:28], neglam, ALU.mult, [B_sm, B_lam], [B_sm])
                        TS('dve', of[:, :], o1[:, 0:256], sm[:, 25:26], None, ALU.mult, None, [BB[obs[0]], B_sm], [B_of])
                        STT('dve', of[:, :], o2[:, 0:256], sm[:, 28:29], of[:, :], ALU.mult, ALU.add,
                            [BB[obs[1]], B_sm, B_of], [B_of])
                        rms_and_store(of[:, :], [B_of], 256, gd[:, :], [B_gd], 0.8, 16 + 2 * h, i)
                    dense_attn(2, QT, B_QT, KT, B_KT, dV, B_dV, 256, diff_slope(h), 256, [0, 1, 2, 3],
                               lambda qt0_: 0, lambda kt, qt: ('tri' if kt == qt else ('full' if kt < qt else None)),
                               None, None, fin_diff)
                if debug:
                    DMA(dbg_cat[:, :], catT[:, :, :].rearrange("p k n -> p (k n)"), B_cat, [Buf("dbgc")])
                T.barrier()
                T.emit()

        def ln_pass(es, src, B_src_fn, gi, bi_, emit_out):
            ys = [alloc(es, f"lny{i}", [128, 1024], F32) for i in range(2)]
            B_ys = [Buf(f"lny{i}") for i in range(2)]
            sq = [alloc(es, f"lnq{i}", [128, 1024], F32) for i in range(2)]
            B_sq = [Buf(f"lnq{i}") for i in range(2)]
            mean = alloc(es, "lnmean", [128, 1024], F32)
            rstd = alloc(es, "lnrstd", [128, 1024], F32)
            nmr = alloc(es, "lnnmr", [128, 1024], F32)
            B_st = Buf("lnstat")
            ones = alloc(es, "lnones", [128, 128], F32)
            B_ones = Buf("lnones")
            osts = [alloc(es, f"lno{i}", [128, 1024], F32) for i in range(2)]
            B_os = [Buf(f"lno{i}") for i in range(2)]
            for b in B_os:
                b.dsem = T.newsem("ln_" + b.name)
            MS('dve', ones[:, :], 1.0, [B_ones])
            for k in range(32):
                s = k % 2
                DMA(ys[s][:, :], src[k], B_src_fn(k), [B_ys[s]])
                ACT(sq[s][:, :], ys[s][:, :], AF.Square, [B_ys[s]], [B_sq[s]])
                for tg in range(2):
                    MM(banks[tg][:, :], ones[:, :], ys[s][:, tg * 512:(tg + 1) * 512], (k == 0), (k == 31),
                       [B_ones, B_ys[s]], [BB[tg]], acc=(k > 0))
                    MM(banks[2 + tg][:, :], ones[:, :], sq[s][:, tg * 512:(tg + 1) * 512], (k == 0), (k == 31),
                       [B_ones, B_sq[s]], [BB[2 + tg]], acc=(k > 0))
            for tg in range(2):
                sl = slice(tg * 512, (tg + 1) * 512)
                ACT(mean[:, sl], banks[tg][:, :], AF.Copy, [BB[tg]], [B_st], scale=1.0 / 4096, acc=True)
                ACT(rstd[:, sl], banks[2 + tg][:, :], AF.Copy, [BB[2 + tg]], [B_st], scale=1.0 / 4096, acc=True)
            TT('dve', nmr[:, :], mean[:, :], mean[:, :], ALU.mult, [B_st], [B_st])
            TT('dve', rstd[:, :], rstd[:, :], nmr[:, :], ALU.subtract, [B_st], [B_st])
            TS('dve', rstd[:, :], rstd[:, :], LN_EPS, None, ALU.add, None, [B_st], [B_st])
            ACT(rstd[:, :], rstd[:, :], AF.Ln, [B_st], [B_st])
            ACT(rstd[:, :], rstd[:, :], AF.Exp, [B_st], [B_st], scale=-0.5)
            STT('dve', nmr[:, :], mean[:, :], -1.0, rstd[:, :], ALU.mult, ALU.mult, [B_st], [B_st])
            for k in range(32):
                s = k % 2
                DMA(ys[s][:, :], src[k], B_src_fn(k), [B_ys[s]])
                TT('dve', ys[s][:, :], ys[s][:, :], rstd[:, :], ALU.mult, [B_ys[s], B_st], [B_ys[s]])
                TT('pool', ys[s][:, :], ys[s][:, :], nmr[:, :], ALU.add, [B_ys[s], B_st], [B_ys[s]])
                TS('dve' if k % 2 == 0 else 'pool', osts[s][:, :], ys[s][:, :], lnp_t[:, gi * 32 + k:gi * 32 + k + 1],
                   lnp_t[:, bi_ * 32 + k:bi_ * 32 + k + 1], ALU.mult, ALU.add, [B_ys[s], B_lnp], [B_os[s]])
                emit_out(k, osts[s], B_os[s])

        x1T = None
        with ExitStack() as es:
            P = make_pipe(es)
            xres = [alloc(es, f"xres{i}", [128, 1024], F32) for i in range(2)]
            B_xres = [Buf(f"xres{i}") for i in range(2)]
            for b in B_xres:
                b.dsem = T.newsem("xr_" + b.name)
                b.dsem2 = T.newsem("xs_" + b.name)
            stc = {'c': 0}

            def mk_pre(k):
                def pre():
                    s = k % 2
                    DMA(xres[s][:, :], xT[k * 128:(k + 1) * 128, 1024:2048], [], [B_xres[s]], dsem=B_xres[s].dsem)
                return pre

            def mk_ev(k):
                def ev(bset):
                    s = k % 2
                    for tg in range(2):
                        sl = slice(tg * 512, (tg + 1) * 512)
                        STT('dve', xres[s][:, sl], xres[s][:, sl], ALPHA, banks[bset + tg][:, :], ALU.mult, ALU.add,
                            [B_xres[s], BB[bset + tg]], [B_xres[s]])
                    DMA(Ys[k], xres[s][:, :], [B_xres[s]], [B_Ys[k]], dsem=B_xres[s].dsem2)
                return ev

            def rhs_cat(k, tg):
                return catT[:, k, tg * 512:(tg + 1) * 512], [B_cat[k]]
            jobs = [dict(src=w_out[k], KC=32, MC=1, rhs=rhs_cat, pre=mk_pre(k), evac=mk_ev(k)) for k in range(32)]
            gemm(P, jobs)
            T.barrier()
            T.emit()
        esBC.__exit__(None, None, None)
        x1T = alloc(top, "x1T", [128, 32, 1024], BF16)
        B_x1 = [Buf(f"x1_{k}") for k in range(32)]
        with ExitStack() as es:
            def out1(k, t, B_t):
                DMA(X1s[k], t[:, :], [B_t], [B_X1s[k]], dsem=B_t.dsem)
                CP('act', x1T[:, k, :], t[:, :], [B_t], [B_x1[k]])
            ln_pass(es, Ys, lambda k: [B_Ys[k]], 0, 1, out1)
            T.barrier()
            T.emit()

        with ExitStack() as es:
            P = make_pipe(es, nwb=2)
            hT = alloc(es, "hT", [128, FPR, 1024], BF16)
            B_h = [Buf(f"h{i}") for i in range(FPR)]
            rt = [alloc(es, f"rt{i}", [128, 512], F32) for i in range(2)]
            B_rt = [Buf(f"rt{i}") for i in range(2)]
            pst = [alloc(es, f"pst{i}", [128, 2, 1024], F32) for i in range(2)]
            B_pst = [Buf(f"pst{i}") for i in range(2)]
            for b in B_pst:
                b.dsem = T.newsem("pl_" + b.name)
                b.dsem2 = T.newsem("ps_" + b.name)
            std = {'r': 0}

            def rhs_x1(k, tg):
                return x1T[:, k, tg * 512:(tg + 1) * 512], [B_x1[k]]

            def mk_ev1(fcl):
                def ev(bset):
                    for tg in range(2):
                        s = std['r'] % 2
                        std['r'] += 1
                        ACT(rt[s][:, :], banks[bset + tg][:, :], AF.Relu, [BB[bset + tg]], [B_rt[s]])
                        TT('pool', hT[:, fcl, tg * 512:(tg + 1) * 512], rt[s][:, :], rt[s][:, :], ALU.mult,
                           [B_rt[s]], [B_h[fcl]], acc=(tg == 1))
                return ev

            def rhs_h(k, tg):
                return hT[:, k, tg * 512:(tg + 1) * 512], [B_h[k]]

            def mk_pre2(rq, db):
                def pre():
                    s = db % 2
                    if rq == 0:
                        srcv = X1s[2 * db:2 * db + 2].rearrange("c p n -> p c n")
                        rb = [B_X1s[2 * db], B_X1s[2 * db + 1]]
                    else:
                        srcv = Y2s[2 * db:2 * db + 2].rearrange("c p n -> p c n")
                        rb = [B_Y2s[db]]
                    DMA(pst[s][:, :, :], srcv, rb, [B_pst[s]], dsem=B_pst[s].dsem)
                return pre

            def mk_ev2(rq, db):
                def ev(bset):
                    s = db % 2
                    for mc in range(2):
                        for tg in range(2):
                            sl = slice(tg * 512, (tg + 1) * 512)
                            STT('dve', pst[s][:, mc, sl], pst[s][:, mc, sl], (ALPHA if rq == 0 else 1.0),
                                banks[bset + mc * 2 + tg][:, :], ALU.mult, ALU.add,
                                [B_pst[s], BB[bset + mc * 2 + tg]], [B_pst[s]])
                    DMA(Y2s[2 * db:2 * db + 2].rearrange("c p n -> p c n"), pst[s][:, :, :], [B_pst[s]], [B_Y2s[db]],
                        dsem=B_pst[s].dsem2)
                return ev
            for rq in range(NFQ):
                jobs = [dict(src=w_ff1[rq * FPR + f], KC=32, MC=1, rhs=rhs_x1, evac=mk_ev1(f)) for f in range(FPR)]
                gemm(P, jobs)
                jobs = [dict(src=w_ff2[db, rq], KC=FPR, MC=2, rhs=rhs_h, pre=mk_pre2(rq, db), evac=mk_ev2(rq, db))
                        for db in range(16)]
                gemm(P, jobs)
            T.barrier()
            T.emit()

        with ExitStack() as es:
            def out2(k, t, B_t):
                DMA(outT[k * 128:(k + 1) * 128, :], t[:, :], [B_t], [B_out], dsem=B_t.dsem, acc=True)
            ln_pass(es, Y2s, lambda k: [B_Y2s[k // 2]], 2, 3, out2)
            T.barrier()
            T.emit()
    return nc, T


def _blk(w, c0, n=128):
    t = np.zeros((4096, 128), np.float32)
    t[:, :n] = w[:, c0:c0 + n]
    return t.reshape(32, 128, 128).transpose(1, 0, 2).reshape(128, 4096)


def _prep_shared(inp):
    w_in = inp["w_in"][0]
    starts = []
    for h in range(16):
        starts.append(h * 128)
    for j in range(24):
        starts.append(2048 + j * 128)
    for j in range(16):
        starts.append(5168 + j * 128)
    for j in range(16):
        starts.append(7216 + j * 128)
    for j in range(16):
        starts.append(9264 + j * 128)
    w_in_r = np.empty((89, 128, 4096), np.float32)
    for b, c0 in enumerate(starts):
        w_in_r[b] = _blk(w_in, c0)
    w_in_r[88] = _blk(w_in, 5120, 48)
    w_out = inp["w_out"][0]
    w_out_r = np.ascontiguousarray(w_out.reshape(32, 128, 32, 128).transpose(2, 1, 0, 3)).reshape(32, 128, 4096)
    w1 = inp["w_ff1"][0]
    w_ff1_r = np.ascontiguousarray(w1.reshape(32, 128, 128, 128).transpose(2, 1, 0, 3)).reshape(128, 128, 4096)
    w2 = inp["w_ff2"][0]
    w_ff2_r = np.ascontiguousarray(
        w2.reshape(NFQ, FPR, 128, 16, 256).transpose(3, 0, 2, 1, 4)).reshape(16, NFQ, 128, FPR * 256)
    w1c = np.empty((2, 2, 128, 4096), np.float32)
    w2c = np.empty((2, 128, 256), np.float32)
    pec = np.empty((2, 128, 32), np.float32)
    for kv, nm in enumerate(["k", "v"]):
        a = inp["cmp_w1_" + nm][0].reshape(32, 128, 256).transpose(1, 0, 2)
        w1c[kv, 0] = a[:, 0:16, :].reshape(128, 4096)
        w1c[kv, 1] = a[:, 16:32, :].reshape(128, 4096)
        w2c[kv] = inp["cmp_w2_" + nm][0].reshape(2, 128, 128).transpose(1, 0, 2).reshape(128, 256)
        pec[kv] = inp["cmp_pe_" + nm][0].T
    gnsa = np.ascontiguousarray(np.broadcast_to(inp["nsa_out_g"][0].reshape(1, 2048), (128, 2048))).astype(np.float32)
    gdif = np.ascontiguousarray(np.broadcast_to(inp["diff_subln_g"][0].reshape(1, 256), (128, 256))).astype(np.float32)
    lv = np.concatenate([inp["lambda_q1"][0], inp["lambda_k1"][0], inp["lambda_q2"][0], inp["lambda_k2"][0]])
    lamv = np.ascontiguousarray(np.broadcast_to(lv.reshape(1, 512), (128, 512))).astype(np.float32)
    lnp = np.concatenate([inp[n][0].reshape(32, 128).T for n in ["ln1_g", "ln1_b", "ln2_g", "ln2_b"]], axis=1)
    j = np.arange(128)[:, None]
    i = np.arange(128)[None, :]
    u = np.arange(DW)[None, :]
    sh = dict(
        w_in=w_in_r, w_out=w_out_r, w_ff1=w_ff1_r, w_ff2=w_ff2_r, w1c=w1c, w2c=w2c, pec=pec,
        gnsa=gnsa, gdif=gdif, lamv=lamv, lnp=np.ascontiguousarray(lnp.astype(np.float32)),
        c_ident=np.eye(128, dtype=np.float32),
        c_tri=(j <= i).astype(np.float32),
        c_expand=(np.arange(2048)[None, :] // 64 == np.arange(32)[:, None]).astype(np.float32),
        c_dclamp=np.maximum(u - DOFF - j, 0).astype(np.float32),
    )
    return sh


def _prep_core(hh):
    q = np.arange(128)[:, None, None]
    it = np.arange(8)[None, :, None]
    tl = 1024 + 128 * it + q
    n = np.arange(128)[None, None, :]
    okn = (n <= 126) & (16 * n + 31 <= tl)
    if hh == 0:
        okn = okn & (n >= 64)
    cmpmask = np.where(okn, 0.0, NEGB).astype(np.float32).reshape(128, 1024)
    jb = np.arange(32)[None, None, :]
    valid = (64 * jb <= tl)
    if hh == 0:
        valid = valid & (jb >= 16)
    tb = tl // 64
    first = 0 if hh == 1 else 16
    forced = (jb == first) | (jb == tb) | (jb == tb - 1)
    A = np.where(valid & ~forced, 1.0, 0.0)
    Bm = np.where(~valid, -1.0, np.where(forced, 1e6, 0.0))
    ctxb = np.full((128, 1), 0.0 if hh == 1 else NEGB, np.float32)
    return dict(p_cmpmask=cmpmask, p_selA=A.astype(np.float32).reshape(128, 256),
                p_selB=Bm.astype(np.float32).reshape(128, 256), p_ctxb=ctxb)


_CACHE = {}


def kernel(**inputs):
    debug = bool(int(os.environ.get("MK_DEBUG", "0")))
    inp = {k: np.asarray(v) for k, v in inputs.items()}
    x = inp["x"]
    sh = _prep_shared(inp)
    in_maps = []
    for c in range(8):
        b, hh = c // 2, c % 2
        xb = x[b]
        xT = np.zeros((4096, 2048), np.float32)
        if hh == 1:
            xT[:, :] = xb.T
        else:
            xT[:, 1024:] = xb[:1024].T
        m = dict(sh)
        m.update(_prep_core(hh))
        m["xT"] = xT
        in_maps.append(m)
    if 'nc' not in _CACHE:
        _CACHE['nc'] = build(debug)[0]
    nc = _CACHE['nc']
    res = run_bass_kernel_spmd(nc, in_maps, core_ids=list(range(8)))
    out = np.empty((4, 2048, 4096), np.float32)
    for c in range(8):
        b, hh = c // 2, c % 2
        out[b, hh * 1024:(hh + 1) * 1024, :] = res.results[c]["outT"].T
    if debug:
        _CACHE['res'] = res
    return out
```

```python
import os
import numpy as np
import concourse.bass as bass
import concourse.mybir as mybir
from concourse.bass_utils import run_bass_kernel_spmd
from contextlib import ExitStack

F32 = mybir.dt.float32
BF16 = mybir.dt.bfloat16
AF = mybir.ActivationFunctionType
ALU = mybir.AluOpType
AX = mybir.AxisListType

ALPHA = 2.0 ** 0.25
QSCALE = 128.0 ** -0.5
LN_EPS = 1e-5
RMS_EPS = 1e-6
NEGB = -30000.0
NFQ = 8
FPR = 128 // NFQ
DOFF = 896
DW = 3200
ENGS = ['pe', 'act', 'dve', 'pool', 'sp']


class Buf:
    def __init__(self, name):
        self.name = name
        self.w = {}
        self.r = {}
        self.dsem = None


class DSem:
    def __init__(self, h):
        self.h = h
        self.cnt = 0


class Trk:
    def __init__(self, nc):
        self.nc = nc
        self.q = {e: [] for e in ENGS}
        self.sem = {e: nc.alloc_semaphore(name='c_' + e) for e in ENGS if e != 'sp'}
        self.cnt = {e: 0 for e in ENGS}
        self.seen = {e: {} for e in ENGS}
        self.same = True
        self.dsems = []
        self.sems = {}
        for e in self.sem:
            self.sems[id(self.sem[e])] = self.sem[e]
        self.ninst = 0

    def newsem(self, name):
        name = f"{name}_{len(self.dsems)}"
        d = DSem(self.nc.alloc_semaphore(name=name))
        self.dsems.append(d)
        self.sems[id(d.h)] = d.h
        return d

    def _wait(self, eng, deps):
        for sid, val in deps.items():
            sem = self.sems[sid]
            own = (eng != 'sp' and sem is self.sem[eng])
            if own:
                if eng == 'pe' or not self.same or val > self.cnt[eng]:
                    continue
            if self.seen[eng].get(sid, 0) >= val:
                continue
            self.seen[eng][sid] = val
            self.ninst += 1
            self.q[eng].append(lambda h, sem=sem, val=val: h.wait_ge(sem, val))

    def _deps(self, reads, writes, acc):
        deps = {}

        def add(d):
            for s, v in d.items():
                if deps.get(s, 0) < v:
                    deps[s] = v
        for b in reads:
            add(b.w)
        for b in writes:
            add(b.r)
            if not acc:
                add(b.w)
        return deps

    def _record(self, tok, reads, writes, acc):
        s, v = tok
        for b in reads:
            b.r[s] = max(b.r.get(s, 0), v)
        for b in writes:
            if acc:
                b.w[s] = max(b.w.get(s, 0), v)
            else:
                b.w = {s: v}
                b.r = {}

    def op(self, eng, fn, reads=(), writes=(), inc=True, acc=False):
        self._wait(eng, self._deps(reads, writes, acc))
        sem = self.sem[eng]
        self.ninst += 1
        if inc:
            self.cnt[eng] += 1
            tok = (id(sem), self.cnt[eng])
            self.q[eng].append(lambda h, fn=fn, sem=sem: fn(h).then_inc(sem, 1))
        else:
            tok = (id(sem), self.cnt[eng] + 1)
            self.q[eng].append(lambda h, fn=fn: fn(h))
        self._record(tok, reads, writes, acc)

    def dma(self, pairs, reads=(), writes=(), dsem=None, acc=False, qeng='sp'):
        if dsem is None:
            b = writes[0]
            if b.dsem is None:
                b.dsem = self.newsem('d_' + b.name)
            dsem = b.dsem
        self._wait(qeng, self._deps(reads, writes, acc))
        for (o, i) in pairs:
            dsem.cnt += 16
            self.ninst += 1
            self.q[qeng].append(lambda h, o=o, i=i, s=dsem.h: h.dma_start(out=o, in_=i).then_inc(s, 16))
        self._record((id(dsem.h), dsem.cnt), reads, writes, acc)

    def barrier(self):
        deps = {}
        for e in self.sem:
            if self.cnt[e] > 0:
                deps[id(self.sem[e])] = self.cnt[e]
        for d in self.dsems:
            if d.cnt > 0:
                deps[id(d.h)] = d.cnt
        for e in ENGS:
            self._wait(e, deps)

    def emit(self):
        q = self.q
        with self.nc.Block() as block:
            @block.tensor
            def _(h):
                for f in q['pe']:
                    f(h)

            @block.scalar
            def _(h):
                for f in q['act']:
                    f(h)

            @block.vector
            def _(h):
                for f in q['dve']:
                    f(h)

            @block.gpsimd
            def _(h):
                for f in q['pool']:
                    f(h)

            @block.sync
            def _(h):
                for f in q['sp']:
                    f(h)
        self.q = {e: [] for e in ENGS}


def nsa_slope(h):
    return float(2.0 ** (-8.0 * (h + 1) / 16))


def diff_slope(h):
    return float(2.0 ** (-8.0 * (h + 1) / 8))


def build(debug=False):
    nc = bass.Bass("TRN2", target_bir_lowering=False)

    def din(name, shape, dt=F32):
        return nc.dram_tensor(name, shape, dt, kind="ExternalInput").ap()

    def dscr(name, shape, dt):
        return nc.dram_tensor(name, shape, dt, kind=("ExternalOutput" if debug else "Internal")).ap()

    xT = din("xT", [4096, 2048])
    w_in = din("w_in", [89, 128, 4096])
    w_out = din("w_out", [32, 128, 4096])
    w_ff1 = din("w_ff1", [128, 128, 4096])
    w_ff2 = din("w_ff2", [16, NFQ, 128, 4096])
    w1c = din("w1c", [2, 2, 128, 4096])
    w2c = din("w2c", [2, 128, 256])
    pec = din("pec", [2, 128, 32])
    gnsa = din("gnsa", [128, 2048])
    gdif = din("gdif", [128, 256])
    lamv = din("lamv", [128, 512])
    lnp = din("lnp", [128, 128])
    c_ident = din("c_ident", [128, 128])
    c_tri = din("c_tri", [128, 128])
    c_expand = din("c_expand", [32, 2048])
    c_dclamp = din("c_dclamp", [128, DW])
    p_ctxb = din("p_ctxb", [128, 1])
    p_cmpmask = din("p_cmpmask", [128, 1024])
    p_selA = din("p_selA", [128, 256])
    p_selB = din("p_selB", [128, 256])
    outT = nc.dram_tensor("outT", [4096, 1024], F32, kind="ExternalOutput").ap()
    S_in = dscr("S_in", [88, 128, 2048], BF16)
    Ys = dscr("Ys", [32, 128, 1024], F32)
    X1s = dscr("X1s", [32, 128, 1024], F32)
    Y2s = dscr("Y2s", [32, 128, 1024], F32)
    dbg_cat = dscr("dbg_cat", [128, 32 * 1024], BF16) if debug else None

    T = Trk(nc)
    B_out = Buf("outT")
    B_Sin = [Buf(f"Sin{i}") for i in range(88)]
    B_Ys = [Buf(f"Ys{i}") for i in range(32)]
    B_X1s = [Buf(f"X1s{i}") for i in range(32)]
    B_Y2s = [Buf(f"Y2s{i}") for i in range(16)]

    def TT(eng, out, in0, in1, op, reads, writes, **kw):
        T.op(eng, lambda h: h.tensor_tensor(out=out, in0=in0, in1=in1, op=op), reads, writes, **kw)

    def STT(eng, out, in0, scalar, in1, op0, op1, reads, writes, **kw):
        T.op(eng, lambda h: h.scalar_tensor_tensor(out=out, in0=in0, scalar=scalar, in1=in1, op0=op0, op1=op1),
             reads, writes, **kw)

    def TS(eng, out, in0, s1, s2, op0, op1, reads, writes, **kw):
        if op1 is None:
            T.op(eng, lambda h: h.tensor_scalar(out=out, in0=in0, scalar1=s1, scalar2=None, op0=op0), reads, writes, **kw)
        else:
            T.op(eng, lambda h: h.tensor_scalar(out=out, in0=in0, scalar1=s1, scalar2=s2, op0=op0, op1=op1),
                 reads, writes, **kw)

    def ACT(out, in_, func, reads, writes, bias=None, scale=None, accum_out=None, **kw):
        kws = {}
        if bias is not None:
            kws['bias'] = bias
        if scale is not None:
            kws['scale'] = scale
        if accum_out is not None:
            kws['accum_out'] = accum_out
        T.op('act', lambda h: h.activation(out=out, in_=in_, func=func, **kws), reads, writes, **kw)

    def CP(eng, out, in_, reads, writes, **kw):
        if eng == 'act':
            ACT(out, in_, AF.Copy, reads, writes, **kw)
        else:
            T.op(eng, lambda h: h.tensor_copy(out=out, in_=in_), reads, writes, **kw)

    def MS(eng, ap, val, writes):
        T.op(eng, lambda h: h.memset(ap, val), (), writes)

    def MM(out, lhsT, rhs, start, stop, reads, writes, inc=True, acc=False):
        T.op('pe', lambda h: h.matmul(out, lhsT, rhs, start=start, stop=stop), reads, writes, inc=inc, acc=acc)

    def DMA(out, in_, reads, writes, dsem=None, acc=False):
        T.dma([(out, in_)], reads, writes, dsem=dsem, acc=acc)

    def REDUCE(eng, out, in_, op, reads, writes):
        T.op(eng, lambda h: h.tensor_reduce(out=out, in_=in_, axis=AX.X, op=op), reads, writes)

    def RECIP(eng, out, in_, reads, writes):
        T.op(eng, lambda h: h.reciprocal(out=out, in_=in_), reads, writes)

    with ExitStack() as top:
        acnt = {'n': 0}

        def alloc(es, name, shape, dt):
            acnt['n'] += 1
            return es.enter_context(nc.sbuf_tensor(f"{name}_{acnt['n']}", shape, dt))

        banks = [top.enter_context(nc.psum_tensor(f"bank{i}", [128, 512], F32)) for i in range(8)]
        BB = [Buf(f"bank{i}") for i in range(8)]

        gates = alloc(top, "gates", [128, 8, 48], F32)
        B_gates = Buf("gates")
        lnp_t = alloc(top, "lnp_t", [128, 128], F32)
        B_lnp = Buf("lnp")
        identf = alloc(top, "identf", [128, 128], F32)
        identb = alloc(top, "identb", [128, 128], BF16)
        B_ident = Buf("ident")
        zcol = alloc(top, "zcol", [128, 1], F32)
        B_zcol = Buf("zcol")
        DMA(lnp_t[:, :], lnp[:, :], [], [B_lnp])
        DMA(identf[:, :], c_ident[:, :], [], [B_ident])
        CP('dve', identb[:, :], identf[:, :], [B_ident], [B_ident])
        MS('dve', zcol[:, :], 0.0, [B_zcol])

        class Pipe:
            pass

        def make_pipe(es, nwb=3):
            P = Pipe()
            P.stg = [alloc(es, f"stg{i}", [128, 4096], F32) for i in range(2)]
            P.B_stg = [Buf(f"stg{i}") for i in range(2)]
            P.wb = [alloc(es, f"wb{i}", [128, 4096], BF16) for i in range(nwb)]
            P.B_wb = [Buf(f"wb{i}") for i in range(nwb)]
            P.nwb = nwb
            P.jc = 0
            return P

        def gemm(P, jobs):
            n = len(jobs)
            base = P.jc

            def issue_dma(j):
                s = (base + j) % 2
                DMA(P.stg[s][:, :], jobs[j]['src'], [], [P.B_stg[s]])

            def issue_cast(j):
                s = (base + j) % 2
                w = (base + j) % P.nwb
                eng = 'dve' if (base + j) % 2 == 0 else 'pool'
                CP(eng, P.wb[w][:, :], P.stg[s][:, :], [P.B_stg[s]], [P.B_wb[w]])
            issue_dma(0)
            if n > 1:
                issue_dma(1)
            issue_cast(0)
            for j in range(n):
                job = jobs[j]
                if j + 1 < n:
                    issue_cast(j + 1)
                if j + 2 < n:
                    issue_dma(j + 2)
                w = (base + j) % P.nwb
                bset = 4 * ((base + j) % 2)
                if job.get('pre') is not None:
                    job['pre']()
                if job.get('custom') is not None:
                    job['custom'](P.wb[w], P.B_wb[w], bset)
                    continue
                KC, MC = job['KC'], job['MC']
                wv = P.wb[w][:, :].rearrange("p (k c) -> p k c", c=MC * 128)
                for k in range(KC):
                    for mc in range(MC):
                        for tg in range(2):
                            bi = bset + mc * 2 + tg
                            rap, rb = job['rhs'](k, tg)
                            last = (k == KC - 1 and mc == MC - 1 and tg == 1)
                            MM(banks[bi][:, :], wv[:, k, mc * 128:(mc + 1) * 128], rap,
                               start=(k == 0), stop=(k == KC - 1), reads=[P.B_wb[w]] + rb, writes=[BB[bi]],
                               inc=last, acc=(k > 0))
                job['evac'](bset)
            P.jc += n

        with ExitStack() as es:
            P = make_pipe(es)
            actT = alloc(es, "actT", [128, 32, 1024], BF16)
            B_act = [Buf(f"act{i}") for i in range(16)]
            xst = [alloc(es, f"xst{i}", [128, 2, 1024], F32) for i in range(2)]
            B_xst = [Buf(f"xst{i}") for i in range(2)]
            ost = [alloc(es, f"ost{i}", [128, 1024], BF16) for i in range(2)]
            B_ost = [Buf(f"ost{i}") for i in range(2)]
            for b in B_ost:
                b.dsem = T.newsem("st_" + b.name)
            xTv = xT.rearrange("(k p) n -> p k n", p=128)
            st = {'oc': 0}

            def load_x(tok0):
                for j in range(16):
                    s = j % 2
                    DMA(xst[s][:, :, :], xTv[:, 2 * j:2 * j + 2, tok0:tok0 + 1024], [], [B_xst[s]])
                    CP('dve' if j % 2 == 0 else 'pool', actT[:, 2 * j:2 * j + 2, :], xst[s][:, :, :], [B_xst[s]], [B_act[j]])

            def rhs_act(k, tg):
                return actT[:, k, tg * 512:(tg + 1) * 512], [B_act[k // 2]]

            def mk_evac_in(blk, tok0, scale):
                def ev(bset):
                    s = st['oc'] % 2
                    st['oc'] += 1
                    for tg in range(2):
                        ACT(ost[s][:, tg * 512:(tg + 1) * 512], banks[bset + tg][:, :], AF.Copy,
                            [BB[bset + tg]], [B_ost[s]], scale=scale, acc=(tg == 1))
                    DMA(S_in[blk][:, tok0:tok0 + 1024], ost[s][:, :], [B_ost[s]], [B_Sin[blk]], dsem=B_ost[s].dsem, acc=True)
                return ev

            def gate_custom(wbt, B_wbt, bset):
                wv = wbt[:, :].rearrange("p (k c) -> p k c", c=128)
                for i in range(8):
                    bi = bset + (i % 4)
                    for k in range(32):
                        MM(banks[bi][:, 0:128], actT[:, k, i * 128:(i + 1) * 128], wv[:, k, :],
                           start=(k == 0), stop=(k == 31), reads=[B_wbt, B_act[k // 2]], writes=[BB[bi]],
                           inc=(k == 31), acc=(k > 0))
                    ACT(gates[:, i, :], banks[bi][:, 0:48], AF.Sigmoid, [BB[bi]], [B_gates], acc=True)

            kv_blocks = list(range(16, 40)) + list(range(56, 88))
            q_blocks = list(range(0, 16)) + list(range(40, 56))
            load_x(0)
            jobs = [dict(src=w_in[b], KC=32, MC=1, rhs=rhs_act, evac=mk_evac_in(b, 0, 1.0)) for b in kv_blocks]
            gemm(P, jobs)
            load_x(1024)
            jobs = [dict(src=w_in[88], custom=gate_custom)]
            for b in range(88):
                jobs.append(dict(src=w_in[b], KC=32, MC=1, rhs=rhs_act,
                                 evac=mk_evac_in(b, 1024, QSCALE if b in q_blocks else 1.0)))
            gemm(P, jobs)
            T.barrier()
            T.emit()

        esBC = ExitStack()
        esBC.__enter__()
        catT = alloc(esBC, "catT", [128, 32, 1024], BF16)
        B_cat = [Buf(f"cat{i}") for i in range(32)]
        kcT_all = alloc(esBC, "kcT_all", [128, 4, 128], BF16)
        vc_all = alloc(esBC, "vc_all", [128, 4, 128], BF16)
        B_kc = Buf("kc")
        B_vc = Buf("vc")

        with ExitStack() as es:
            stg = alloc(es, "cstg", [128, 4096], F32)
            B_stg = Buf("cstg")
            w1b = [alloc(es, f"w1b{kv}", [128, 32, 256], BF16) for kv in range(2)]
            B_w1b = [Buf(f"w1b{kv}") for kv in range(2)]
            w2f = alloc(es, "w2f", [128, 256], F32)
            w2b = [alloc(es, f"w2b{kv}", [128, 2, 128], BF16) for kv in range(2)]
            B_w2 = [Buf(f"w2_{kv}") for kv in range(2)]
            B_w2f = Buf("w2f")
            pef = alloc(es, "pef", [128, 32], F32)
            peb = [alloc(es, f"peb{kv}", [128, 32], BF16) for kv in range(2)]
            B_pe = [Buf(f"pe{kv}") for kv in range(2)]
            B_pef = Buf("pef")
            cT = [alloc(es, f"cT{i}", [128, 2048], BF16) for i in range(2)]
            B_cT = [Buf(f"cT{i}") for i in range(2)]
            b1 = alloc(es, "b1", [128, 1], F32)
            B_b1 = Buf("b1")
            gx = alloc(es, "gx", [128, 128], F32)
            gw = alloc(es, "gw", [128, 128], F32)
            B_gx = Buf("gx")
            B_gw = Buf("gw")
            hid = [alloc(es, f"hid{hc}", [128, 128], BF16) for hc in range(2)]
            B_hid = [Buf(f"hid{hc}") for hc in range(2)]
            for kv in range(2):
                for half in range(2):
                    DMA(stg[:, :], w1c[kv, half], [], [B_stg])
                    CP('dve', w1b[kv][:, half * 16:(half + 1) * 16, :],
                       stg[:, :].rearrange("p (l h) -> p l h", h=256), [B_stg], [B_w1b[kv]], acc=(half == 1))
                DMA(w2f[:, :], w2c[kv], [], [B_w2f])
                CP('dve', w2b[kv][:, :, :], w2f[:, :].rearrange("p (c d) -> p c d", d=128), [B_w2f], [B_w2[kv]])
                DMA(pef[:, :], pec[kv], [], [B_pef])
                CP('dve', peb[kv][:, :], pef[:, :], [B_pef], [B_pe[kv]])
            MS('dve', kcT_all[:, :, :], 0.0, [B_kc])
            MS('dve', vc_all[:, :, :], 0.0, [B_vc])
            for hc in range(2):
                MS('pool', hid[hc][:, :], 0.0, [B_hid[hc]])
            cnt = 0
            for g in range(4):
                for kv in range(2):
                    blk = 16 + kv * 4 + g
                    s = cnt % 2
                    cnt += 1
                    DMA(cT[s][:, :], S_in[blk], [B_Sin[blk]], [B_cT[s]])
                    for hc in range(2):
                        bi = 6 + (hc % 2)
                        for l in range(32):
                            MM(banks[bi][:, 0:127], w1b[kv][:, l, hc * 128:(hc + 1) * 128],
                               cT[s][:, l:l + 16 * 126 + 1:16], start=(l == 0), stop=(l == 31),
                               reads=[B_w1b[kv], B_cT[s]], writes=[BB[bi]], inc=False, acc=(l > 0))
                        for l in range(32):
                            MM(banks[bi][:, 128:129], w1b[kv][:, l, hc * 128:(hc + 1) * 128],
                               peb[kv][:, l:l + 1], start=(l == 0), stop=(l == 31),
                               reads=[B_w1b[kv], B_pe[kv]], writes=[BB[bi]], inc=(l == 31), acc=True)
                        CP('dve', b1[:, :], banks[bi][:, 128:129], [BB[bi]], [B_b1])
                        ACT(gx[:, 0:127], banks[bi][:, 0:127], AF.Identity, [BB[bi], B_b1], [B_gx], bias=b1[:, 0:1])
                        TT('dve', gw[:, 0:127], gx[:, 0:127], gx[:, 0:127], ALU.mult, [B_gx], [B_gw])
                        TS('dve', gw[:, 0:127], gw[:, 0:127], 0.044715, 1.0, ALU.mult, ALU.add, [B_gw], [B_gw])
                        TT('dve', gw[:, 0:127], gw[:, 0:127], gx[:, 0:127], ALU.mult, [B_gw, B_gx], [B_gw])
                        ACT(gw[:, 0:127], gw[:, 0:127], AF.Tanh, [B_gw], [B_gw], scale=0.7978845608028654)
                        STT('dve', gw[:, 0:127], gw[:, 0:127], 1.0, gx[:, 0:127], ALU.add, ALU.mult, [B_gw, B_gx], [B_gw])
                        TS('dve', hid[hc][:, 0:127], gw[:, 0:127], 0.5, None, ALU.mult, None, [B_gw], [B_hid[hc]])
                    if kv == 0:
                        for hc in range(2):
                            MM(banks[6][:, 0:127], w2b[0][:, hc, :], hid[hc][:, 0:127], start=(hc == 0), stop=(hc == 1),
                               reads=[B_w2[0], B_hid[hc]], writes=[BB[6]], inc=(hc == 1), acc=(hc == 1))
                        CP('act', kcT_all[:, g, 0:127], banks[6][:, 0:127], [BB[6]], [B_kc], acc=True)
                    else:
                        for hc in range(2):
                            MM(banks[6][0:127, 0:128], hid[hc][:, 0:127], w2b[1][:, hc, :], start=(hc == 0), stop=(hc == 1),
                               reads=[B_w2[1], B_hid[hc]], writes=[BB[6]], inc=(hc == 1), acc=(hc == 1))
                        CP('act', vc_all[0:127, g, :], banks[6][0:127, 0:128], [BB[6]], [B_vc], acc=True)
            T.barrier()
            T.emit()

        with ExitStack() as es:
            dcl = alloc(es, "dcl", [128, DW], F32)
            B_dcl = Buf("dcl")
            DMA(dcl[:, :], c_dclamp[:, :], [], [B_dcl])
            tmpf = [alloc(es, f"tmpf{i}", [128, 512], F32) for i in range(2)]
            B_tmpf = [Buf(f"tmpf{i}") for i in range(2)]
            tri = alloc(es, "tri", [128, 128], BF16)
            tri2 = alloc(es, "tri2", [128, 128], BF16)
            B_tri = Buf("tri")
            DMA(tmpf[1][:, 0:128], c_tri[:, :], [], [B_tmpf[1]])
            CP('dve', tri[:, :], tmpf[1][:, 0:128], [B_tmpf[1]], [B_tri])
            TS('dve', tri2[:, :], tmpf[1][:, 0:128], -1.0, 1.0, ALU.mult, ALU.add, [B_tmpf[1]], [B_tri], acc=True)
            expb = alloc(es, "expb", [32, 2048], BF16)
            B_exp = Buf("exp")
            for q4 in range(4):
                DMA(tmpf[0][0:32, :], c_expand[:, q4 * 512:(q4 + 1) * 512], [], [B_tmpf[0]])
                CP('dve', expb[:, q4 * 512:(q4 + 1) * 512], tmpf[0][0:32, :], [B_tmpf[0]], [B_exp], acc=True)
            ctxb = alloc(es, "ctxb", [128, 1], F32)
            B_ctxb = Buf("ctxb")
            DMA(ctxb[:, :], p_ctxb[:, :], [], [B_ctxb])
            cmpmask = alloc(es, "cmpmask", [128, 8, 128], F32)
            B_cm = Buf("cmpmask")
            DMA(cmpmask[:, :, :], p_cmpmask.rearrange("p (i n) -> p i n", n=128), [], [B_cm])
            selA = alloc(es, "selA", [128, 8, 32], F32)
            selB = alloc(es, "selB", [128, 8, 32], F32)
            B_sAB = Buf("selAB")
            DMA(selA[:, :, :], p_selA.rearrange("p (i n) -> p i n", n=32), [], [B_sAB])
            DMA(selB[:, :, :], p_selB.rearrange("p (i n) -> p i n", n=32), [], [B_sAB], acc=True,
                dsem=T.newsem("selB"))
            gn = alloc(es, "gn", [128, 512], F32)
            gd = alloc(es, "gd", [128, 256], F32)
            B_g = Buf("gn")
            B_gd = Buf("gd")
            DMA(gd[:, :], gdif[:, :], [], [B_gd])
            lam_t = alloc(es, "lam_t", [128, 512], F32)
            B_lam = Buf("lam")
            lsc = alloc(es, "lsc", [128, 8], F32)
            DMA(lam_t[:, :], lamv[:, :], [], [B_lam])
            TT('dve', lam_t[:, 0:128], lam_t[:, 0:128], lam_t[:, 128:256], ALU.mult, [B_lam], [B_lam])
            TT('dve', lam_t[:, 256:384], lam_t[:, 256:384], lam_t[:, 384:512], ALU.mult, [B_lam], [B_lam])
            REDUCE('dve', lsc[:, 0:1], lam_t[:, 0:128], ALU.add, [B_lam], [B_lam])
            REDUCE('dve', lsc[:, 1:2], lam_t[:, 256:384], ALU.add, [B_lam], [B_lam])
            ACT(lsc[:, 2:4], lsc[:, 0:2], AF.Exp, [B_lam], [B_lam])
            TT('dve', lsc[:, 4:5], lsc[:, 3:4], lsc[:, 2:3], ALU.subtract, [B_lam], [B_lam])
            TS('dve', lsc[:, 5:6], lsc[:, 4:5], -0.2, None, ALU.add, None, [B_lam], [B_lam])
            neglam = lsc[:, 5:6]

            PT = [alloc(es, f"PT{i}", [128, 512], BF16) for i in range(3)]
            B_PT = [Buf(f"PT{i}") for i in range(3)]
            rg = {'t': 0, 'p': 0, 's': 0, 'm': 0}
            sm = alloc(es, "sm", [128, 64], F32)
            B_sm = Buf("sm")
            sqs = alloc(es, "sqs", [128, 256], F32)
            B_sqs = Buf("sqs")
            ob = alloc(es, "ob", [128, 256], BF16)
            B_ob = Buf("ob")
            of = alloc(es, "of", [128, 256], F32)
            B_of = Buf("of")

            def rms_and_store(src_ap, src_bufs, width, gain_ap, gain_bufs, extra, chunk0, i):
                TT('pool', sqs[:, 0:width], src_ap, src_ap, ALU.mult, src_bufs, [B_sqs])
                REDUCE('dve', sm[:, 0:1], sqs[:, 0:width], ALU.add, [B_sqs], [B_sm])
                TS('dve', sm[:, 0:1], sm[:, 0:1], 1.0 / width, RMS_EPS, ALU.mult, ALU.add, [B_sm], [B_sm])
                ACT(sm[:, 1:2], sm[:, 0:1], AF.Ln, [B_sm], [B_sm])
                ACT(sm[:, 2:3], sm[:, 1:2], AF.Exp, [B_sm], [B_sm], scale=-0.5)
                if extra != 1.0:
                    TS('dve', sm[:, 2:3], sm[:, 2:3], extra, None, ALU.mult, None, [B_sm], [B_sm])
                STT('dve', ob[:, 0:width], src_ap, sm[:, 2:3], gain_ap, ALU.mult, ALU.mult,
                    src_bufs + [B_sm] + gain_bufs, [B_ob])
                for hf in range(width // 128):
                    bi = 6 + (rg['m'] % 2)
                    rg['m'] += 1
                    MM(banks[bi][:, 0:128], ob[:, hf * 128:(hf + 1) * 128], identb[:, :], True, True,
                       [B_ob, B_ident], [BB[bi]])
                    CP('act', catT[:, chunk0 + hf, i * 128:(i + 1) * 128], banks[bi][:, 0:128], [BB[bi]],
                       [B_cat[chunk0 + hf]], acc=True)

            def load_V(VT, B_VT, Vtok, B_Vtok, ncol):
                for kt in range(16):
                    bi = 6 + (rg['m'] % 2)
                    rg['m'] += 1
                    for c in range(ncol):
                        MM(banks[bi][:, c * 128:(c + 1) * 128], VT[c][:, kt * 128:(kt + 1) * 128], identb[:, :], True, True,
                           [B_VT[c], B_ident], [BB[bi]], inc=(c == ncol - 1), acc=(c > 0))
                    CP('act' if kt % 2 == 0 else 'dve', Vtok[:, kt, 0:ncol * 128], banks[bi][:, 0:ncol * 128],
                       [BB[bi]], [B_Vtok], acc=True)

            def dense_attn(nm, QT, B_QT, KT, B_KT, Vtok, B_Vtok, dv, slope, CN, chunks, kt_lo_fn, pair_fn, MT, B_MT,
                           finalize):
                ntile = CN // 128
                for c in chunks:
                    qt0 = 8 + c * ntile
                    kts = list(range(kt_lo_fn(qt0), qt0 + ntile))
                    obank = {}
                    for m in range(nm):
                        for ip in range(ntile):
                            obank[(m, ip)] = 2 + m * ntile + ip
                    first = {}
                    sbank = {}

                    def emit_S(kt_):
                        sb_ = rg['s'] % 2
                        rg['s'] += 1
                        sbank[kt_] = sb_
                        for m in range(nm):
                            MM(banks[sb_][:, m * CN:(m + 1) * CN], KT[m][:, kt_ * 128:(kt_ + 1) * 128],
                               QT[m][:, c * CN:(c + 1) * CN], True, True, [B_KT[m], B_QT[m]], [BB[sb_]],
                               inc=(m == nm - 1), acc=(m > 0))
                    emit_S(kts[0])
                    for kidx, kt in enumerate(kts):
                        if kidx + 1 < len(kts):
                            emit_S(kts[kidx + 1])
                        sbk = sbank[kt]
                        u0 = 1024 + c * CN - kt * 128 + DOFF
                        tf = rg['t'] % 2
                        rg['t'] += 1
                        if nm == 1:
                            STT('dve', tmpf[tf][:, 0:CN], dcl[:, u0:u0 + CN], -slope, banks[sbk][:, 0:CN],
                                ALU.mult, ALU.add, [B_dcl, BB[sbk]], [B_tmpf[tf]])
                        else:
                            STT('dve', tmpf[tf][:, 0:nm * CN].rearrange("p (m n) -> p m n", m=nm),
                                dcl[:, u0:u0 + CN].unsqueeze(1).broadcast_to([128, nm, CN]), -slope,
                                banks[sbk][:, 0:nm * CN].rearrange("p (m n) -> p m n", m=nm),
                                ALU.mult, ALU.add, [B_dcl, BB[sbk]], [B_tmpf[tf]])
                        pi = rg['p'] % 3
                        rg['p'] += 1
                        ACT(PT[pi][:, 0:nm * CN], tmpf[tf][:, 0:nm * CN], AF.Exp, [B_tmpf[tf], B_ctxb, B_zcol],
                            [B_PT[pi]], bias=(ctxb[:, 0:1] if kt < 8 else zcol[:, 0:1]))
                        if MT is not None:
                            TT('pool', PT[pi][:, 0:CN], PT[pi][:, 0:CN], MT[:, kt, :], ALU.mult,
                               [B_PT[pi], B_MT], [B_PT[pi]])
                        for ip in range(ntile):
                            qt = qt0 + ip
                            pm = pair_fn(kt, qt)
                            if pm is None or pm == 'full':
                                continue
                            msk = tri if pm == 'tri' else tri2
                            if nm == 1:
                                TT('pool', PT[pi][:, ip * 128:(ip + 1) * 128], PT[pi][:, ip * 128:(ip + 1) * 128],
                                   msk[:, :], ALU.mult, [B_PT[pi], B_tri], [B_PT[pi]])
                            else:
                                v = PT[pi][:, 0:nm * CN].rearrange("p (m n) -> p m n", m=nm)[:, :, ip * 128:(ip + 1) * 128]
                                TT('pool', v, v, msk[:, :].unsqueeze(1).broadcast_to([128, nm, 128]), ALU.mult,
                                   [B_PT[pi], B_tri], [B_PT[pi]])
                        avs = []
                        for m in range(nm):
                            for ip in range(ntile):
                                qt = qt0 + ip
                                if pair_fn(kt, qt) is None:
                                    continue
                                avs.append((m, ip))
                        for idx, (m, ip) in enumerate(avs):
                            qt = qt0 + ip
                            ob_ = obank[(m, ip)]
                            st_ = (m, ip) not in first
                            first[(m, ip)] = True
                            MM(banks[ob_][:, 0:dv + 1], PT[pi][:, m * CN + ip * 128:m * CN + (ip + 1) * 128],
                               Vtok[:, kt, 0:dv + 1], st_, (kt == qt), [B_PT[pi], B_Vtok], [BB[ob_]],
                               inc=(idx == len(avs) - 1), acc=(not st_))
                    finalize([(c * ntile + ip, [obank[(m, ip)] for m in range(nm)]) for ip in range(ntile)])

            with ExitStack() as es2:
                QT = [alloc(es2, f"QT{r}", [128, 1024], BF16) for r in range(4)]
                B_QT = [Buf(f"QT{r}") for r in range(4)]
                sK = alloc(es2, "sK", [128, 2048], BF16)
                wK = alloc(es2, "wK", [128, 2048], BF16)
                B_sK = Buf("sK")
                B_wK = Buf("wK")
                VTs = [alloc(es2, "VTs0", [128, 2048], BF16)] * 2
                B_VTs = [Buf("VTs0")] * 2
                sV = alloc(es2, "sV", [128, 16, 132], BF16)
                wV = alloc(es2, "wV", [128, 16, 132], BF16)
                B_sV = Buf("sV")
                B_wV = Buf("wV")
                MS('pool', sV[:, :, 128:129], 1.0, [B_sV])
                MS('pool', wV[:, :, 128:129], 1.0, [B_wV])
                acc = alloc(es2, "acc", [128, 8, 4, 128], F32)
                B_acc = [Buf(f"acc{i}") for i in range(8)]
                MT = alloc(es2, "MT", [128, 16, 512], BF16)
                B_MT = Buf("MT")
                NL = 2
                E4 = [alloc(es2, f"E4{j}", [128, 4, 128], F32) for j in range(NL)]
                B_E4 = [Buf(f"E4{j}") for j in range(NL)]
                pb4 = [alloc(es2, f"pb4{j}", [128, 4, 128], BF16) for j in range(NL)]
                B_pb4 = [Buf(f"pb4{j}") for j in range(NL)]
                pT4 = [alloc(es2, f"pT4{j}", [128, 4, 128], BF16) for j in range(NL)]
                B_pT4 = [Buf(f"pT4{j}") for j in range(NL)]
                rs4 = [alloc(es2, f"rs4{j}", [128, 8], F32) for j in range(NL)]
                B_rs4 = [Buf(f"rs4{j}") for j in range(NL)]
                Ppad = [alloc(es2, f"Ppad{j}", [128, 132], F32) for j in range(NL)]
                B_Pp = [Buf(f"Ppad{j}") for j in range(NL)]
                for j in range(NL):
                    MS('dve', Ppad[j][:, :], 0.0, [B_Pp[j]])
                impt = [alloc(es2, f"impt{j}", [128, 3, 32], F32) for j in range(NL)]
                B_imp = [Buf(f"imp{j}") for j in range(NL)]
                c3 = [alloc(es2, f"c3{j}", [128, 32, 32], F32) for j in range(NL)]
                B_c3 = [Buf(f"c3{j}") for j in range(NL)]
                selb = [alloc(es2, f"selb{j}", [128, 32], BF16) for j in range(NL)]
                B_selb = [Buf(f"selb{j}") for j in range(NL)]
                selT = alloc(es2, "selT", [32, 1024], BF16)
                B_selT = Buf("selT")
                rq4 = [alloc(es2, f"rq4{j}", [128, 12], F32) for j in range(4)]
                B_rq4 = [Buf(f"rq4{j}") for j in range(4)]
                fsm = [alloc(es2, f"fsm{j}", [128, 4], F32) for j in range(4)]
                B_fsm = [Buf(f"fsm{j}") for j in range(4)]

                def v4(ap):
                    return ap.rearrange("p (r n) -> p r n", r=4)

                def fin_add(items, r, h, br):
                    for ip, (i, obs) in enumerate(items):
                        TS('dve', fsm[ip][:, 0:1], banks[obs[0]][:, 128:129], 1e-30, None, ALU.add, None,
                           [BB[obs[0]]], [B_fsm[ip]])
                    for ip, (i, obs) in enumerate(items):
                        RECIP('dve', fsm[ip][:, 1:2], fsm[ip][:, 0:1], [B_fsm[ip]], [B_fsm[ip]])
                    for ip, (i, obs) in enumerate(items):
                        TT('dve', fsm[ip][:, 2:3], fsm[ip][:, 1:2], gates[:, i, h * 3 + br:h * 3 + br + 1], ALU.mult,
                           [B_fsm[ip], B_gates], [B_fsm[ip]])
                    for ip, (i, obs) in enumerate(items):
                        STT('dve', acc[:, i, r, :], banks[obs[0]][:, 0:128], fsm[ip][:, 2:3], acc[:, i, r, :],
                            ALU.mult, ALU.add, [BB[obs[0]], B_fsm[ip], B_acc[i]], [B_acc[i]])

                def rms_batch(g, c):
                    sq = [E4[0][:, :, :], E4[1][:, :, :], v4(tmpf[0][:, :]), v4(tmpf[1][:, :])]
                    B_sq = [B_E4[0], B_E4[1], B_tmpf[0], B_tmpf[1]]
                    obt = [pb4[0], pb4[1], pT4[0], pT4[1]]
                    B_obt = [B_pb4[0], B_pb4[1], B_pT4[0], B_pT4[1]]
                    bk = [6, 7, 0, 1]
                    its = [(ip, 4 * c + ip) for ip in range(4)]
                    for ip, i in its:
                        TT('pool', sq[ip], acc[:, i, :, :], acc[:, i, :, :], ALU.mult, [B_acc[i]], [B_sq[ip]])
                    for ip, i in its:
                        REDUCE('dve', rq4[ip][:, 0:4], sq[ip], ALU.add, [B_sq[ip]], [B_rq4[ip]])
                    for ip, i in its:
                        TS('dve', rq4[ip][:, 0:4], rq4[ip][:, 0:4], 1.0 / 128, RMS_EPS, ALU.mult, ALU.add,
                           [B_rq4[ip]], [B_rq4[ip]])
                    for ip, i in its:
                        ACT(rq4[ip][:, 4:8], rq4[ip][:, 0:4], AF.Ln, [B_rq4[ip]], [B_rq4[ip]])
                    for ip, i in its:
                        ACT(rq4[ip][:, 8:12], rq4[ip][:, 4:8], AF.Exp, [B_rq4[ip]], [B_rq4[ip]], scale=-0.5)
                    for ip, i in its:
                        TT('dve', sq[ip], acc[:, i, :, :], rq4[ip][:, 8:12].unsqueeze(2).broadcast_to([128, 4, 128]),
                           ALU.mult, [B_acc[i], B_rq4[ip]], [B_sq[ip]])
                    for ip, i in its:
                        TT('pool', obt[ip][:, :, :], sq[ip], v4(gn[:, :]), ALU.mult, [B_sq[ip], B_g], [B_obt[ip]])
                    for ip, i in its:
                        for r in range(4):
                            MM(banks[bk[ip]][:, r * 128:(r + 1) * 128], obt[ip][:, r, :], identb[:, :], True, True,
                               [B_obt[ip], B_ident], [BB[bk[ip]]], inc=(r == 3), acc=(r > 0))
                    for ip, i in its:
                        CP('act', catT[:, 4 * g:4 * g + 4, i * 128:(i + 1) * 128], v4(banks[bk[ip]][:, :]),
                           [BB[bk[ip]]], [B_cat[4 * g + r] for r in range(4)], acc=True)

                def pair_win(kt, qt):
                    if kt == qt:
                        return 'tri'
                    if kt == qt - 4:
                        return 'tri2'
                    if qt - 4 < kt < qt:
                        return 'full'
                    return None

                for g in range(4):
                    DMA(gn[:, :], gnsa[:, g * 512:(g + 1) * 512], [], [B_g])
                    for r in range(4):
                        DMA(QT[r][:, :], S_in[g * 4 + r][:, 1024:2048], [B_Sin[g * 4 + r]], [B_QT[r]])
                    DMA(sK[:, :], S_in[16 + 2 * 4 + g], [B_Sin[16 + 8 + g]], [B_sK])
                    DMA(wK[:, :], S_in[16 + 4 * 4 + g], [B_Sin[16 + 16 + g]], [B_wK])
                    DMA(VTs[0][:, :], S_in[16 + 3 * 4 + g], [B_Sin[16 + 12 + g]], [B_VTs[0]])
                    load_V([VTs[0]], [B_VTs[0]], sV, B_sV, 1)
                    DMA(VTs[1][:, :], S_in[16 + 5 * 4 + g], [B_Sin[16 + 20 + g]], [B_VTs[1]])
                    load_V([VTs[1]], [B_VTs[1]], wV, B_wV, 1)
                    for i0 in range(0, 8, NL):
                        ch = [(j, i0 + j) for j in range(NL)]
                        for j, i in ch:
                            A = 2 + 3 * j
                            for r in range(4):
                                MM(banks[A][:, r * 128:(r + 1) * 128], QT[r][:, i * 128:(i + 1) * 128], kcT_all[:, g, :],
                                   True, True, [B_QT[r], B_kc], [BB[A]], inc=(r == 3), acc=(r > 0))
                        for j, i in ch:
                            A = 2 + 3 * j
                            TT('dve', E4[j][:, :, :], v4(banks[A][:, :]),
                               cmpmask[:, i, :].unsqueeze(1).broadcast_to([128, 4, 128]), ALU.add,
                               [BB[A], B_cm], [B_E4[j]])
                        for j, i in ch:
                            ACT(E4[j][:, :, :], E4[j][:, :, :], AF.Exp, [B_E4[j]], [B_E4[j]])
                        for j, i in ch:
                            REDUCE('dve', rs4[j][:, 0:4], E4[j][:, :, :], ALU.add, [B_E4[j]], [B_rs4[j]])
                        for j, i in ch:
                            TS('dve', rs4[j][:, 0:4], rs4[j][:, 0:4], 1e-30, None, ALU.add, None, [B_rs4[j]], [B_rs4[j]])
                        for j, i in ch:
                            RECIP('dve', rs4[j][:, 4:8], rs4[j][:, 0:4], [B_rs4[j]], [B_rs4[j]])
                        for j, i in ch:
                            TT('dve', E4[j][:, :, :], E4[j][:, :, :],
                               rs4[j][:, 4:8].unsqueeze(2).broadcast_to([128, 4, 128]), ALU.mult,
                               [B_E4[j], B_rs4[j]], [B_E4[j]])
                        for j, i in ch:
                            REDUCE('dve', Ppad[j][:, 1:129], E4[j][:, :, :].rearrange("p r n -> p n r"), ALU.add,
                                   [B_E4[j]], [B_Pp[j]])
                        for j, i in ch:
                            CP('pool', pb4[j][:, :, :], E4[j][:, :, :], [B_E4[j]], [B_pb4[j]])
                        for j, i in ch:
                            Bk = 3 + 3 * j
                            for r in range(4):
                                MM(banks[Bk][:, r * 128:(r + 1) * 128], pb4[j][:, r, :], identb[:, :], True, True,
                                   [B_pb4[j], B_ident], [BB[Bk]], inc=(r == 3), acc=(r > 0))
                        for j, i in ch:
                            Bk = 3 + 3 * j
                            CP('act', pT4[j][:, :, :], v4(banks[Bk][:, :]), [BB[Bk]], [B_pT4[j]])
                        for j, i in ch:
                            Ck = 4 + 3 * j
                            for r in range(4):
                                MM(banks[Ck][:, r * 128:(r + 1) * 128], pT4[j][:, r, :], vc_all[:, g, :], True, True,
                                   [B_pT4[j], B_vc], [BB[Ck]], inc=(r == 3), acc=(r > 0))
                        for j, i in ch:
                            Ck = 4 + 3 * j
                            gv = gates[:, i, g * 12:(g + 1) * 12].rearrange("p (r b) -> p r b", b=3)[:, :, 0:1]
                            TT('dve', acc[:, i, :, :], v4(banks[Ck][:, :]), gv.broadcast_to([128, 4, 128]), ALU.mult,
                               [BB[Ck], B_gates], [B_acc[i]])
                        for j, i in ch:
                            Av = Ppad[j][:, 1:129].rearrange("p (j f) -> p j f", f=4)
                            TT('dve', impt[j][:, 0, :], Av[:, :, 0], Av[:, :, 1], ALU.add, [B_Pp[j]], [B_imp[j]])
                        for j, i in ch:
                            Av = Ppad[j][:, 1:129].rearrange("p (j f) -> p j f", f=4)
                            Bv = Ppad[j][:, 0:128].rearrange("p (j f) -> p j f", f=4)
                            TT('dve', impt[j][:, 1, :], Av[:, :, 3], Bv[:, :, 0], ALU.add, [B_Pp[j], B_imp[j]], [B_imp[j]])
                        for j, i in ch:
                            Av = Ppad[j][:, 1:129].rearrange("p (j f) -> p j f", f=4)
                            TT('dve', impt[j][:, 0, :], impt[j][:, 0, :], Av[:, :, 2], ALU.add, [B_Pp[j], B_imp[j]], [B_imp[j]])
                        for j, i in ch:
                            STT('dve', impt[j][:, 0, :], impt[j][:, 1, :], 0.5, impt[j][:, 0, :], ALU.mult, ALU.add,
                                [B_imp[j]], [B_imp[j]])
                        for j, i in ch:
                            TT('dve', impt[j][:, 0, :], impt[j][:, 0, :], selA[:, i, :], ALU.mult, [B_imp[j], B_sAB], [B_imp[j]])
                        for j, i in ch:
                            TT('dve', impt[j][:, 2, :], impt[j][:, 0, :], selB[:, i, :], ALU.add, [B_imp[j], B_sAB], [B_imp[j]])
                        for j, i in ch:
                            sc = impt[j][:, 2, :]
                            TT('dve', c3[j][:, :, :], sc.unsqueeze(1).broadcast_to([128, 32, 32]),
                               sc.unsqueeze(2).broadcast_to([128, 32, 32]), ALU.is_gt, [B_imp[j]], [B_c3[j]])
                        for j, i in ch:
                            REDUCE('dve', impt[j][:, 1, :], c3[j][:, :, :], ALU.add, [B_c3[j]], [B_imp[j]])
                        for j, i in ch:
                            TS('dve', selb[j][:, :], impt[j][:, 1, :], 15.5, None, ALU.is_lt, None, [B_imp[j]], [B_selb[j]])
                        for j, i in ch:
                            MM(banks[j][0:32, 0:128], selb[j][:, :], identb[:, :], True, True, [B_selb[j], B_ident], [BB[j]])
                        for j, i in ch:
                            CP('act', selT[:, i * 128:(i + 1) * 128], banks[j][0:32, 0:128], [BB[j]], [B_selT], acc=True)
                    for c in range(2):
                        qt0 = 8 + 4 * c
                        for kt in range(0, qt0 + 4):
                            bi = 6 + (rg['m'] % 2)
                            rg['m'] += 1
                            MM(banks[bi][:, :], expb[:, kt * 128:(kt + 1) * 128], selT[:, c * 512:(c + 1) * 512],
                               True, True, [B_exp, B_selT], [BB[bi]])
                            CP('act', MT[:, kt, :], banks[bi][:, :], [BB[bi]], [B_MT], acc=True)
                            if kt >= qt0:
                                ip = kt - qt0
                                TT('pool', MT[:, kt, ip * 128:(ip + 1) * 128], MT[:, kt, ip * 128:(ip + 1) * 128],
                                   tri[:, :], ALU.mult, [B_MT, B_tri], [B_MT])
                        for r in range(4):
                            h = g * 4 + r
                            dense_attn(1, [QT[r]], [B_QT[r]], [sK], [B_sK], sV, B_sV, 128, nsa_slope(h), 512, [c],
                                       lambda qt0_: 0, lambda kt, qt: ('full' if kt <= qt else None), MT, B_MT,
                                       lambda items, r=r, h=h: fin_add(items, r, h, 1))
                    for c in range(2):
                        for r in range(4):
                            h = g * 4 + r
                            dense_attn(1, [QT[r]], [B_QT[r]], [wK], [B_wK], wV, B_wV, 128, nsa_slope(h), 512, [c],
                                       lambda qt0_: qt0_ - 4, pair_win, None, None,
                                       lambda items, r=r, h=h: fin_add(items, r, h, 2))
                        rms_batch(g, c)
                T.barrier()
                T.emit()

            with ExitStack() as es2:
                QT = [alloc(es2, f"dQT{m}", [128, 1024], BF16) for m in range(2)]
                B_QT = [Buf(f"dQT{m}") for m in range(2)]
                KT = [alloc(es2, f"dKT{m}", [128, 2048], BF16) for m in range(2)]
                B_KT = [Buf(f"dKT{m}") for m in range(2)]
                VT = [alloc(es2, f"dVT{m}", [128, 2048], BF16) for m in range(2)]
                B_VT = [Buf(f"dVT{m}") for m in range(2)]
                dV = alloc(es2, "dV", [128, 16, 260], BF16)
                B_dV = Buf("dV")
                MS('pool', dV[:, :, 256:257], 1.0, [B_dV])
                of2 = [alloc(es2, f"of2{j}", [128, 256], F32) for j in range(2)]
                B_of2 = [Buf(f"of2{j}") for j in range(2)]
                sq2 = [alloc(es2, f"sq2{j}", [128, 256], F32) for j in range(2)]
                B_sq2 = [Buf(f"sq2{j}") for j in range(2)]
                ob2 = [alloc(es2, f"ob2{j}", [128, 256], BF16) for j in range(2)]
                B_ob2 = [Buf(f"ob2{j}") for j in range(2)]
                d8 = [alloc(es2, f"d8{j}", [128, 8], F32) for j in range(2)]
                B_d8 = [Buf(f"d8{j}") for j in range(2)]

                def fin_diff(items, h):
                    E = list(enumerate(items))
                    for ip, (i, obs) in E:
                        TS('dve', d8[ip][:, 0:1], banks[obs[0]][:, 256:257], 1e-30, None, ALU.add, None, [BB[obs[0]]], [B_d8[ip]])
                    for ip, (i, obs) in E:
                        TS('dve', d8[ip][:, 2:3], banks[obs[1]][:, 256:257], 1e-30, None, ALU.add, None, [BB[obs[1]]], [B_d8[ip]])
                    for ip, (i, obs) in E:
                        RECIP('dve', d8[ip][:, 1:2], d8[ip][:, 0:1], [B_d8[ip]], [B_d8[ip]])
                    for ip, (i, obs) in E:
                        RECIP('dve', d8[ip][:, 3:4], d8[ip][:, 2:3], [B_d8[ip]], [B_d8[ip]])
                    for ip, (i, obs) in E:
                        TT('dve', d8[ip][:, 4:5], d8[ip][:, 3:4], neglam, ALU.mult, [B_d8[ip], B_lam], [B_d8[ip]])
                    for ip, (i, obs) in E:
                        TS('dve', of2[ip][:, :], banks[obs[0]][:, 0:256], d8[ip][:, 1:2], None, ALU.mult, None,
                           [BB[obs[0]], B_d8[ip]], [B_of2[ip]])
                    for ip, (i, obs) in E:
                        STT('dve', of2[ip][:, :], banks[obs[1]][:, 0:256], d8[ip][:, 4:5], of2[ip][:, :], ALU.mult, ALU.add,
                            [BB[obs[1]], B_d8[ip], B_of2[ip]], [B_of2[ip]])
                    for ip, (i, obs) in E:
                        TT('pool', sq2[ip][:, :], of2[ip][:, :], of2[ip][:, :], ALU.mult, [B_of2[ip]], [B_sq2[ip]])
                    for ip, (i, obs) in E:
                        REDUCE('dve', d8[ip][:, 5:6], sq2[ip][:, :], ALU.add, [B_sq2[ip]], [B_d8[ip]])
                    for ip, (i, obs) in E:
                        TS('dve', d8[ip][:, 5:6], d8[ip][:, 5:6], 1.0 / 256, RMS_EPS, ALU.mult, ALU.add, [B_d8[ip]], [B_d8[ip]])
                    for ip, (i, obs) in E:
                        ACT(d8[ip][:, 6:7], d8[ip][:, 5:6], AF.Ln, [B_d8[ip]], [B_d8[ip]])
                    for ip, (i, obs) in E:
                        ACT(d8[ip][:, 7:8], d8[ip][:, 6:7], AF.Exp, [B_d8[ip]], [B_d8[ip]], scale=-0.5)
                    for ip, (i, obs) in E:
                        TS('dve', d8[ip][:, 7:8], d8[ip][:, 7:8], 0.8, None, ALU.mult, None, [B_d8[ip]], [B_d8[ip]])
                    for ip, (i, obs) in E:
                        STT('dve', ob2[ip][:, :], of2[ip][:, :], d8[ip][:, 7:8], gd[:, :], ALU.mult, ALU.mult,
                            [B_of2[ip], B_d8[ip], B_gd], [B_ob2[ip]])
                    for ip, (i, obs) in E:
                        bk = 6 + ip
                        for hf in range(2):
                            MM(banks[bk][:, hf * 128:(hf + 1) * 128], ob2[ip][:, hf * 128:(hf + 1) * 128], identb[:, :],
                               True, True, [B_ob2[ip], B_ident], [BB[bk]], inc=(hf == 1), acc=(hf > 0))
                    for ip, (i, obs) in E:
                        bk = 6 + ip
                        CP('act', catT[:, 16 + 2 * h:16 + 2 * h + 2, i * 128:(i + 1) * 128],
                           banks[bk][:, 0:256].rearrange("p (c q) -> p c q", c=2), [BB[bk]],
                           [B_cat[16 + 2 * h], B_cat[16 + 2 * h + 1]], acc=True)

                for h in range(8):
                    for m in range(2):
                        DMA(QT[m][:, :], S_in[40 + 2 * h + m][:, 1024:2048], [B_Sin[40 + 2 * h + m]], [B_QT[m]])
                        DMA(KT[m][:, :], S_in[56 + 2 * h + m], [B_Sin[56 + 2 * h + m]], [B_KT[m]])
                        DMA(VT[m][:, :], S_in[72 + 2 * h + m], [B_Sin[72 + 2 * h + m]], [B_VT[m]])
                    load_V(VT, B_VT, dV, B_dV, 2)
                    dense_attn(2, QT, B_QT, KT, B_KT, dV, B_dV, 256, diff_slope(h), 256, [0, 1, 2, 3],
                               lambda qt0_: 0, lambda kt, qt: ('tri' if kt == qt else ('full' if kt < qt else None)),
                               None, None, lambda items, h=h: fin_diff(items, h))
                if debug:
                    DMA(dbg_cat[:, :], catT[:, :, :].rearrange("p k n -> p (k n)"), B_cat, [Buf("dbgc")])
                T.barrier()
                T.emit()

        def ln_pass(es, src, B_src_fn, gi, bi_, emit_out):
            ys = [alloc(es, f"lny{i}", [128, 1024], F32) for i in range(2)]
            B_ys = [Buf(f"lny{i}") for i in range(2)]
            sq = [alloc(es, f"lnq{i}", [128, 1024], F32) for i in range(2)]
            B_sq = [Buf(f"lnq{i}") for i in range(2)]
            mean = alloc(es, "lnmean", [128, 1024], F32)
            rstd = alloc(es, "lnrstd", [128, 1024], F32)
            nmr = alloc(es, "lnnmr", [128, 1024], F32)
            B_st = Buf("lnstat")
            ones = alloc(es, "lnones", [128, 128], F32)
            B_ones = Buf("lnones")
            osts = [alloc(es, f"lno{i}", [128, 1024], F32) for i in range(2)]
            B_os = [Buf(f"lno{i}") for i in range(2)]
            for b in B_os:
                b.dsem = T.newsem("ln_" + b.name)
            MS('dve', ones[:, :], 1.0, [B_ones])
            for k in range(32):
                s = k % 2
                DMA(ys[s][:, :], src[k], B_src_fn(k), [B_ys[s]])
                ACT(sq[s][:, :], ys[s][:, :], AF.Square, [B_ys[s]], [B_sq[s]])
                for tg in range(2):
                    MM(banks[tg][:, :], ones[:, :], ys[s][:, tg * 512:(tg + 1) * 512], (k == 0), (k == 31),
                       [B_ones, B_ys[s]], [BB[tg]], acc=(k > 0))
                    MM(banks[2 + tg][:, :], ones[:, :], sq[s][:, tg * 512:(tg + 1) * 512], (k == 0), (k == 31),
                       [B_ones, B_sq[s]], [BB[2 + tg]], acc=(k > 0))
            for tg in range(2):
                sl = slice(tg * 512, (tg + 1) * 512)
                ACT(mean[:, sl], banks[tg][:, :], AF.Copy, [BB[tg]], [B_st], scale=1.0 / 4096, acc=True)
                ACT(rstd[:, sl], banks[2 + tg][:, :], AF.Copy, [BB[2 + tg]], [B_st], scale=1.0 / 4096, acc=True)
            TT('dve', nmr[:, :], mean[:, :], mean[:, :], ALU.mult, [B_st], [B_st])
            TT('dve', rstd[:, :], rstd[:, :], nmr[:, :], ALU.subtract, [B_st], [B_st])
            TS('dve', rstd[:, :], rstd[:, :], LN_EPS, None, ALU.add, None, [B_st], [B_st])
            ACT(rstd[:, :], rstd[:, :], AF.Ln, [B_st], [B_st])
            ACT(rstd[:, :], rstd[:, :], AF.Exp, [B_st], [B_st], scale=-0.5)
            STT('dve', nmr[:, :], mean[:, :], -1.0, rstd[:, :], ALU.mult, ALU.mult, [B_st], [B_st])
            for k in range(32):
                s = k % 2
                DMA(ys[s][:, :], src[k], B_src_fn(k), [B_ys[s]])
                TT('dve', ys[s][:, :], ys[s][:, :], rstd[:, :], ALU.mult, [B_ys[s], B_st], [B_ys[s]])
                TT('pool', ys[s][:, :], ys[s][:, :], nmr[:, :], ALU.add, [B_ys[s], B_st], [B_ys[s]])
                TS('dve' if k % 2 == 0 else 'pool', osts[s][:, :], ys[s][:, :], lnp_t[:, gi * 32 + k:gi * 32 + k + 1],
                   lnp_t[:, bi_ * 32 + k:bi_ * 32 + k + 1], ALU.mult, ALU.add, [B_ys[s], B_lnp], [B_os[s]])
                emit_out(k, osts[s], B_os[s])

        x1T = None
        with ExitStack() as es:
            P = make_pipe(es)
            xres = [alloc(es, f"xres{i}", [128, 1024], F32) for i in range(2)]
            B_xres = [Buf(f"xres{i}") for i in range(2)]
            for b in B_xres:
                b.dsem = T.newsem("xr_" + b.name)
                b.dsem2 = T.newsem("xs_" + b.name)
            stc = {'c': 0}

            def mk_pre(k):
                def pre():
                    s = k % 2
                    DMA(xres[s][:, :], xT[k * 128:(k + 1) * 128, 1024:2048], [], [B_xres[s]], dsem=B_xres[s].dsem)
                return pre

            def mk_ev(k):
                def ev(bset):
                    s = k % 2
                    for tg in range(2):
                        sl = slice(tg * 512, (tg + 1) * 512)
                        STT('dve', xres[s][:, sl], xres[s][:, sl], ALPHA, banks[bset + tg][:, :], ALU.mult, ALU.add,
                            [B_xres[s], BB[bset + tg]], [B_xres[s]])
                    DMA(Ys[k], xres[s][:, :], [B_xres[s]], [B_Ys[k]], dsem=B_xres[s].dsem2)
                return ev

            def rhs_cat(k, tg):
                return catT[:, k, tg * 512:(tg + 1) * 512], [B_cat[k]]
            jobs = [dict(src=w_out[k], KC=32, MC=1, rhs=rhs_cat, pre=mk_pre(k), evac=mk_ev(k)) for k in range(32)]
            gemm(P, jobs)
            T.barrier()
            T.emit()
        esBC.__exit__(None, None, None)
        x1T = alloc(top, "x1T", [128, 32, 1024], BF16)
        B_x1 = [Buf(f"x1_{k}") for k in range(32)]
        with ExitStack() as es:
            def out1(k, t, B_t):
                DMA(X1s[k], t[:, :], [B_t], [B_X1s[k]], dsem=B_t.dsem)
                CP('act', x1T[:, k, :], t[:, :], [B_t], [B_x1[k]])
            ln_pass(es, Ys, lambda k: [B_Ys[k]], 0, 1, out1)
            T.barrier()
            T.emit()

        with ExitStack() as es:
            P = make_pipe(es, nwb=2)
            hT = alloc(es, "hT", [128, FPR, 1024], BF16)
            B_h = [Buf(f"h{i}") for i in range(FPR)]
            rt = [alloc(es, f"rt{i}", [128, 512], F32) for i in range(2)]
            B_rt = [Buf(f"rt{i}") for i in range(2)]
            pst = [alloc(es, f"pst{i}", [128, 2, 1024], F32) for i in range(2)]
            B_pst = [Buf(f"pst{i}") for i in range(2)]
            for b in B_pst:
                b.dsem = T.newsem("pl_" + b.name)
                b.dsem2 = T.newsem("ps_" + b.name)
            std = {'r': 0}

            def rhs_x1(k, tg):
                return x1T[:, k, tg * 512:(tg + 1) * 512], [B_x1[k]]

            def mk_ev1(fcl):
                def ev(bset):
                    for tg in range(2):
                        s = std['r'] % 2
                        std['r'] += 1
                        ACT(rt[s][:, :], banks[bset + tg][:, :], AF.Relu, [BB[bset + tg]], [B_rt[s]])
                        TT('pool', hT[:, fcl, tg * 512:(tg + 1) * 512], rt[s][:, :], rt[s][:, :], ALU.mult,
                           [B_rt[s]], [B_h[fcl]], acc=(tg == 1))
                return ev

            def rhs_h(k, tg):
                return hT[:, k, tg * 512:(tg + 1) * 512], [B_h[k]]

            def mk_pre2(rq, db):
                def pre():
                    s = db % 2
                    if rq == 0:
                        srcv = X1s[2 * db:2 * db + 2].rearrange("c p n -> p c n")
                        rb = [B_X1s[2 * db], B_X1s[2 * db + 1]]
                    else:
                        srcv = Y2s[2 * db:2 * db + 2].rearrange("c p n -> p c n")
                        rb = [B_Y2s[db]]
                    DMA(pst[s][:, :, :], srcv, rb, [B_pst[s]], dsem=B_pst[s].dsem)
                return pre

            def mk_ev2(rq, db):
                def ev(bset):
                    s = db % 2
                    for mc in range(2):
                        for tg in range(2):
                            sl = slice(tg * 512, (tg + 1) * 512)
                            STT('dve', pst[s][:, mc, sl], pst[s][:, mc, sl], (ALPHA if rq == 0 else 1.0),
                                banks[bset + mc * 2 + tg][:, :], ALU.mult, ALU.add,
                                [B_pst[s], BB[bset + mc * 2 + tg]], [B_pst[s]])
                    DMA(Y2s[2 * db:2 * db + 2].rearrange("c p n -> p c n"), pst[s][:, :, :], [B_pst[s]], [B_Y2s[db]],
                        dsem=B_pst[s].dsem2)
                return ev
            for rq in range(NFQ):
                jobs = [dict(src=w_ff1[rq * FPR + f], KC=32, MC=1, rhs=rhs_x1, evac=mk_ev1(f)) for f in range(FPR)]
                gemm(P, jobs)
                jobs = [dict(src=w_ff2[db, rq], KC=FPR, MC=2, rhs=rhs_h, pre=mk_pre2(rq, db), evac=mk_ev2(rq, db))
                        for db in range(16)]
                gemm(P, jobs)
            T.barrier()
            T.emit()

        with ExitStack() as es:
            def out2(k, t, B_t):
                DMA(outT[k * 128:(k + 1) * 128, :], t[:, :], [B_t], [B_out], dsem=B_t.dsem, acc=True)
            ln_pass(es, Y2s, lambda k: [B_Y2s[k // 2]], 2, 3, out2)
            T.barrier()
            T.emit()
    return nc, T


def _blk(w, c0, n=128):
    t = np.zeros((4096, 128), np.float32)
    t[:, :n] = w[:, c0:c0 + n]
    return t.reshape(32, 128, 128).transpose(1, 0, 2).reshape(128, 4096)


def _prep_shared(inp):
    w_in = inp["w_in"][0]
    starts = []
    for h in range(16):
        starts.append(h * 128)
    for j in range(24):
        starts.append(2048 + j * 128)
    for j in range(16):
        starts.append(5168 + j * 128)
    for j in range(16):
        starts.append(7216 + j * 128)
    for j in range(16):
        starts.append(9264 + j * 128)
    w_in_r = np.empty((89, 128, 4096), np.float32)
    for b, c0 in enumerate(starts):
        w_in_r[b] = _blk(w_in, c0)
    w_in_r[88] = _blk(w_in, 5120, 48)
    w_out = inp["w_out"][0]
    w_out_r = np.ascontiguousarray(w_out.reshape(32, 128, 32, 128).transpose(2, 1, 0, 3)).reshape(32, 128, 4096)
    w1 = inp["w_ff1"][0]
    w_ff1_r = np.ascontiguousarray(w1.reshape(32, 128, 128, 128).transpose(2, 1, 0, 3)).reshape(128, 128, 4096)
    w2 = inp["w_ff2"][0]
    w_ff2_r = np.ascontiguousarray(
        w2.reshape(NFQ, FPR, 128, 16, 256).transpose(3, 0, 2, 1, 4)).reshape(16, NFQ, 128, FPR * 256)
    w1c = np.empty((2, 2, 128, 4096), np.float32)
    w2c = np.empty((2, 128, 256), np.float32)
    pec = np.empty((2, 128, 32), np.float32)
    for kv, nm in enumerate(["k", "v"]):
        a = inp["cmp_w1_" + nm][0].reshape(32, 128, 256).transpose(1, 0, 2)
        w1c[kv, 0] = a[:, 0:16, :].reshape(128, 4096)
        w1c[kv, 1] = a[:, 16:32, :].reshape(128, 4096)
        w2c[kv] = inp["cmp_w2_" + nm][0].reshape(2, 128, 128).transpose(1, 0, 2).reshape(128, 256)
        pec[kv] = inp["cmp_pe_" + nm][0].T
    gnsa = np.ascontiguousarray(np.broadcast_to(inp["nsa_out_g"][0].reshape(1, 2048), (128, 2048))).astype(np.float32)
    gdif = np.ascontiguousarray(np.broadcast_to(inp["diff_subln_g"][0].reshape(1, 256), (128, 256))).astype(np.float32)
    lv = np.concatenate([inp["lambda_q1"][0], inp["lambda_k1"][0], inp["lambda_q2"][0], inp["lambda_k2"][0]])
    lamv = np.ascontiguousarray(np.broadcast_to(lv.reshape(1, 512), (128, 512))).astype(np.float32)
    lnp = np.concatenate([inp[n][0].reshape(32, 128).T for n in ["ln1_g", "ln1_b", "ln2_g", "ln2_b"]], axis=1)
    j = np.arange(128)[:, None]
    i = np.arange(128)[None, :]
    u = np.arange(DW)[None, :]
    sh = dict(
        w_in=w_in_r, w_out=w_out_r, w_ff1=w_ff1_r, w_ff2=w_ff2_r, w1c=w1c, w2c=w2c, pec=pec,
        gnsa=gnsa, gdif=gdif, lamv=lamv, lnp=np.ascontiguousarray(lnp.astype(np.float32)),
        c_ident=np.eye(128, dtype=np.float32),
        c_tri=(j <= i).astype(np.float32),
        c_expand=(np.arange(2048)[None, :] // 64 == np.arange(32)[:, None]).astype(np.float32),
        c_dclamp=np.maximum(u - DOFF - j, 0).astype(np.float32),
    )
    return sh


def _prep_core(hh):
    q = np.arange(128)[:, None, None]
    it = np.arange(8)[None, :, None]
    tl = 1024 + 128 * it + q
    n = np.arange(128)[None, None, :]
    okn = (n <= 126) & (16 * n + 31 <= tl)
    if hh == 0:
        okn = okn & (n >= 64)
    cmpmask = np.where(okn, 0.0, NEGB).astype(np.float32).reshape(128, 1024)
    jb = np.arange(32)[None, None, :]
    valid = (64 * jb <= tl)
    if hh == 0:
        valid = valid & (jb >= 16)
    tb = tl // 64
    first = 0 if hh == 1 else 16
    forced = (jb == first) | (jb == tb) | (jb == tb - 1)
    A = np.where(valid & ~forced, 1.0, 0.0)
    Bm = np.where(~valid, -1.0, np.where(forced, 1e6, 0.0))
    ctxb = np.full((128, 1), 0.0 if hh == 1 else NEGB, np.float32)
    return dict(p_cmpmask=cmpmask, p_selA=A.astype(np.float32).reshape(128, 256),
                p_selB=Bm.astype(np.float32).reshape(128, 256), p_ctxb=ctxb)


_CACHE = {}


def kernel(**inputs):
    debug = bool(int(os.environ.get("MK_DEBUG", "0")))
    inp = {k: np.asarray(v) for k, v in inputs.items()}
    x = inp["x"]
    sh = _prep_shared(inp)
    in_maps = []
    for c in range(8):
        b, hh = c // 2, c % 2
        xb = x[b]
        xT = np.zeros((4096, 2048), np.float32)
        if hh == 1:
            xT[:, :] = xb.T
        else:
            xT[:, 1024:] = xb[:1024].T
        m = dict(sh)
        m.update(_prep_core(hh))
        m["xT"] = xT
        in_maps.append(m)
    if 'nc' not in _CACHE:
        _CACHE['nc'] = build(debug)[0]
    nc = _CACHE['nc']
    res = run_bass_kernel_spmd(nc, in_maps, core_ids=list(range(8)))
    out = np.empty((4, 2048, 4096), np.float32)
    for c in range(8):
        b, hh = c // 2, c % 2
        out[b, hh * 1024:(hh + 1) * 1024, :] = res.results[c]["outT"].T
    if debug:
        _CACHE['res'] = res
    return out
```

```python
import os
import numpy as np
import concourse.bass as bass
import concourse.mybir as mybir
from concourse.bass_utils import run_bass_kernel_spmd
from contextlib import ExitStack

F32 = mybir.dt.float32
BF16 = mybir.dt.bfloat16
AF = mybir.ActivationFunctionType
ALU = mybir.AluOpType
AX = mybir.AxisListType

ALPHA = 2.0 ** 0.25
QSCALE = 128.0 ** -0.5
LN_EPS = 1e-5
RMS_EPS = 1e-6
NEGB = -30000.0
NFQ = 8
FPR = 128 // NFQ
DOFF = 896
DW = 3200
ENGS = ['pe', 'act', 'dve', 'pool', 'sp']


class Buf:
    def __init__(self, name):
        self.name = name
        self.w = {}
        self.r = {}
        self.dsem = None


class DSem:
    def __init__(self, h):
        self.h = h
        self.cnt = 0


class Trk:
    def __init__(self, nc):
        self.nc = nc
        self.q = {e: [] for e in ENGS}
        self.sem = {e: nc.alloc_semaphore(name='c_' + e) for e in ENGS if e != 'sp'}
        self.cnt = {e: 0 for e in ENGS}
        self.seen = {e: {} for e in ENGS}
        self.same = True
        self.dsems = []
        self.sems = {}
        for e in self.sem:
            self.sems[id(self.sem[e])] = self.sem[e]
        self.ninst = 0

    def newsem(self, name):
        name = f"{name}_{len(self.dsems)}"
        d = DSem(self.nc.alloc_semaphore(name=name))
        self.dsems.append(d)
        self.sems[id(d.h)] = d.h
        return d

    def _wait(self, eng, deps):
        for sid, val in deps.items():
            sem = self.sems[sid]
            own = (eng != 'sp' and sem is self.sem[eng])
            if own:
                if eng == 'pe' or not self.same or val > self.cnt[eng]:
                    continue
            if self.seen[eng].get(sid, 0) >= val:
                continue
            self.seen[eng][sid] = val
            self.ninst += 1
            self.q[eng].append(lambda h, sem=sem, val=val: h.wait_ge(sem, val))

    def _deps(self, reads, writes, acc):
        deps = {}

        def add(d):
            for s, v in d.items():
                if deps.get(s, 0) < v:
                    deps[s] = v
        for b in reads:
            add(b.w)
        for b in writes:
            add(b.r)
            if not acc:
                add(b.w)
        return deps

    def _record(self, tok, reads, writes, acc):
        s, v = tok
        for b in reads:
            b.r[s] = max(b.r.get(s, 0), v)
        for b in writes:
            if acc:
                b.w[s] = max(b.w.get(s, 0), v)
            else:
                b.w = {s: v}
                b.r = {}

    def op(self, eng, fn, reads=(), writes=(), inc=True, acc=False):
        self._wait(eng, self._deps(reads, writes, acc))
        sem = self.sem[eng]
        self.ninst += 1
        if inc:
            self.cnt[eng] += 1
            tok = (id(sem), self.cnt[eng])
            self.q[eng].append(lambda h, fn=fn, sem=sem: fn(h).then_inc(sem, 1))
        else:
            tok = (id(sem), self.cnt[eng] + 1)
            self.q[eng].append(lambda h, fn=fn: fn(h))
        self._record(tok, reads, writes, acc)

    def dma(self, pairs, reads=(), writes=(), dsem=None, acc=False, qeng='sp'):
        if dsem is None:
            b = writes[0]
            if b.dsem is None:
                b.dsem = self.newsem('d_' + b.name)
            dsem = b.dsem
        self._wait(qeng, self._deps(reads, writes, acc))
        for (o, i) in pairs:
            dsem.cnt += 16
            self.ninst += 1
            self.q[qeng].append(lambda h, o=o, i=i, s=dsem.h: h.dma_start(out=o, in_=i).then_inc(s, 16))
        self._record((id(dsem.h), dsem.cnt), reads, writes, acc)

    def barrier(self):
        deps = {}
        for e in self.sem:
            if self.cnt[e] > 0:
                deps[id(self.sem[e])] = self.cnt[e]
        for d in self.dsems:
            if d.cnt > 0:
                deps[id(d.h)] = d.cnt
        for e in ENGS:
            self._wait(e, deps)

    def emit(self):
        q = self.q
        with self.nc.Block() as block:
            @block.tensor
            def _(h):
                for f in q['pe']:
                    f(h)

            @block.scalar
            def _(h):
                for f in q['act']:
                    f(h)

            @block.vector
            def _(h):
                for f in q['dve']:
                    f(h)

            @block.gpsimd
            def _(h):
                for f in q['pool']:
                    f(h)

            @block.sync
            def _(h):
                for f in q['sp']:
                    f(h)
        self.q = {e: [] for e in ENGS}


def nsa_slope(h):
    return float(2.0 ** (-8.0 * (h + 1) / 16))


def diff_slope(h):
    return float(2.0 ** (-8.0 * (h + 1) / 8))


def build(debug=False):
    nc = bass.Bass("TRN2", target_bir_lowering=False)

    def din(name, shape, dt=F32):
        return nc.dram_tensor(name, shape, dt, kind="ExternalInput").ap()

    def dscr(name, shape, dt):
        return nc.dram_tensor(name, shape, dt, kind=("ExternalOutput" if debug else "Internal")).ap()

    xT = din("xT", [4096, 2048])
    w_in = din("w_in", [89, 128, 4096])
    w_out = din("w_out", [32, 128, 4096])
    w_ff1 = din("w_ff1", [128, 128, 4096])
    w_ff2 = din("w_ff2", [16, NFQ, 128, 4096])
    w1c = din("w1c", [2, 2, 128, 4096])
    w2c = din("w2c", [2, 128, 256])
    pec = din("pec", [2, 128, 32])
    gnsa = din("gnsa", [128, 2048])
    gdif = din("gdif", [128, 256])
    lamv = din("lamv", [128, 512])
    lnp = din("lnp", [128, 128])
    c_ident = din("c_ident", [128, 128])
    c_tri = din("c_tri", [128, 128])
    c_expand = din("c_expand", [32, 2048])
    c_dclamp = din("c_dclamp", [128, DW])
    p_ctxb = din("p_ctxb", [128, 1])
    p_cmpmask = din("p_cmpmask", [128, 1024])
    p_selA = din("p_selA", [128, 256])
    p_selB = din("p_selB", [128, 256])
    outT = nc.dram_tensor("outT", [4096, 1024], F32, kind="ExternalOutput").ap()
    S_in = dscr("S_in", [88, 128, 2048], BF16)
    Ys = dscr("Ys", [32, 128, 1024], F32)
    X1s = dscr("X1s", [32, 128, 1024], F32)
    Y2s = dscr("Y2s", [32, 128, 1024], F32)
    dbg_cat = dscr("dbg_cat", [128, 32 * 1024], BF16) if debug else None

    T = Trk(nc)
    B_out = Buf("outT")
    B_Sin = [Buf(f"Sin{i}") for i in range(88)]
    B_Ys = [Buf(f"Ys{i}") for i in range(32)]
    B_X1s = [Buf(f"X1s{i}") for i in range(32)]
    B_Y2s = [Buf(f"Y2s{i}") for i in range(16)]

    def TT(eng, out, in0, in1, op, reads, writes, **kw):
        T.op(eng, lambda h: h.tensor_tensor(out=out, in0=in0, in1=in1, op=op), reads, writes, **kw)

    def STT(eng, out, in0, scalar, in1, op0, op1, reads, writes, **kw):
        T.op(eng, lambda h: h.scalar_tensor_tensor(out=out, in0=in0, scalar=scalar, in1=in1, op0=op0, op1=op1),
             reads, writes, **kw)

    def TS(eng, out, in0, s1, s2, op0, op1, reads, writes, **kw):
        if op1 is None:
            T.op(eng, lambda h: h.tensor_scalar(out=out, in0=in0, scalar1=s1, scalar2=None, op0=op0), reads, writes, **kw)
        else:
            T.op(eng, lambda h: h.tensor_scalar(out=out, in0=in0, scalar1=s1, scalar2=s2, op0=op0, op1=op1),
                 reads, writes, **kw)

    def ACT(out, in_, func, reads, writes, bias=None, scale=None, accum_out=None, **kw):
        kws = {}
        if bias is not None:
            kws['bias'] = bias
        if scale is not None:
            kws['scale'] = scale
        if accum_out is not None:
            kws['accum_out'] = accum_out
        T.op('act', lambda h: h.activation(out=out, in_=in_, func=func, **kws), reads, writes, **kw)

    def CP(eng, out, in_, reads, writes, **kw):
        if eng == 'act':
            ACT(out, in_, AF.Copy, reads, writes, **kw)
        else:
            T.op(eng, lambda h: h.tensor_copy(out=out, in_=in_), reads, writes, **kw)

    def MS(eng, ap, val, writes):
        T.op(eng, lambda h: h.memset(ap, val), (), writes)

    def MM(out, lhsT, rhs, start, stop, reads, writes, inc=True, acc=False):
        T.op('pe', lambda h: h.matmul(out, lhsT, rhs, start=start, stop=stop), reads, writes, inc=inc, acc=acc)

    def DMA(out, in_, reads, writes, dsem=None, acc=False):
        T.dma([(out, in_)], reads, writes, dsem=dsem, acc=acc)

    def REDUCE(eng, out, in_, op, reads, writes):
        T.op(eng, lambda h: h.tensor_reduce(out=out, in_=in_, axis=AX.X, op=op), reads, writes)

    def RECIP(eng, out, in_, reads, writes):
        T.op(eng, lambda h: h.reciprocal(out=out, in_=in_), reads, writes)

    with ExitStack() as top:
        acnt = {'n': 0}

        def alloc(es, name, shape, dt):
            acnt['n'] += 1
            return es.enter_context(nc.sbuf_tensor(f"{name}_{acnt['n']}", shape, dt))

        banks = [top.enter_context(nc.psum_tensor(f"bank{i}", [128, 512], F32)) for i in range(8)]
        BB = [Buf(f"bank{i}") for i in range(8)]

        gates = alloc(top, "gates", [128, 8, 48], F32)
        B_gates = Buf("gates")
        lnp_t = alloc(top, "lnp_t", [128, 128], F32)
        B_lnp = Buf("lnp")
        identf = alloc(top, "identf", [128, 128], F32)
        identb = alloc(top, "identb", [128, 128], BF16)
        B_ident = Buf("ident")
        zcol = alloc(top, "zcol", [128, 1], F32)
        B_zcol = Buf("zcol")
        DMA(lnp_t[:, :], lnp[:, :], [], [B_lnp])
        DMA(identf[:, :], c_ident[:, :], [], [B_ident])
        CP('dve', identb[:, :], identf[:, :], [B_ident], [B_ident])
        MS('dve', zcol[:, :], 0.0, [B_zcol])

        class Pipe:
            pass

        def make_pipe(es, nwb=3):
            P = Pipe()
            P.stg = [alloc(es, f"stg{i}", [128, 4096], F32) for i in range(2)]
            P.B_stg = [Buf(f"stg{i}") for i in range(2)]
            P.wb = [alloc(es, f"wb{i}", [128, 4096], BF16) for i in range(nwb)]
            P.B_wb = [Buf(f"wb{i}") for i in range(nwb)]
            P.nwb = nwb
            P.jc = 0
            return P

        def gemm(P, jobs):
            n = len(jobs)
            base = P.jc

            def issue_dma(j):
                s = (base + j) % 2
                DMA(P.stg[s][:, :], jobs[j]['src'], [], [P.B_stg[s]])

            def issue_cast(j):
                s = (base + j) % 2
                w = (base + j) % P.nwb
                eng = 'dve' if (base + j) % 2 == 0 else 'pool'
                CP(eng, P.wb[w][:, :], P.stg[s][:, :], [P.B_stg[s]], [P.B_wb[w]])
            issue_dma(0)
            if n > 1:
                issue_dma(1)
            issue_cast(0)
            for j in range(n):
                job = jobs[j]
                if j + 1 < n:
                    issue_cast(j + 1)
                if j + 2 < n:
                    issue_dma(j + 2)
                w = (base + j) % P.nwb
                bset = 4 * ((base + j) % 2)
                if job.get('pre') is not None:
                    job['pre']()
                if job.get('custom') is not None:
                    job['custom'](P.wb[w], P.B_wb[w], bset)
                    continue
                KC, MC = job['KC'], job['MC']
                wv = P.wb[w][:, :].rearrange("p (k c) -> p k c", c=MC * 128)
                for k in range(KC):
                    for mc in range(MC):
                        for tg in range(2):
                            bi = bset + mc * 2 + tg
                            rap, rb = job['rhs'](k, tg)
                            last = (k == KC - 1 and mc == MC - 1 and tg == 1)
                            MM(banks[bi][:, :], wv[:, k, mc * 128:(mc + 1) * 128], rap,
                               start=(k == 0), stop=(k == KC - 1), reads=[P.B_wb[w]] + rb, writes=[BB[bi]],
                               inc=last, acc=(k > 0))
                job['evac'](bset)
            P.jc += n

        with ExitStack() as es:
            P = make_pipe(es)
            actT = alloc(es, "actT", [128, 32, 1024], BF16)
            B_act = [Buf(f"act{i}") for i in range(16)]
            xst = [alloc(es, f"xst{i}", [128, 2, 1024], F32) for i in range(2)]
            B_xst = [Buf(f"xst{i}") for i in range(2)]
            ost = [alloc(es, f"ost{i}", [128, 1024], BF16) for i in range(2)]
            B_ost = [Buf(f"ost{i}") for i in range(2)]
            for b in B_ost:
                b.dsem = T.newsem("st_" + b.name)
            xTv = xT.rearrange("(k p) n -> p k n", p=128)
            st = {'oc': 0}

            def load_x(tok0):
                for j in range(16):
                    s = j % 2
                    DMA(xst[s][:, :, :], xTv[:, 2 * j:2 * j + 2, tok0:tok0 + 1024], [], [B_xst[s]])
                    CP('dve' if j % 2 == 0 else 'pool', actT[:, 2 * j:2 * j + 2, :], xst[s][:, :, :], [B_xst[s]], [B_act[j]])

            def rhs_act(k, tg):
                return actT[:, k, tg * 512:(tg + 1) * 512], [B_act[k // 2]]

            def mk_evac_in(blk, tok0, scale):
                def ev(bset):
                    s = st['oc'] % 2
                    st['oc'] += 1
                    for tg in range(2):
                        ACT(ost[s][:, tg * 512:(tg + 1) * 512], banks[bset + tg][:, :], AF.Copy,
                            [BB[bset + tg]], [B_ost[s]], scale=scale, acc=(tg == 1))
                    DMA(S_in[blk][:, tok0:tok0 + 1024], ost[s][:, :], [B_ost[s]], [B_Sin[blk]], dsem=B_ost[s].dsem, acc=True)
                return ev

            def gate_custom(wbt, B_wbt, bset):
                wv = wbt[:, :].rearrange("p (k c) -> p k c", c=128)
                for i in range(8):
                    bi = bset + (i % 4)
                    for k in range(32):
                        MM(banks[bi][:, 0:128], actT[:, k, i * 128:(i + 1) * 128], wv[:, k, :],
                           start=(k == 0), stop=(k == 31), reads=[B_wbt, B_act[k // 2]], writes=[BB[bi]],
                           inc=(k == 31), acc=(k > 0))
                    ACT(gates[:, i, :], banks[bi][:, 0:48], AF.Sigmoid, [BB[bi]], [B_gates], acc=True)

            kv_blocks = list(range(16, 40)) + list(range(56, 88))
            q_blocks = list(range(0, 16)) + list(range(40, 56))
            load_x(0)
            jobs = [dict(src=w_in[b], KC=32, MC=1, rhs=rhs_act, evac=mk_evac_in(b, 0, 1.0)) for b in kv_blocks]
            gemm(P, jobs)
            load_x(1024)
            jobs = [dict(src=w_in[88], custom=gate_custom)]
            for b in range(88):
                jobs.append(dict(src=w_in[b], KC=32, MC=1, rhs=rhs_act,
                                 evac=mk_evac_in(b, 1024, QSCALE if b in q_blocks else 1.0)))
            gemm(P, jobs)
            T.barrier()
            T.emit()

        esBC = ExitStack()
        esBC.__enter__()
        catT = alloc(esBC, "catT", [128, 32, 1024], BF16)
        B_cat = [Buf(f"cat{i}") for i in range(32)]
        kcT_all = alloc(esBC, "kcT_all", [128, 4, 128], BF16)
        vc_all = alloc(esBC, "vc_all", [128, 4, 128], BF16)
        B_kc = Buf("kc")
        B_vc = Buf("vc")

        with ExitStack() as es:
            stg = alloc(es, "cstg", [128, 4096], F32)
            B_stg = Buf("cstg")
            w1b = [alloc(es, f"w1b{kv}", [128, 32, 256], BF16) for kv in range(2)]
            B_w1b = [Buf(f"w1b{kv}") for kv in range(2)]
            w2f = alloc(es, "w2f", [128, 256], F32)
            w2b = [alloc(es, f"w2b{kv}", [128, 2, 128], BF16) for kv in range(2)]
            B_w2 = [Buf(f"w2_{kv}") for kv in range(2)]
            B_w2f = Buf("w2f")
            pef = alloc(es, "pef", [128, 32], F32)
            peb = [alloc(es, f"peb{kv}", [128, 32], BF16) for kv in range(2)]
            B_pe = [Buf(f"pe{kv}") for kv in range(2)]
            B_pef = Buf("pef")
            cT = [alloc(es, f"cT{i}", [128, 2048], BF16) for i in range(2)]
            B_cT = [Buf(f"cT{i}") for i in range(2)]
            b1 = alloc(es, "b1", [128, 1], F32)
            B_b1 = Buf("b1")
            gx = alloc(es, "gx", [128, 128], F32)
            gw = alloc(es, "gw", [128, 128], F32)
            B_gx = Buf("gx")
            B_gw = Buf("gw")
            hid = [alloc(es, f"hid{hc}", [128, 128], BF16) for hc in range(2)]
            B_hid = [Buf(f"hid{hc}") for hc in range(2)]
            for kv in range(2):
                for half in range(2):
                    DMA(stg[:, :], w1c[kv, half], [], [B_stg])
                    CP('dve', w1b[kv][:, half * 16:(half + 1) * 16, :],
                       stg[:, :].rearrange("p (l h) -> p l h", h=256), [B_stg], [B_w1b[kv]], acc=(half == 1))
                DMA(w2f[:, :], w2c[kv], [], [B_w2f])
                CP('dve', w2b[kv][:, :, :], w2f[:, :].rearrange("p (c d) -> p c d", d=128), [B_w2f], [B_w2[kv]])
                DMA(pef[:, :], pec[kv], [], [B_pef])
                CP('dve', peb[kv][:, :], pef[:, :], [B_pef], [B_pe[kv]])
            MS('dve', kcT_all[:, :, :], 0.0, [B_kc])
            MS('dve', vc_all[:, :, :], 0.0, [B_vc])
            for hc in range(2):
                MS('pool', hid[hc][:, :], 0.0, [B_hid[hc]])
            cnt = 0
            for g in range(4):
                for kv in range(2):
                    blk = 16 + kv * 4 + g
                    s = cnt % 2
                    cnt += 1
                    DMA(cT[s][:, :], S_in[blk], [B_Sin[blk]], [B_cT[s]])
                    for hc in range(2):
                        bi = 6 + (hc % 2)
                        for l in range(32):
                            MM(banks[bi][:, 0:127], w1b[kv][:, l, hc * 128:(hc + 1) * 128],
                               cT[s][:, l:l + 16 * 126 + 1:16], start=(l == 0), stop=(l == 31),
                               reads=[B_w1b[kv], B_cT[s]], writes=[BB[bi]], inc=False, acc=(l > 0))
                        for l in range(32):
                            MM(banks[bi][:, 128:129], w1b[kv][:, l, hc * 128:(hc + 1) * 128],
                               peb[kv][:, l:l + 1], start=(l == 0), stop=(l == 31),
                               reads=[B_w1b[kv], B_pe[kv]], writes=[BB[bi]], inc=(l == 31), acc=True)
                        CP('dve', b1[:, :], banks[bi][:, 128:129], [BB[bi]], [B_b1])
                        ACT(gx[:, 0:127], banks[bi][:, 0:127], AF.Identity, [BB[bi], B_b1], [B_gx], bias=b1[:, 0:1])
                        TT('dve', gw[:, 0:127], gx[:, 0:127], gx[:, 0:127], ALU.mult, [B_gx], [B_gw])
                        TS('dve', gw[:, 0:127], gw[:, 0:127], 0.044715, 1.0, ALU.mult, ALU.add, [B_gw], [B_gw])
                        TT('dve', gw[:, 0:127], gw[:, 0:127], gx[:, 0:127], ALU.mult, [B_gw, B_gx], [B_gw])
                        ACT(gw[:, 0:127], gw[:, 0:127], AF.Tanh, [B_gw], [B_gw], scale=0.7978845608028654)
                        STT('dve', gw[:, 0:127], gw[:, 0:127], 1.0, gx[:, 0:127], ALU.add, ALU.mult, [B_gw, B_gx], [B_gw])
                        TS('dve', hid[hc][:, 0:127], gw[:, 0:127], 0.5, None, ALU.mult, None, [B_gw], [B_hid[hc]])
                    if kv == 0:
                        for hc in range(2):
                            MM(banks[6][:, 0:127], w2b[0][:, hc, :], hid[hc][:, 0:127], start=(hc == 0), stop=(hc == 1),
                               reads=[B_w2[0], B_hid[hc]], writes=[BB[6]], inc=(hc == 1), acc=(hc == 1))
                        CP('act', kcT_all[:, g, 0:127], banks[6][:, 0:127], [BB[6]], [B_kc], acc=True)
                    else:
                        for hc in range(2):
                            MM(banks[6][0:127, 0:128], hid[hc][:, 0:127], w2b[1][:, hc, :], start=(hc == 0), stop=(hc == 1),
                               reads=[B_w2[1], B_hid[hc]], writes=[BB[6]], inc=(hc == 1), acc=(hc == 1))
                        CP('act', vc_all[0:127, g, :], banks[6][0:127, 0:128], [BB[6]], [B_vc], acc=True)
            T.barrier()
            T.emit()

        with ExitStack() as es:
            dcl = alloc(es, "dcl", [128, DW], F32)
            B_dcl = Buf("dcl")
            DMA(dcl[:, :], c_dclamp[:, :], [], [B_dcl])
            tmpf = [alloc(es, f"tmpf{i}", [128, 512], F32) for i in range(3)]
            B_tmpf = [Buf(f"tmpf{i}") for i in range(3)]
            tri = alloc(es, "tri", [128, 128], BF16)
            tri2 = alloc(es, "tri2", [128, 128], BF16)
            B_tri = Buf("tri")
            DMA(tmpf[1][:, 0:128], c_tri[:, :], [], [B_tmpf[1]])
            CP('dve', tri[:, :], tmpf[1][:, 0:128], [B_tmpf[1]], [B_tri])
            TS('dve', tri2[:, :], tmpf[1][:, 0:128], -1.0, 1.0, ALU.mult, ALU.add, [B_tmpf[1]], [B_tri], acc=True)
            expb = alloc(es, "expb", [32, 2048], BF16)
            B_exp = Buf("exp")
            for q4 in range(4):
                DMA(tmpf[0][0:32, :], c_expand[:, q4 * 512:(q4 + 1) * 512], [], [B_tmpf[0]])
                CP('dve', expb[:, q4 * 512:(q4 + 1) * 512], tmpf[0][0:32, :], [B_tmpf[0]], [B_exp], acc=True)
            ctxb = alloc(es, "ctxb", [128, 1], F32)
            B_ctxb = Buf("ctxb")
            DMA(ctxb[:, :], p_ctxb[:, :], [], [B_ctxb])
            cmpmask = alloc(es, "cmpmask", [128, 8, 128], F32)
            B_cm = Buf("cmpmask")
            DMA(cmpmask[:, :, :], p_cmpmask.rearrange("p (i n) -> p i n", n=128), [], [B_cm])
            selA = alloc(es, "selA", [128, 8, 32], F32)
            selB = alloc(es, "selB", [128, 8, 32], F32)
            B_sAB = Buf("selAB")
            DMA(selA[:, :, :], p_selA.rearrange("p (i n) -> p i n", n=32), [], [B_sAB])
            DMA(selB[:, :, :], p_selB.rearrange("p (i n) -> p i n", n=32), [], [B_sAB], acc=True,
                dsem=T.newsem("selB"))
            gn = alloc(es, "gn", [128, 512], F32)
            gd = alloc(es, "gd", [128, 256], F32)
            B_g = Buf("gn")
            B_gd = Buf("gd")
            DMA(gd[:, :], gdif[:, :], [], [B_gd])
            lam_t = alloc(es, "lam_t", [128, 512], F32)
            B_lam = Buf("lam")
            lsc = alloc(es, "lsc", [128, 8], F32)
            DMA(lam_t[:, :], lamv[:, :], [], [B_lam])
            TT('dve', lam_t[:, 0:128], lam_t[:, 0:128], lam_t[:, 128:256], ALU.mult, [B_lam], [B_lam])
            TT('dve', lam_t[:, 256:384], lam_t[:, 256:384], lam_t[:, 384:512], ALU.mult, [B_lam], [B_lam])
            REDUCE('dve', lsc[:, 0:1], lam_t[:, 0:128], ALU.add, [B_lam], [B_lam])
            REDUCE('dve', lsc[:, 1:2], lam_t[:, 256:384], ALU.add, [B_lam], [B_lam])
            ACT(lsc[:, 2:4], lsc[:, 0:2], AF.Exp, [B_lam], [B_lam])
            TT('dve', lsc[:, 4:5], lsc[:, 3:4], lsc[:, 2:3], ALU.subtract, [B_lam], [B_lam])
            TS('dve', lsc[:, 5:6], lsc[:, 4:5], -0.2, None, ALU.add, None, [B_lam], [B_lam])
            neglam = lsc[:, 5:6]

            PT = [alloc(es, f"PT{i}", [128, 512], BF16) for i in range(4)]
            B_PT = [Buf(f"PT{i}") for i in range(4)]
            rg = {'t': 0, 'p': 0, 's': 0, 'm': 0}
            sm = alloc(es, "sm", [128, 64], F32)
            B_sm = Buf("sm")
            sqs = alloc(es, "sqs", [128, 256], F32)
            B_sqs = Buf("sqs")
            ob = alloc(es, "ob", [128, 256], BF16)
            B_ob = Buf("ob")
            of = alloc(es, "of", [128, 256], F32)
            B_of = Buf("of")

            def rms_and_store(src_ap, src_bufs, width, gain_ap, gain_bufs, extra, chunk0, i):
                TT('pool', sqs[:, 0:width], src_ap, src_ap, ALU.mult, src_bufs, [B_sqs])
                REDUCE('dve', sm[:, 0:1], sqs[:, 0:width], ALU.add, [B_sqs], [B_sm])
                TS('dve', sm[:, 0:1], sm[:, 0:1], 1.0 / width, RMS_EPS, ALU.mult, ALU.add, [B_sm], [B_sm])
                ACT(sm[:, 1:2], sm[:, 0:1], AF.Ln, [B_sm], [B_sm])
                ACT(sm[:, 2:3], sm[:, 1:2], AF.Exp, [B_sm], [B_sm], scale=-0.5)
                if extra != 1.0:
                    TS('dve', sm[:, 2:3], sm[:, 2:3], extra, None, ALU.mult, None, [B_sm], [B_sm])
                STT('dve', ob[:, 0:width], src_ap, sm[:, 2:3], gain_ap, ALU.mult, ALU.mult,
                    src_bufs + [B_sm] + gain_bufs, [B_ob])
                for hf in range(width // 128):
                    bi = 6 + (rg['m'] % 2)
                    rg['m'] += 1
                    MM(banks[bi][:, 0:128], ob[:, hf * 128:(hf + 1) * 128], identb[:, :], True, True,
                       [B_ob, B_ident], [BB[bi]])
                    CP('act', catT[:, chunk0 + hf, i * 128:(i + 1) * 128], banks[bi][:, 0:128], [BB[bi]],
                       [B_cat[chunk0 + hf]], acc=True)

            def load_V(VT, B_VT, Vtok, B_Vtok, ncol):
                for kt in range(16):
                    bi = 6 + (rg['m'] % 2)
                    rg['m'] += 1
                    for c in range(ncol):
                        MM(banks[bi][:, c * 128:(c + 1) * 128], VT[c][:, kt * 128:(kt + 1) * 128], identb[:, :], True, True,
                           [B_VT[c], B_ident], [BB[bi]], inc=(c == ncol - 1), acc=(c > 0))
                    CP('act' if kt % 2 == 0 else 'dve', Vtok[:, kt, 0:ncol * 128], banks[bi][:, 0:ncol * 128],
                       [BB[bi]], [B_Vtok], acc=True)

            def dense_attn(nm, QT, B_QT, KT, B_KT, Vtok, B_Vtok, dv, slope, CN, chunks, kt_lo_fn, pair_fn, MT, B_MT,
                           finalize):
                ntile = CN // 128
                for c in chunks:
                    qt0 = 8 + c * ntile
                    kts = list(range(kt_lo_fn(qt0), qt0 + ntile))
                    obank = {}
                    for m in range(nm):
                        for ip in range(ntile):
                            obank[(m, ip)] = 2 + m * ntile + ip
                    first = {}
                    sbank = {}

                    def emit_S(kt_):
                        sb_ = (0, 1, 6)[rg['s'] % 3]
                        rg['s'] += 1
                        sbank[kt_] = sb_
                        for m in range(nm):
                            MM(banks[sb_][:, m * CN:(m + 1) * CN], KT[m][:, kt_ * 128:(kt_ + 1) * 128],
                               QT[m][:, c * CN:(c + 1) * CN], True, True, [B_KT[m], B_QT[m]], [BB[sb_]],
                               inc=(m == nm - 1), acc=(m > 0))
                    emit_S(kts[0])
                    if len(kts) > 1:
                        emit_S(kts[1])
                    for kidx, kt in enumerate(kts):
                        if kidx + 2 < len(kts):
                            emit_S(kts[kidx + 2])
                        sbk = sbank[kt]
                        u0 = 1024 + c * CN - kt * 128 + DOFF
                        tf = rg['t'] % 3
                        rg['t'] += 1
                        if nm == 1:
                            STT('dve', tmpf[tf][:, 0:CN], dcl[:, u0:u0 + CN], -slope, banks[sbk][:, 0:CN],
                                ALU.mult, ALU.add, [B_dcl, BB[sbk]], [B_tmpf[tf]])
                        else:
                            STT('dve', tmpf[tf][:, 0:nm * CN].rearrange("p (m n) -> p m n", m=nm),
                                dcl[:, u0:u0 + CN].unsqueeze(1).broadcast_to([128, nm, CN]), -slope,
                                banks[sbk][:, 0:nm * CN].rearrange("p (m n) -> p m n", m=nm),
                                ALU.mult, ALU.add, [B_dcl, BB[sbk]], [B_tmpf[tf]])
                        pi = rg['p'] % 4
                        rg['p'] += 1
                        ACT(PT[pi][:, 0:nm * CN], tmpf[tf][:, 0:nm * CN], AF.Exp, [B_tmpf[tf], B_ctxb, B_zcol],
                            [B_PT[pi]], bias=(ctxb[:, 0:1] if kt < 8 else zcol[:, 0:1]))
                        if MT is not None:
                            TT('pool', PT[pi][:, 0:CN], PT[pi][:, 0:CN], MT[:, kt, :], ALU.mult,
                               [B_PT[pi], B_MT], [B_PT[pi]])
                        for ip in range(ntile):
                            qt = qt0 + ip
                            pm = pair_fn(kt, qt)
                            if pm is None or pm == 'full':
                                continue
                            msk = tri if pm == 'tri' else tri2
                            if nm == 1:
                                TT('pool', PT[pi][:, ip * 128:(ip + 1) * 128], PT[pi][:, ip * 128:(ip + 1) * 128],
                                   msk[:, :], ALU.mult, [B_PT[pi], B_tri], [B_PT[pi]])
                            else:
                                v = PT[pi][:, 0:nm * CN].rearrange("p (m n) -> p m n", m=nm)[:, :, ip * 128:(ip + 1) * 128]
                                TT('pool', v, v, msk[:, :].unsqueeze(1).broadcast_to([128, nm, 128]), ALU.mult,
                                   [B_PT[pi], B_tri], [B_PT[pi]])
                        avs = []
                        for m in range(nm):
                            for ip in range(ntile):
                                qt = qt0 + ip
                                if pair_fn(kt, qt) is None:
                                    continue
                                avs.append((m, ip))
                        for idx, (m, ip) in enumerate(avs):
                            qt = qt0 + ip
                            ob_ = obank[(m, ip)]
                            st_ = (m, ip) not in first
                            first[(m, ip)] = True
                            MM(banks[ob_][:, 0:dv + 1], PT[pi][:, m * CN + ip * 128:m * CN + (ip + 1) * 128],
                               Vtok[:, kt, 0:dv + 1], st_, (kt == qt), [B_PT[pi], B_Vtok], [BB[ob_]],
                               inc=(idx == len(avs) - 1), acc=(not st_))
                    finalize([(c * ntile + ip, [obank[(m, ip)] for m in range(nm)]) for ip in range(ntile)])

            with ExitStack() as es2:
                QT = [alloc(es2, f"QT{r}", [128, 1024], BF16) for r in range(4)]
                B_QT = [Buf(f"QT{r}") for r in range(4)]
                sK = alloc(es2, "sK", [128, 2048], BF16)
                wK = alloc(es2, "wK", [128, 2048], BF16)
                B_sK = Buf("sK")
                B_wK = Buf("wK")
                VTs = [alloc(es2, "VTs0", [128, 2048], BF16)] * 2
                B_VTs = [Buf("VTs0")] * 2
                sV = alloc(es2, "sV", [128, 16, 132], BF16)
                wV = alloc(es2, "wV", [128, 16, 132], BF16)
                B_sV = Buf("sV")
                B_wV = Buf("wV")
                MS('pool', sV[:, :, 128:129], 1.0, [B_sV])
                MS('pool', wV[:, :, 128:129], 1.0, [B_wV])
                acc = alloc(es2, "acc", [128, 8, 4, 128], F32)
                B_acc = [Buf(f"acc{i}") for i in range(8)]
                MT = alloc(es2, "MT", [128, 16, 512], BF16)
                B_MT = Buf("MT")
                NL = 2
                E4 = [alloc(es2, f"E4{j}", [128, 4, 128], F32) for j in range(NL)]
                B_E4 = [Buf(f"E4{j}") for j in range(NL)]
                pb4 = [alloc(es2, f"pb4{j}", [128, 4, 128], BF16) for j in range(NL)]
                B_pb4 = [Buf(f"pb4{j}") for j in range(NL)]
                pT4 = [alloc(es2, f"pT4{j}", [128, 4, 128], BF16) for j in range(NL)]
                B_pT4 = [Buf(f"pT4{j}") for j in range(NL)]
                rs4 = [alloc(es2, f"rs4{j}", [128, 8], F32) for j in range(NL)]
                B_rs4 = [Buf(f"rs4{j}") for j in range(NL)]
                Ppad = [alloc(es2, f"Ppad{j}", [128, 132], F32) for j in range(NL)]
                B_Pp = [Buf(f"Ppad{j}") for j in range(NL)]
                for j in range(NL):
                    MS('dve', Ppad[j][:, :], 0.0, [B_Pp[j]])
                impt = [alloc(es2, f"impt{j}", [128, 3, 32], F32) for j in range(NL)]
                B_imp = [Buf(f"imp{j}") for j in range(NL)]
                c3 = [alloc(es2, f"c3{j}", [128, 32, 32], F32) for j in range(NL)]
                B_c3 = [Buf(f"c3{j}") for j in range(NL)]
                selb = [alloc(es2, f"selb{j}", [128, 32], BF16) for j in range(NL)]
                B_selb = [Buf(f"selb{j}") for j in range(NL)]
                selT = alloc(es2, "selT", [32, 1024], BF16)
                B_selT = Buf("selT")
                rq4 = [alloc(es2, f"rq4{j}", [128, 12], F32) for j in range(4)]
                B_rq4 = [Buf(f"rq4{j}") for j in range(4)]
                fsm = [alloc(es2, f"fsm{j}", [128, 4], F32) for j in range(4)]
                B_fsm = [Buf(f"fsm{j}") for j in range(4)]

                def v4(ap):
                    return ap.rearrange("p (r n) -> p r n", r=4)

                def fin_add(items, r, h, br):
                    for ip, (i, obs) in enumerate(items):
                        TS('dve', fsm[ip][:, 0:1], banks[obs[0]][:, 128:129], 1e-30, None, ALU.add, None,
                           [BB[obs[0]]], [B_fsm[ip]])
                    for ip, (i, obs) in enumerate(items):
                        RECIP('dve', fsm[ip][:, 1:2], fsm[ip][:, 0:1], [B_fsm[ip]], [B_fsm[ip]])
                    for ip, (i, obs) in enumerate(items):
                        TT('dve', fsm[ip][:, 2:3], fsm[ip][:, 1:2], gates[:, i, h * 3 + br:h * 3 + br + 1], ALU.mult,
                           [B_fsm[ip], B_gates], [B_fsm[ip]])
                    for ip, (i, obs) in enumerate(items):
                        STT('dve', acc[:, i, r, :], banks[obs[0]][:, 0:128], fsm[ip][:, 2:3], acc[:, i, r, :],
                            ALU.mult, ALU.add, [BB[obs[0]], B_fsm[ip], B_acc[i]], [B_acc[i]])

                def rms_batch(g, c):
                    sq = [E4[0][:, :, :], E4[1][:, :, :], v4(tmpf[0][:, :]), v4(tmpf[1][:, :])]
                    B_sq = [B_E4[0], B_E4[1], B_tmpf[0], B_tmpf[1]]
                    obt = [pb4[0], pb4[1], pT4[0], pT4[1]]
                    B_obt = [B_pb4[0], B_pb4[1], B_pT4[0], B_pT4[1]]
                    bk = [6, 7, 0, 1]
                    its = [(ip, 4 * c + ip) for ip in range(4)]
                    for ip, i in its:
                        TT('pool', sq[ip], acc[:, i, :, :], acc[:, i, :, :], ALU.mult, [B_acc[i]], [B_sq[ip]])
                    for ip, i in its:
                        REDUCE('dve', rq4[ip][:, 0:4], sq[ip], ALU.add, [B_sq[ip]], [B_rq4[ip]])
                    for ip, i in its:
                        TS('dve', rq4[ip][:, 0:4], rq4[ip][:, 0:4], 1.0 / 128, RMS_EPS, ALU.mult, ALU.add,
                           [B_rq4[ip]], [B_rq4[ip]])
                    for ip, i in its:
                        ACT(rq4[ip][:, 4:8], rq4[ip][:, 0:4], AF.Ln, [B_rq4[ip]], [B_rq4[ip]])
                    for ip, i in its:
                        ACT(rq4[ip][:, 8:12], rq4[ip][:, 4:8], AF.Exp, [B_rq4[ip]], [B_rq4[ip]], scale=-0.5)
                    for ip, i in its:
                        TT('dve', sq[ip], acc[:, i, :, :], rq4[ip][:, 8:12].unsqueeze(2).broadcast_to([128, 4, 128]),
                           ALU.mult, [B_acc[i], B_rq4[ip]], [B_sq[ip]])
                    for ip, i in its:
                        TT('pool', obt[ip][:, :, :], sq[ip], v4(gn[:, :]), ALU.mult, [B_sq[ip], B_g], [B_obt[ip]])
                    for ip, i in its:
                        for r in range(4):
                            MM(banks[bk[ip]][:, r * 128:(r + 1) * 128], obt[ip][:, r, :], identb[:, :], True, True,
                               [B_obt[ip], B_ident], [BB[bk[ip]]], inc=(r == 3), acc=(r > 0))
                    for ip, i in its:
                        CP('act', catT[:, 4 * g:4 * g + 4, i * 128:(i + 1) * 128], v4(banks[bk[ip]][:, :]),
                           [BB[bk[ip]]], [B_cat[4 * g + r] for r in range(4)], acc=True)

                def pair_win(kt, qt):
                    if kt == qt:
                        return 'tri'
                    if kt == qt - 4:
                        return 'tri2'
                    if qt - 4 < kt < qt:
                        return 'full'
                    return None

                for g in range(4):
                    DMA(gn[:, :], gnsa[:, g * 512:(g + 1) * 512], [], [B_g])
                    for r in range(4):
                        DMA(QT[r][:, :], S_in[g * 4 + r][:, 1024:2048], [B_Sin[g * 4 + r]], [B_QT[r]])
                    DMA(sK[:, :], S_in[16 + 2 * 4 + g], [B_Sin[16 + 8 + g]], [B_sK])
                    DMA(wK[:, :], S_in[16 + 4 * 4 + g], [B_Sin[16 + 16 + g]], [B_wK])
                    DMA(VTs[0][:, :], S_in[16 + 3 * 4 + g], [B_Sin[16 + 12 + g]], [B_VTs[0]])
                    load_V([VTs[0]], [B_VTs[0]], sV, B_sV, 1)
                    DMA(VTs[1][:, :], S_in[16 + 5 * 4 + g], [B_Sin[16 + 20 + g]], [B_VTs[1]])
                    load_V([VTs[1]], [B_VTs[1]], wV, B_wV, 1)
                    for i0 in range(0, 8, NL):
                        ch = [(j, i0 + j) for j in range(NL)]
                        for j, i in ch:
                            A = 2 + 3 * j
                            for r in range(4):
                                MM(banks[A][:, r * 128:(r + 1) * 128], QT[r][:, i * 128:(i + 1) * 128], kcT_all[:, g, :],
                                   True, True, [B_QT[r], B_kc], [BB[A]], inc=(r == 3), acc=(r > 0))
                        for j, i in ch:
                            A = 2 + 3 * j
                            TT('dve', E4[j][:, :, :], v4(banks[A][:, :]),
                               cmpmask[:, i, :].unsqueeze(1).broadcast_to([128, 4, 128]), ALU.add,
                               [BB[A], B_cm], [B_E4[j]])
                        for j, i in ch:
                            ACT(E4[j][:, :, :], E4[j][:, :, :], AF.Exp, [B_E4[j]], [B_E4[j]])
                        for j, i in ch:
                            REDUCE('dve', rs4[j][:, 0:4], E4[j][:, :, :], ALU.add, [B_E4[j]], [B_rs4[j]])
                        for j, i in ch:
                            TS('dve', rs4[j][:, 0:4], rs4[j][:, 0:4], 1e-30, None, ALU.add, None, [B_rs4[j]], [B_rs4[j]])
                        for j, i in ch:
                            RECIP('dve', rs4[j][:, 4:8], rs4[j][:, 0:4], [B_rs4[j]], [B_rs4[j]])
                        for j, i in ch:
                            TT('dve', E4[j][:, :, :], E4[j][:, :, :],
                               rs4[j][:, 4:8].unsqueeze(2).broadcast_to([128, 4, 128]), ALU.mult,
                               [B_E4[j], B_rs4[j]], [B_E4[j]])
                        for j, i in ch:
                            REDUCE('dve', Ppad[j][:, 1:129], E4[j][:, :, :].rearrange("p r n -> p n r"), ALU.add,
                                   [B_E4[j]], [B_Pp[j]])
                        for j, i in ch:
                            CP('pool', pb4[j][:, :, :], E4[j][:, :, :], [B_E4[j]], [B_pb4[j]])
                        for j, i in ch:
                            Bk = 3 + 3 * j
                            for r in range(4):
                                MM(banks[Bk][:, r * 128:(r + 1) * 128], pb4[j][:, r, :], identb[:, :], True, True,
                                   [B_pb4[j], B_ident], [BB[Bk]], inc=(r == 3), acc=(r > 0))
                        for j, i in ch:
                            Bk = 3 + 3 * j
                            CP('act', pT4[j][:, :, :], v4(banks[Bk][:, :]), [BB[Bk]], [B_pT4[j]])
                        for j, i in ch:
                            Ck = 4 + 3 * j
                            for r in range(4):
                                MM(banks[Ck][:, r * 128:(r + 1) * 128], pT4[j][:, r, :], vc_all[:, g, :], True, True,
                                   [B_pT4[j], B_vc], [BB[Ck]], inc=(r == 3), acc=(r > 0))
                        for j, i in ch:
                            Ck = 4 + 3 * j
                            gv = gates[:, i, g * 12:(g + 1) * 12].rearrange("p (r b) -> p r b", b=3)[:, :, 0:1]
                            TT('dve', acc[:, i, :, :], v4(banks[Ck][:, :]), gv.broadcast_to([128, 4, 128]), ALU.mult,
                               [BB[Ck], B_gates], [B_acc[i]])
                        for j, i in ch:
                            Av = Ppad[j][:, 1:129].rearrange("p (j f) -> p j f", f=4)
                            TT('dve', impt[j][:, 0, :], Av[:, :, 0], Av[:, :, 1], ALU.add, [B_Pp[j]], [B_imp[j]])
                        for j, i in ch:
                            Av = Ppad[j][:, 1:129].rearrange("p (j f) -> p j f", f=4)
                            Bv = Ppad[j][:, 0:128].rearrange("p (j f) -> p j f", f=4)
                            TT('dve', impt[j][:, 1, :], Av[:, :, 3], Bv[:, :, 0], ALU.add, [B_Pp[j], B_imp[j]], [B_imp[j]])
                        for j, i in ch:
                            Av = Ppad[j][:, 1:129].rearrange("p (j f) -> p j f", f=4)
                            TT('dve', impt[j][:, 0, :], impt[j][:, 0, :], Av[:, :, 2], ALU.add, [B_Pp[j], B_imp[j]], [B_imp[j]])
                        for j, i in ch:
                            STT('dve', impt[j][:, 0, :], impt[j][:, 1, :], 0.5, impt[j][:, 0, :], ALU.mult, ALU.add,
                                [B_imp[j]], [B_imp[j]])
                        for j, i in ch:
                            TT('dve', impt[j][:, 0, :], impt[j][:, 0, :], selA[:, i, :], ALU.mult, [B_imp[j], B_sAB], [B_imp[j]])
                        for j, i in ch:
                            TT('dve', impt[j][:, 2, :], impt[j][:, 0, :], selB[:, i, :], ALU.add, [B_imp[j], B_sAB], [B_imp[j]])
                        for j, i in ch:
                            sc = impt[j][:, 2, :]
                            TT('dve', c3[j][:, :, :], sc.unsqueeze(1).broadcast_to([128, 32, 32]),
                               sc.unsqueeze(2).broadcast_to([128, 32, 32]), ALU.is_gt, [B_imp[j]], [B_c3[j]])
                        for j, i in ch:
                            REDUCE('dve', impt[j][:, 1, :], c3[j][:, :, :], ALU.add, [B_c3[j]], [B_imp[j]])
                        for j, i in ch:
                            TS('dve', selb[j][:, :], impt[j][:, 1, :], 15.5, None, ALU.is_lt, None, [B_imp[j]], [B_selb[j]])
                        for j, i in ch:
                            MM(banks[j][0:32, 0:128], selb[j][:, :], identb[:, :], True, True, [B_selb[j], B_ident], [BB[j]])
                        for j, i in ch:
                            CP('act', selT[:, i * 128:(i + 1) * 128], banks[j][0:32, 0:128], [BB[j]], [B_selT], acc=True)
                    for c in range(2):
                        qt0 = 8 + 4 * c
                        for kt in range(0, qt0 + 4):
                            bi = 6 + (rg['m'] % 2)
                            rg['m'] += 1
                            MM(banks[bi][:, :], expb[:, kt * 128:(kt + 1) * 128], selT[:, c * 512:(c + 1) * 512],
                               True, True, [B_exp, B_selT], [BB[bi]])
                            CP('act', MT[:, kt, :], banks[bi][:, :], [BB[bi]], [B_MT], acc=True)
                            if kt >= qt0:
                                ip = kt - qt0
                                TT('pool', MT[:, kt, ip * 128:(ip + 1) * 128], MT[:, kt, ip * 128:(ip + 1) * 128],
                                   tri[:, :], ALU.mult, [B_MT, B_tri], [B_MT])
                        for r in range(4):
                            h = g * 4 + r
                            dense_attn(1, [QT[r]], [B_QT[r]], [sK], [B_sK], sV, B_sV, 128, nsa_slope(h), 512, [c],
                                       lambda qt0_: 0, lambda kt, qt: ('full' if kt <= qt else None), MT, B_MT,
                                       lambda items, r=r, h=h: fin_add(items, r, h, 1))
                    for c in range(2):
                        for r in range(4):
                            h = g * 4 + r
                            dense_attn(1, [QT[r]], [B_QT[r]], [wK], [B_wK], wV, B_wV, 128, nsa_slope(h), 512, [c],
                                       lambda qt0_: qt0_ - 4, pair_win, None, None,
                                       lambda items, r=r, h=h: fin_add(items, r, h, 2))
                        rms_batch(g, c)
                T.barrier()
                T.emit()

            with ExitStack() as es2:
                QT = [alloc(es2, f"dQT{m}", [128, 1024], BF16) for m in range(2)]
                B_QT = [Buf(f"dQT{m}") for m in range(2)]
                KT = [alloc(es2, f"dKT{m}", [128, 2048], BF16) for m in range(2)]
                B_KT = [Buf(f"dKT{m}") for m in range(2)]
                VT = [alloc(es2, f"dVT{m}", [128, 2048], BF16) for m in range(2)]
                B_VT = [Buf(f"dVT{m}") for m in range(2)]
                dV = alloc(es2, "dV", [128, 16, 260], BF16)
                B_dV = Buf("dV")
                MS('pool', dV[:, :, 256:257], 1.0, [B_dV])
                of2 = [alloc(es2, f"of2{j}", [128, 256], F32) for j in range(2)]
                B_of2 = [Buf(f"of2{j}") for j in range(2)]
                sq2 = [alloc(es2, f"sq2{j}", [128, 256], F32) for j in range(2)]
                B_sq2 = [Buf(f"sq2{j}") for j in range(2)]
                ob2 = [alloc(es2, f"ob2{j}", [128, 256], BF16) for j in range(2)]
                B_ob2 = [Buf(f"ob2{j}") for j in range(2)]
                d8 = [alloc(es2, f"d8{j}", [128, 8], F32) for j in range(2)]
                B_d8 = [Buf(f"d8{j}") for j in range(2)]

                def fin_diff(items, h):
                    E = list(enumerate(items))
                    for ip, (i, obs) in E:
                        TS('dve', d8[ip][:, 0:1], banks[obs[0]][:, 256:257], 1e-30, None, ALU.add, None, [BB[obs[0]]], [B_d8[ip]])
                    for ip, (i, obs) in E:
                        TS('dve', d8[ip][:, 2:3], banks[obs[1]][:, 256:257], 1e-30, None, ALU.add, None, [BB[obs[1]]], [B_d8[ip]])
                    for ip, (i, obs) in E:
                        RECIP('dve', d8[ip][:, 1:2], d8[ip][:, 0:1], [B_d8[ip]], [B_d8[ip]])
                    for ip, (i, obs) in E:
                        RECIP('dve', d8[ip][:, 3:4], d8[ip][:, 2:3], [B_d8[ip]], [B_d8[ip]])
                    for ip, (i, obs) in E:
                        TT('dve', d8[ip][:, 4:5], d8[ip][:, 3:4], neglam, ALU.mult, [B_d8[ip], B_lam], [B_d8[ip]])
                    for ip, (i, obs) in E:
                        TS('dve', of2[ip][:, :], banks[obs[0]][:, 0:256], d8[ip][:, 1:2], None, ALU.mult, None,
                           [BB[obs[0]], B_d8[ip]], [B_of2[ip]])
                    for ip, (i, obs) in E:
                        STT('dve', of2[ip][:, :], banks[obs[1]][:, 0:256], d8[ip][:, 4:5], of2[ip][:, :], ALU.mult, ALU.add,
                            [BB[obs[1]], B_d8[ip], B_of2[ip]], [B_of2[ip]])
                    for ip, (i, obs) in E:
                        TT('pool', sq2[ip][:, :], of2[ip][:, :], of2[ip][:, :], ALU.mult, [B_of2[ip]], [B_sq2[ip]])
                    for ip, (i, obs) in E:
                        REDUCE('dve', d8[ip][:, 5:6], sq2[ip][:, :], ALU.add, [B_sq2[ip]], [B_d8[ip]])
                    for ip, (i, obs) in E:
                        TS('dve', d8[ip][:, 5:6], d8[ip][:, 5:6], 1.0 / 256, RMS_EPS, ALU.mult, ALU.add, [B_d8[ip]], [B_d8[ip]])
                    for ip, (i, obs) in E:
                        ACT(d8[ip][:, 6:7], d8[ip][:, 5:6], AF.Ln, [B_d8[ip]], [B_d8[ip]])
                    for ip, (i, obs) in E:
                        ACT(d8[ip][:, 7:8], d8[ip][:, 6:7], AF.Exp, [B_d8[ip]], [B_d8[ip]], scale=-0.5)
                    for ip, (i, obs) in E:
                        TS('dve', d8[ip][:, 7:8], d8[ip][:, 7:8], 0.8, None, ALU.mult, None, [B_d8[ip]], [B_d8[ip]])
                    for ip, (i, obs) in E:
                        STT('dve', ob2[ip][:, :], of2[ip][:, :], d8[ip][:, 7:8], gd[:, :], ALU.mult, ALU.mult,
                            [B_of2[ip], B_d8[ip], B_gd], [B_ob2[ip]])
                    for ip, (i, obs) in E:
                        bk = 6 + ip
                        for hf in range(2):
                            MM(banks[bk][:, hf * 128:(hf + 1) * 128], ob2[ip][:, hf * 128:(hf + 1) * 128], identb[:, :],
                               True, True, [B_ob2[ip], B_ident], [BB[bk]], inc=(hf == 1), acc=(hf > 0))
                    for ip, (i, obs) in E:
                        bk = 6 + ip
                        CP('act', catT[:, 16 + 2 * h:16 + 2 * h + 2, i * 128:(i + 1) * 128],
                           banks[bk][:, 0:256].rearrange("p (c q) -> p c q", c=2), [BB[bk]],
                           [B_cat[16 + 2 * h], B_cat[16 + 2 * h + 1]], acc=True)

                for h in range(8):
                    for m in range(2):
                        DMA(QT[m][:, :], S_in[40 + 2 * h + m][:, 1024:2048], [B_Sin[40 + 2 * h + m]], [B_QT[m]])
                        DMA(KT[m][:, :], S_in[56 + 2 * h + m], [B_Sin[56 + 2 * h + m]], [B_KT[m]])
                        DMA(VT[m][:, :], S_in[72 + 2 * h + m], [B_Sin[72 + 2 * h + m]], [B_VT[m]])
                    load_V(VT, B_VT, dV, B_dV, 2)
                    dense_attn(2, QT, B_QT, KT, B_KT, dV, B_dV, 256, diff_slope(h), 256, [0, 1, 2, 3],
                               lambda qt0_: 0, lambda kt, qt: ('tri' if kt == qt else ('full' if kt < qt else None)),
                               None, None, lambda items, h=h: fin_diff(items, h))
                if debug:
                    DMA(dbg_cat[:, :], catT[:, :, :].rearrange("p k n -> p (k n)"), B_cat, [Buf("dbgc")])
                T.barrier()
                T.emit()

        def ln_pass(es, src, B_src_fn, gi, bi_, emit_out):
            ys = [alloc(es, f"lny{i}", [128, 1024], F32) for i in range(2)]
            B_ys = [Buf(f"lny{i}") for i in range(2)]
            sq = [alloc(es, f"lnq{i}", [128, 1024], F32) for i in range(2)]
            B_sq = [Buf(f"lnq{i}") for i in range(2)]
            mean = alloc(es, "lnmean", [128, 1024], F32)
            rstd = alloc(es, "lnrstd", [128, 1024], F32)
            nmr = alloc(es, "lnnmr", [128, 1024], F32)
            B_st = Buf("lnstat")
            ones = alloc(es, "lnones", [128, 128], F32)
            B_ones = Buf("lnones")
            osts = [alloc(es, f"lno{i}", [128, 1024], F32) for i in range(2)]
            B_os = [Buf(f"lno{i}") for i in range(2)]
            for b in B_os:
                b.dsem = T.newsem("ln_" + b.name)
            MS('dve', ones[:, :], 1.0, [B_ones])
            for k in range(32):
                s = k % 2
                DMA(ys[s][:, :], src[k], B_src_fn(k), [B_ys[s]])
                ACT(sq[s][:, :], ys[s][:, :], AF.Square, [B_ys[s]], [B_sq[s]])
                for tg in range(2):
                    MM(banks[tg][:, :], ones[:, :], ys[s][:, tg * 512:(tg + 1) * 512], (k == 0), (k == 31),
                       [B_ones, B_ys[s]], [BB[tg]], acc=(k > 0))
                    MM(banks[2 + tg][:, :], ones[:, :], sq[s][:, tg * 512:(tg + 1) * 512], (k == 0), (k == 31),
                       [B_ones, B_sq[s]], [BB[2 + tg]], acc=(k > 0))
            for tg in range(2):
                sl = slice(tg * 512, (tg + 1) * 512)
                ACT(mean[:, sl], banks[tg][:, :], AF.Copy, [BB[tg]], [B_st], scale=1.0 / 4096, acc=True)
                ACT(rstd[:, sl], banks[2 + tg][:, :], AF.Copy, [BB[2 + tg]], [B_st], scale=1.0 / 4096, acc=True)
            TT('dve', nmr[:, :], mean[:, :], mean[:, :], ALU.mult, [B_st], [B_st])
            TT('dve', rstd[:, :], rstd[:, :], nmr[:, :], ALU.subtract, [B_st], [B_st])
            TS('dve', rstd[:, :], rstd[:, :], LN_EPS, None, ALU.add, None, [B_st], [B_st])
            ACT(rstd[:, :], rstd[:, :], AF.Ln, [B_st], [B_st])
            ACT(rstd[:, :], rstd[:, :], AF.Exp, [B_st], [B_st], scale=-0.5)
            STT('dve', nmr[:, :], mean[:, :], -1.0, rstd[:, :], ALU.mult, ALU.mult, [B_st], [B_st])
            for k in range(32):
                s = k % 2
                DMA(ys[s][:, :], src[k], B_src_fn(k), [B_ys[s]])
                TT('dve', ys[s][:, :], ys[s][:, :], rstd[:, :], ALU.mult, [B_ys[s], B_st], [B_ys[s]])
                TT('pool', ys[s][:, :], ys[s][:, :], nmr[:, :], ALU.add, [B_ys[s], B_st], [B_ys[s]])
                TS('dve' if k % 2 == 0 else 'pool', osts[s][:, :], ys[s][:, :], lnp_t[:, gi * 32 + k:gi * 32 + k + 1],
                   lnp_t[:, bi_ * 32 + k:bi_ * 32 + k + 1], ALU.mult, ALU.add, [B_ys[s], B_lnp], [B_os[s]])
                emit_out(k, osts[s], B_os[s])

        x1T = None
        with ExitStack() as es:
            P = make_pipe(es)
            xres = [alloc(es, f"xres{i}", [128, 1024], F32) for i in range(2)]
            B_xres = [Buf(f"xres{i}") for i in range(2)]
            for b in B_xres:
                b.dsem = T.newsem("xr_" + b.name)
                b.dsem2 = T.newsem("xs_" + b.name)
            stc = {'c': 0}

            def mk_pre(k):
                def pre():
                    s = k % 2
                    DMA(xres[s][:, :], xT[k * 128:(k + 1) * 128, 1024:2048], [], [B_xres[s]], dsem=B_xres[s].dsem)
                return pre

            def mk_ev(k):
                def ev(bset):
                    s = k % 2
                    for tg in range(2):
                        sl = slice(tg * 512, (tg + 1) * 512)
                        STT('dve', xres[s][:, sl], xres[s][:, sl], ALPHA, banks[bset + tg][:, :], ALU.mult, ALU.add,
                            [B_xres[s], BB[bset + tg]], [B_xres[s]])
                    DMA(Ys[k], xres[s][:, :], [B_xres[s]], [B_Ys[k]], dsem=B_xres[s].dsem2)
                return ev

            def rhs_cat(k, tg):
                return catT[:, k, tg * 512:(tg + 1) * 512], [B_cat[k]]
            jobs = [dict(src=w_out[k], KC=32, MC=1, rhs=rhs_cat, pre=mk_pre(k), evac=mk_ev(k)) for k in range(32)]
            gemm(P, jobs)
            T.barrier()
            T.emit()
        esBC.__exit__(None, None, None)
        x1T = alloc(top, "x1T", [128, 32, 1024], BF16)
        B_x1 = [Buf(f"x1_{k}") for k in range(32)]
        with ExitStack() as es:
            def out1(k, t, B_t):
                DMA(X1s[k], t[:, :], [B_t], [B_X1s[k]], dsem=B_t.dsem)
                CP('act', x1T[:, k, :], t[:, :], [B_t], [B_x1[k]])
            ln_pass(es, Ys, lambda k: [B_Ys[k]], 0, 1, out1)
            T.barrier()
            T.emit()

        with ExitStack() as es:
            P = make_pipe(es, nwb=2)
            hT = alloc(es, "hT", [128, FPR, 1024], BF16)
            B_h = [Buf(f"h{i}") for i in range(FPR)]
            rt = [alloc(es, f"rt{i}", [128, 512], F32) for i in range(2)]
            B_rt = [Buf(f"rt{i}") for i in range(2)]
            pst = [alloc(es, f"pst{i}", [128, 2, 1024], F32) for i in range(2)]
            B_pst = [Buf(f"pst{i}") for i in range(2)]
            for b in B_pst:
                b.dsem = T.newsem("pl_" + b.name)
                b.dsem2 = T.newsem("ps_" + b.name)
            std = {'r': 0}

            def rhs_x1(k, tg):
                return x1T[:, k, tg * 512:(tg + 1) * 512], [B_x1[k]]

            def mk_ev1(fcl):
                def ev(bset):
                    for tg in range(2):
                        s = std['r'] % 2
                        std['r'] += 1
                        ACT(rt[s][:, :], banks[bset + tg][:, :], AF.Relu, [BB[bset + tg]], [B_rt[s]])
                        TT('pool', hT[:, fcl, tg * 512:(tg + 1) * 512], rt[s][:, :], rt[s][:, :], ALU.mult,
                           [B_rt[s]], [B_h[fcl]], acc=(tg == 1))
                return ev

            def rhs_h(k, tg):
                return hT[:, k, tg * 512:(tg + 1) * 512], [B_h[k]]

            def mk_pre2(rq, db):
                def pre():
                    s = db % 2
                    if rq == 0:
                        srcv = X1s[2 * db:2 * db + 2].rearrange("c p n -> p c n")
                        rb = [B_X1s[2 * db], B_X1s[2 * db + 1]]
                    else:
                        srcv = Y2s[2 * db:2 * db + 2].rearrange("c p n -> p c n")
                        rb = [B_Y2s[db]]
                    DMA(pst[s][:, :, :], srcv, rb, [B_pst[s]], dsem=B_pst[s].dsem)
                return pre

            def mk_ev2(rq, db):
                def ev(bset):
                    s = db % 2
                    for mc in range(2):
                        for tg in range(2):
                            sl = slice(tg * 512, (tg + 1) * 512)
                            STT('dve', pst[s][:, mc, sl], pst[s][:, mc, sl], (ALPHA if rq == 0 else 1.0),
                                banks[bset + mc * 2 + tg][:, :], ALU.mult, ALU.add,
                                [B_pst[s], BB[bset + mc * 2 + tg]], [B_pst[s]])
                    DMA(Y2s[2 * db:2 * db + 2].rearrange("c p n -> p c n"), pst[s][:, :, :], [B_pst[s]], [B_Y2s[db]],
                        dsem=B_pst[s].dsem2)
                return ev
            for rq in range(NFQ):
                jobs = [dict(src=w_ff1[rq * FPR + f], KC=32, MC=1, rhs=rhs_x1, evac=mk_ev1(f)) for f in range(FPR)]
                gemm(P, jobs)
                jobs = [dict(src=w_ff2[db, rq], KC=FPR, MC=2, rhs=rhs_h, pre=mk_pre2(rq, db), evac=mk_ev2(rq, db))
                        for db in range(16)]
                gemm(P, jobs)
            T.barrier()
            T.emit()

        with ExitStack() as es:
            def out2(k, t, B_t):
                DMA(outT[k * 128:(k + 1) * 128, :], t[:, :], [B_t], [B_out], dsem=B_t.dsem, acc=True)
            ln_pass(es, Y2s, lambda k: [B_Y2s[k // 2]], 2, 3, out2)
            T.barrier()
            T.emit()
    return nc, T


def _blk(w, c0, n=128):
    t = np.zeros((4096, 128), np.float32)
    t[:, :n] = w[:, c0:c0 + n]
    return t.reshape(32, 128, 128).transpose(1, 0, 2).reshape(128, 4096)


def _prep_shared(inp):
    w_in = inp["w_in"][0]
    starts = []
    for h in range(16):
        starts.append(h * 128)
    for j in range(24):
        starts.append(2048 + j * 128)
    for j in range(16):
        starts.append(5168 + j * 128)
    for j in range(16):
        starts.append(7216 + j * 128)
    for j in range(16):
        starts.append(9264 + j * 128)
    w_in_r = np.empty((89, 128, 4096), np.float32)
    for b, c0 in enumerate(starts):
        w_in_r[b] = _blk(w_in, c0)
    w_in_r[88] = _blk(w_in, 5120, 48)
    w_out = inp["w_out"][0]
    w_out_r = np.ascontiguousarray(w_out.reshape(32, 128, 32, 128).transpose(2, 1, 0, 3)).reshape(32, 128, 4096)
    w1 = inp["w_ff1"][0]
    w_ff1_r = np.ascontiguousarray(w1.reshape(32, 128, 128, 128).transpose(2, 1, 0, 3)).reshape(128, 128, 4096)
    w2 = inp["w_ff2"][0]
    w_ff2_r = np.ascontiguousarray(
        w2.reshape(NFQ, FPR, 128, 16, 256).transpose(3, 0, 2, 1, 4)).reshape(16, NFQ, 128, FPR * 256)
    w1c = np.empty((2, 2, 128, 4096), np.float32)
    w2c = np.empty((2, 128, 256), np.float32)
    pec = np.empty((2, 128, 32), np.float32)
    for kv, nm in enumerate(["k", "v"]):
        a = inp["cmp_w1_" + nm][0].reshape(32, 128, 256).transpose(1, 0, 2)
        w1c[kv, 0] = a[:, 0:16, :].reshape(128, 4096)
        w1c[kv, 1] = a[:, 16:32, :].reshape(128, 4096)
        w2c[kv] = inp["cmp_w2_" + nm][0].reshape(2, 128, 128).transpose(1, 0, 2).reshape(128, 256)
        pec[kv] = inp["cmp_pe_" + nm][0].T
    gnsa = np.ascontiguousarray(np.broadcast_to(inp["nsa_out_g"][0].reshape(1, 2048), (128, 2048))).astype(np.float32)
    gdif = np.ascontiguousarray(np.broadcast_to(inp["diff_subln_g"][0].reshape(1, 256), (128, 256))).astype(np.float32)
    lv = np.concatenate([inp["lambda_q1"][0], inp["lambda_k1"][0], inp["lambda_q2"][0], inp["lambda_k2"][0]])
    lamv = np.ascontiguousarray(np.broadcast_to(lv.reshape(1, 512), (128, 512))).astype(np.float32)
    lnp = np.concatenate([inp[n][0].reshape(32, 128).T for n in ["ln1_g", "ln1_b", "ln2_g", "ln2_b"]], axis=1)
    j = np.arange(128)[:, None]
    i = np.arange(128)[None, :]
    u = np.arange(DW)[None, :]
    sh = dict(
        w_in=w_in_r, w_out=w_out_r, w_ff1=w_ff1_r, w_ff2=w_ff2_r, w1c=w1c, w2c=w2c, pec=pec,
        gnsa=gnsa, gdif=gdif, lamv=lamv, lnp=np.ascontiguousarray(lnp.astype(np.float32)),
        c_ident=np.eye(128, dtype=np.float32),
        c_tri=(j <= i).astype(np.float32),
        c_expand=(np.arange(2048)[None, :] // 64 == np.arange(32)[:, None]).astype(np.float32),
        c_dclamp=np.maximum(u - DOFF - j, 0).astype(np.float32),
    )
    return sh


def _prep_core(hh):
    q = np.arange(128)[:, None, None]
    it = np.arange(8)[None, :, None]
    tl = 1024 + 128 * it + q
    n = np.arange(128)[None, None, :]
    okn = (n <= 126) & (16 * n + 31 <= tl)
    if hh == 0:
        okn = okn & (n >= 64)
    cmpmask = np.where(okn, 0.0, NEGB).astype(np.float32).reshape(128, 1024)
    jb = np.arange(32)[None, None, :]
    valid = (64 * jb <= tl)
    if hh == 0:
        valid = valid & (jb >= 16)
    tb = tl // 64
    first = 0 if hh == 1 else 16
    forced = (jb == first) | (jb == tb) | (jb == tb - 1)
    A = np.where(valid & ~forced, 1.0, 0.0)
    Bm = np.where(~valid, -1.0, np.where(forced, 1e6, 0.0))
    ctxb = np.full((128, 1), 0.0 if hh == 1 else NEGB, np.float32)
    return dict(p_cmpmask=cmpmask, p_selA=A.astype(np.float32).reshape(128, 256),
                p_selB=Bm.astype(np.float32).reshape(128, 256), p_ctxb=ctxb)


_CACHE = {}


def kernel(**inputs):
    debug = bool(int(os.environ.get("MK_DEBUG", "0")))
    inp = {k: np.asarray(v) for k, v in inputs.items()}
    x = inp["x"]
    sh = _prep_shared(inp)
    in_maps = []
    for c in range(8):
        b, hh = c // 2, c % 2
        xb = x[b]
        xT = np.zeros((4096, 2048), np.float32)
        if hh == 1:
            xT[:, :] = xb.T
        else:
            xT[:, 1024:] = xb[:1024].T
        m = dict(sh)
        m.update(_prep_core(hh))
        m["xT"] = xT
        in_maps.append(m)
    if 'nc' not in _CACHE:
        _CACHE['nc'] = build(debug)[0]
    nc = _CACHE['nc']
    res = run_bass_kernel_spmd(nc, in_maps, core_ids=list(range(8)))
    out = np.empty((4, 2048, 4096), np.float32)
    for c in range(8):
        b, hh = c // 2, c % 2
        out[b, hh * 1024:(hh + 1) * 1024, :] = res.results[c]["outT"].T
    if debug:
        _CACHE['res'] = res
    return out
```

```python
import os
import numpy as np
import concourse.bass as bass
import concourse.mybir as mybir
from concourse.bass_utils import run_bass_kernel_spmd
from contextlib import ExitStack

F32 = mybir.dt.float32
BF16 = mybir.dt.bfloat16
AF = mybir.ActivationFunctionType
ALU = mybir.AluOpType
AX = mybir.AxisListType

ALPHA = 2.0 ** 0.25
QSCALE = 128.0 ** -0.5
LN_EPS = 1e-5
RMS_EPS = 1e-6
NEGB = -30000.0
NFQ = 8
FPR = 128 // NFQ
DOFF = 896
DW = 3200
ENGS = ['pe', 'act', 'dve', 'pool', 'sp']


class Buf:
    def __init__(self, name):
        self.name = name
        self.w = {}
        self.r = {}
        self.dsem = None


class DSem:
    def __init__(self, h):
        self.h = h
        self.cnt = 0


class Trk:
    def __init__(self, nc):
        self.nc = nc
        self.q = {e: [] for e in ENGS}
        self.sem = {e: nc.alloc_semaphore(name='c_' + e) for e in ENGS if e != 'sp'}
        self.cnt = {e: 0 for e in ENGS}
        self.seen = {e: {} for e in ENGS}
        self.same = True
        self.dsems = []
        self.sems = {}
        for e in self.sem:
            self.sems[id(self.sem[e])] = self.sem[e]
        self.ninst = 0

    def newsem(self, name):
        name = f"{name}_{len(self.dsems)}"
        d = DSem(self.nc.alloc_semaphore(name=name))
        self.dsems.append(d)
        self.sems[id(d.h)] = d.h
        return d

    def _wait(self, eng, deps):
        for sid, val in deps.items():
            sem = self.sems[sid]
            own = (eng != 'sp' and sem is self.sem[eng])
            if own:
                if eng == 'pe' or not self.same or val > self.cnt[eng]:
                    continue
            if self.seen[eng].get(sid, 0) >= val:
                continue
            self.seen[eng][sid] = val
            self.ninst += 1
            self.q[eng].append(lambda h, sem=sem, val=val: h.wait_ge(sem, val))

    def _deps(self, reads, writes, acc):
        deps = {}

        def add(d):
            for s, v in d.items():
                if deps.get(s, 0) < v:
                    deps[s] = v
        for b in reads:
            add(b.w)
        for b in writes:
            add(b.r)
            if not acc:
                add(b.w)
        return deps

    def _record(self, tok, reads, writes, acc):
        s, v = tok
        for b in reads:
            b.r[s] = max(b.r.get(s, 0), v)
        for b in writes:
            if acc:
                b.w[s] = max(b.w.get(s, 0), v)
            else:
                b.w = {s: v}
                b.r = {}

    def op(self, eng, fn, reads=(), writes=(), inc=True, acc=False):
        self._wait(eng, self._deps(reads, writes, acc))
        sem = self.sem[eng]
        self.ninst += 1
        if inc:
            self.cnt[eng] += 1
            tok = (id(sem), self.cnt[eng])
            self.q[eng].append(lambda h, fn=fn, sem=sem: fn(h).then_inc(sem, 1))
        else:
            tok = (id(sem), self.cnt[eng] + 1)
            self.q[eng].append(lambda h, fn=fn: fn(h))
        self._record(tok, reads, writes, acc)

    def dma(self, pairs, reads=(), writes=(), dsem=None, acc=False, qeng='sp'):
        if dsem is None:
            b = writes[0]
            if b.dsem is None:
                b.dsem = self.newsem('d_' + b.name)
            dsem = b.dsem
        self._wait(qeng, self._deps(reads, writes, acc))
        for (o, i) in pairs:
            dsem.cnt += 16
            self.ninst += 1
            self.q[qeng].append(lambda h, o=o, i=i, s=dsem.h: h.dma_start(out=o, in_=i).then_inc(s, 16))
        self._record((id(dsem.h), dsem.cnt), reads, writes, acc)

    def barrier(self):
        deps = {}
        for e in self.sem:
            if self.cnt[e] > 0:
                deps[id(self.sem[e])] = self.cnt[e]
        for d in self.dsems:
            if d.cnt > 0:
                deps[id(d.h)] = d.cnt
        for e in ENGS:
            self._wait(e, deps)

    def emit(self):
        q = self.q
        with self.nc.Block() as block:
            @block.tensor
            def _(h):
                for f in q['pe']:
                    f(h)

            @block.scalar
            def _(h):
                for f in q['act']:
                    f(h)

            @block.vector
            def _(h):
                for f in q['dve']:
                    f(h)

            @block.gpsimd
            def _(h):
                for f in q['pool']:
                    f(h)

            @block.sync
            def _(h):
                for f in q['sp']:
                    f(h)
        self.q = {e: [] for e in ENGS}


def nsa_slope(h):
    return float(2.0 ** (-8.0 * (h + 1) / 16))


def diff_slope(h):
    return float(2.0 ** (-8.0 * (h + 1) / 8))


def build(debug=False):
    nc = bass.Bass("TRN2", target_bir_lowering=False)

    def din(name, shape, dt=F32):
        return nc.dram_tensor(name, shape, dt, kind="ExternalInput").ap()

    def dscr(name, shape, dt):
        return nc.dram_tensor(name, shape, dt, kind=("ExternalOutput" if debug else "Internal")).ap()

    xT = din("xT", [4096, 2048])
    w_in = din("w_in", [89, 128, 4096])
    w_out = din("w_out", [32, 128, 4096])
    w_ff1 = din("w_ff1", [128, 128, 4096])
    w_ff2 = din("w_ff2", [16, NFQ, 128, 4096])
    w1c = din("w1c", [2, 2, 128, 4096])
    w2c = din("w2c", [2, 128, 256])
    pec = din("pec", [2, 128, 32])
    gnsa = din("gnsa", [128, 2048])
    gdif = din("gdif", [128, 256])
    lamv = din("lamv", [128, 512])
    lnp = din("lnp", [128, 128])
    c_ident = din("c_ident", [128, 128])
    c_tri = din("c_tri", [128, 128])
    c_expand = din("c_expand", [32, 2048])
    c_dclamp = din("c_dclamp", [128, DW])
    p_ctxb = din("p_ctxb", [128, 1])
    p_cmpmask = din("p_cmpmask", [128, 1024])
    p_selA = din("p_selA", [128, 256])
    p_selB = din("p_selB", [128, 256])
    outT = nc.dram_tensor("outT", [4096, 1024], F32, kind="ExternalOutput").ap()
    S_in = dscr("S_in", [88, 128, 2048], BF16)
    Ys = dscr("Ys", [32, 128, 1024], F32)
    X1s = dscr("X1s", [32, 128, 1024], F32)
    Y2s = dscr("Y2s", [32, 128, 1024], F32)
    dbg_cat = dscr("dbg_cat", [128, 32 * 1024], BF16) if debug else None

    T = Trk(nc)
    B_out = Buf("outT")
    B_Sin = [Buf(f"Sin{i}") for i in range(88)]
    B_Ys = [Buf(f"Ys{i}") for i in range(32)]
    B_X1s = [Buf(f"X1s{i}") for i in range(32)]
    B_Y2s = [Buf(f"Y2s{i}") for i in range(16)]

    def TT(eng, out, in0, in1, op, reads, writes, **kw):
        T.op(eng, lambda h: h.tensor_tensor(out=out, in0=in0, in1=in1, op=op), reads, writes, **kw)

    def STT(eng, out, in0, scalar, in1, op0, op1, reads, writes, **kw):
        T.op(eng, lambda h: h.scalar_tensor_tensor(out=out, in0=in0, scalar=scalar, in1=in1, op0=op0, op1=op1),
             reads, writes, **kw)

    def TS(eng, out, in0, s1, s2, op0, op1, reads, writes, **kw):
        if op1 is None:
            T.op(eng, lambda h: h.tensor_scalar(out=out, in0=in0, scalar1=s1, scalar2=None, op0=op0), reads, writes, **kw)
        else:
            T.op(eng, lambda h: h.tensor_scalar(out=out, in0=in0, scalar1=s1, scalar2=s2, op0=op0, op1=op1),
                 reads, writes, **kw)

    def ACT(out, in_, func, reads, writes, bias=None, scale=None, accum_out=None, **kw):
        kws = {}
        if bias is not None:
            kws['bias'] = bias
        if scale is not None:
            kws['scale'] = scale
        if accum_out is not None:
            kws['accum_out'] = accum_out
        T.op('act', lambda h: h.activation(out=out, in_=in_, func=func, **kws), reads, writes, **kw)

    def CP(eng, out, in_, reads, writes, **kw):
        if eng == 'act':
            ACT(out, in_, AF.Copy, reads, writes, **kw)
        else:
            T.op(eng, lambda h: h.tensor_copy(out=out, in_=in_), reads, writes, **kw)

    def MS(eng, ap, val, writes):
        T.op(eng, lambda h: h.memset(ap, val), (), writes)

    def MM(out, lhsT, rhs, start, stop, reads, writes, inc=True, acc=False):
        T.op('pe', lambda h: h.matmul(out, lhsT, rhs, start=start, stop=stop), reads, writes, inc=inc, acc=acc)

    def DMA(out, in_, reads, writes, dsem=None, acc=False):
        T.dma([(out, in_)], reads, writes, dsem=dsem, acc=acc)

    def REDUCE(eng, out, in_, op, reads, writes):
        T.op(eng, lambda h: h.tensor_reduce(out=out, in_=in_, axis=AX.X, op=op), reads, writes)

    def RECIP(eng, out, in_, reads, writes):
        T.op(eng, lambda h: h.reciprocal(out=out, in_=in_), reads, writes)

    with ExitStack() as top:
        acnt = {'n': 0}

        def alloc(es, name, shape, dt):
            acnt['n'] += 1
            return es.enter_context(nc.sbuf_tensor(f"{name}_{acnt['n']}", shape, dt))

        banks = [top.enter_context(nc.psum_tensor(f"bank{i}", [128, 512], F32)) for i in range(8)]
        BB = [Buf(f"bank{i}") for i in range(8)]

        gates = alloc(top, "gates", [128, 8, 48], F32)
        B_gates = Buf("gates")
        lnp_t = alloc(top, "lnp_t", [128, 128], F32)
        B_lnp = Buf("lnp")
        identf = alloc(top, "identf", [128, 128], F32)
        identb = alloc(top, "identb", [128, 128], BF16)
        B_ident = Buf("ident")
        zcol = alloc(top, "zcol", [128, 1], F32)
        B_zcol = Buf("zcol")
        DMA(lnp_t[:, :], lnp[:, :], [], [B_lnp])
        DMA(identf[:, :], c_ident[:, :], [], [B_ident])
        CP('dve', identb[:, :], identf[:, :], [B_ident], [B_ident])
        MS('dve', zcol[:, :], 0.0, [B_zcol])

        class Pipe:
            pass

        def make_pipe(es, nwb=3):
            P = Pipe()
            P.stg = [alloc(es, f"stg{i}", [128, 4096], F32) for i in range(2)]
            P.B_stg = [Buf(f"stg{i}") for i in range(2)]
            P.wb = [alloc(es, f"wb{i}", [128, 4096], BF16) for i in range(nwb)]
            P.B_wb = [Buf(f"wb{i}") for i in range(nwb)]
            P.nwb = nwb
            P.jc = 0
            return P

        def gemm(P, jobs):
            n = len(jobs)
            base = P.jc

            def issue_dma(j):
                s = (base + j) % 2
                DMA(P.stg[s][:, :], jobs[j]['src'], [], [P.B_stg[s]])

            def issue_cast(j):
                s = (base + j) % 2
                w = (base + j) % P.nwb
                eng = 'dve' if (base + j) % 2 == 0 else 'pool'
                CP(eng, P.wb[w][:, :], P.stg[s][:, :], [P.B_stg[s]], [P.B_wb[w]])
            issue_dma(0)
            if n > 1:
                issue_dma(1)
            issue_cast(0)
            for j in range(n):
                job = jobs[j]
                if j + 1 < n:
                    issue_cast(j + 1)
                if j + 2 < n:
                    issue_dma(j + 2)
                w = (base + j) % P.nwb
                bset = 4 * ((base + j) % 2)
                if job.get('pre') is not None:
                    job['pre']()
                if job.get('custom') is not None:
                    job['custom'](P.wb[w], P.B_wb[w], bset)
                    continue
                KC, MC = job['KC'], job['MC']
                wv = P.wb[w][:, :].rearrange("p (k c) -> p k c", c=MC * 128)
                for k in range(KC):
                    for mc in range(MC):
                        for tg in range(2):
                            bi = bset + mc * 2 + tg
                            rap, rb = job['rhs'](k, tg)
                            last = (k == KC - 1 and mc == MC - 1 and tg == 1)
                            MM(banks[bi][:, :], wv[:, k, mc * 128:(mc + 1) * 128], rap,
                               start=(k == 0), stop=(k == KC - 1), reads=[P.B_wb[w]] + rb, writes=[BB[bi]],
                               inc=last, acc=(k > 0))
                job['evac'](bset)
            P.jc += n

        with ExitStack() as es:
            P = make_pipe(es)
            actT = alloc(es, "actT", [128, 32, 1024], BF16)
            B_act = [Buf(f"act{i}") for i in range(16)]
            xst = [alloc(es, f"xst{i}", [128, 2, 1024], F32) for i in range(2)]
            B_xst = [Buf(f"xst{i}") for i in range(2)]
            ost = [alloc(es, f"ost{i}", [128, 1024], BF16) for i in range(2)]
            B_ost = [Buf(f"ost{i}") for i in range(2)]
            for b in B_ost:
                b.dsem = T.newsem("st_" + b.name)
            xTv = xT.rearrange("(k p) n -> p k n", p=128)
            st = {'oc': 0}

            def load_x(tok0):
                for j in range(16):
                    s = j % 2
                    DMA(xst[s][:, :, :], xTv[:, 2 * j:2 * j + 2, tok0:tok0 + 1024], [], [B_xst[s]])
                    CP('dve' if j % 2 == 0 else 'pool', actT[:, 2 * j:2 * j + 2, :], xst[s][:, :, :], [B_xst[s]], [B_act[j]])

            def rhs_act(k, tg):
                return actT[:, k, tg * 512:(tg + 1) * 512], [B_act[k // 2]]

            def mk_evac_in(blk, tok0, scale):
                def ev(bset):
                    s = st['oc'] % 2
                    st['oc'] += 1
                    for tg in range(2):
                        ACT(ost[s][:, tg * 512:(tg + 1) * 512], banks[bset + tg][:, :], AF.Copy,
                            [BB[bset + tg]], [B_ost[s]], scale=scale, acc=(tg == 1))
                    DMA(S_in[blk][:, tok0:tok0 + 1024], ost[s][:, :], [B_ost[s]], [B_Sin[blk]], dsem=B_ost[s].dsem, acc=True)
                return ev

            def gate_custom(wbt, B_wbt, bset):
                wv = wbt[:, :].rearrange("p (k c) -> p k c", c=128)
                for i in range(8):
                    bi = bset + (i % 4)
                    for k in range(32):
                        MM(banks[bi][:, 0:128], actT[:, k, i * 128:(i + 1) * 128], wv[:, k, :],
                           start=(k == 0), stop=(k == 31), reads=[B_wbt, B_act[k // 2]], writes=[BB[bi]],
                           inc=(k == 31), acc=(k > 0))
                    ACT(gates[:, i, :], banks[bi][:, 0:48], AF.Sigmoid, [BB[bi]], [B_gates], acc=True)

            kv_blocks = list(range(16, 40)) + list(range(56, 88))
            q_blocks = list(range(0, 16)) + list(range(40, 56))
            load_x(0)
            jobs = [dict(src=w_in[b], KC=32, MC=1, rhs=rhs_act, evac=mk_evac_in(b, 0, 1.0)) for b in kv_blocks]
            gemm(P, jobs)
            load_x(1024)
            jobs = [dict(src=w_in[88], custom=gate_custom)]
            for b in range(88):
                jobs.append(dict(src=w_in[b], KC=32, MC=1, rhs=rhs_act,
                                 evac=mk_evac_in(b, 1024, QSCALE if b in q_blocks else 1.0)))
            gemm(P, jobs)
            T.barrier()
            T.emit()

        esBC = ExitStack()
        esBC.__enter__()
        catT = alloc(esBC, "catT", [128, 32, 1024], BF16)
        B_cat = [Buf(f"cat{i}") for i in range(32)]
        kcT_all = alloc(esBC, "kcT_all", [128, 4, 128], BF16)
        vc_all = alloc(esBC, "vc_all", [128, 4, 128], BF16)
        B_kc = Buf("kc")
        B_vc = Buf("vc")

        with ExitStack() as es:
            stg = alloc(es, "cstg", [128, 4096], F32)
            B_stg = Buf("cstg")
            w1b = [alloc(es, f"w1b{kv}", [128, 32, 256], BF16) for kv in range(2)]
            B_w1b = [Buf(f"w1b{kv}") for kv in range(2)]
            w2f = alloc(es, "w2f", [128, 256], F32)
            w2b = [alloc(es, f"w2b{kv}", [128, 2, 128], BF16) for kv in range(2)]
            B_w2 = [Buf(f"w2_{kv}") for kv in range(2)]
            B_w2f = Buf("w2f")
            pef = alloc(es, "pef", [128, 32], F32)
            peb = [alloc(es, f"peb{kv}", [128, 32], BF16) for kv in range(2)]
            B_pe = [Buf(f"pe{kv}") for kv in range(2)]
            B_pef = Buf("pef")
            cT = [alloc(es, f"cT{i}", [128, 2048], BF16) for i in range(2)]
            B_cT = [Buf(f"cT{i}") for i in range(2)]
            b1 = alloc(es, "b1", [128, 1], F32)
            B_b1 = Buf("b1")
            gx = alloc(es, "gx", [128, 128], F32)
            gw = alloc(es, "gw", [128, 128], F32)
            B_gx = Buf("gx")
            B_gw = Buf("gw")
            hid = [alloc(es, f"hid{hc}", [128, 128], BF16) for hc in range(2)]
            B_hid = [Buf(f"hid{hc}") for hc in range(2)]
            for kv in range(2):
                for half in range(2):
                    DMA(stg[:, :], w1c[kv, half], [], [B_stg])
                    CP('dve', w1b[kv][:, half * 16:(half + 1) * 16, :],
                       stg[:, :].rearrange("p (l h) -> p l h", h=256), [B_stg], [B_w1b[kv]], acc=(half == 1))
                DMA(w2f[:, :], w2c[kv], [], [B_w2f])
                CP('dve', w2b[kv][:, :, :], w2f[:, :].rearrange("p (c d) -> p c d", d=128), [B_w2f], [B_w2[kv]])
                DMA(pef[:, :], pec[kv], [], [B_pef])
                CP('dve', peb[kv][:, :], pef[:, :], [B_pef], [B_pe[kv]])
            MS('dve', kcT_all[:, :, :], 0.0, [B_kc])
            MS('dve', vc_all[:, :, :], 0.0, [B_vc])
            for hc in range(2):
                MS('pool', hid[hc][:, :], 0.0, [B_hid[hc]])
            cnt = 0
            for g in range(4):
                for kv in range(2):
                    blk = 16 + kv * 4 + g
                    s = cnt % 2
                    cnt += 1
                    DMA(cT[s][:, :], S_in[blk], [B_Sin[blk]], [B_cT[s]])
                    for hc in range(2):
                        bi = 6 + (hc % 2)
                        for l in range(32):
                            MM(banks[bi][:, 0:127], w1b[kv][:, l, hc * 128:(hc + 1) * 128],
                               cT[s][:, l:l + 16 * 126 + 1:16], start=(l == 0), stop=(l == 31),
                               reads=[B_w1b[kv], B_cT[s]], writes=[BB[bi]], inc=False, acc=(l > 0))
                        for l in range(32):
                            MM(banks[bi][:, 128:129], w1b[kv][:, l, hc * 128:(hc + 1) * 128],
                               peb[kv][:, l:l + 1], start=(l == 0), stop=(l == 31),
                               reads=[B_w1b[kv], B_pe[kv]], writes=[BB[bi]], inc=(l == 31), acc=True)
                        CP('dve', b1[:, :], banks[bi][:, 128:129], [BB[bi]], [B_b1])
                        ACT(gx[:, 0:127], banks[bi][:, 0:127], AF.Identity, [BB[bi], B_b1], [B_gx], bias=b1[:, 0:1])
                        TT('dve', gw[:, 0:127], gx[:, 0:127], gx[:, 0:127], ALU.mult, [B_gx], [B_gw])
                        TS('dve', gw[:, 0:127], gw[:, 0:127], 0.044715, 1.0, ALU.mult, ALU.add, [B_gw], [B_gw])
                        TT('dve', gw[:, 0:127], gw[:, 0:127], gx[:, 0:127], ALU.mult, [B_gw, B_gx], [B_gw])
                        ACT(gw[:, 0:127], gw[:, 0:127], AF.Tanh, [B_gw], [B_gw], scale=0.7978845608028654)
                        STT('dve', gw[:, 0:127], gw[:, 0:127], 1.0, gx[:, 0:127], ALU.add, ALU.mult, [B_gw, B_gx], [B_gw])
                        TS('dve', hid[hc][:, 0:127], gw[:, 0:127], 0.5, None, ALU.mult, None, [B_gw], [B_hid[hc]])
                    if kv == 0:
                        for hc in range(2):
                            MM(banks[6][:, 0:127], w2b[0][:, hc, :], hid[hc][:, 0:127], start=(hc == 0), stop=(hc == 1),
                               reads=[B_w2[0], B_hid[hc]], writes=[BB[6]], inc=(hc == 1), acc=(hc == 1))
                        CP('act', kcT_all[:, g, 0:127], banks[6][:, 0:127], [BB[6]], [B_kc], acc=True)
                    else:
                        for hc in range(2):
                            MM(banks[6][0:127, 0:128], hid[hc][:, 0:127], w2b[1][:, hc, :], start=(hc == 0), stop=(hc == 1),
                               reads=[B_w2[1], B_hid[hc]], writes=[BB[6]], inc=(hc == 1), acc=(hc == 1))
                        CP('act', vc_all[0:127, g, :], banks[6][0:127, 0:128], [BB[6]], [B_vc], acc=True)
            T.barrier()
            T.emit()

        with ExitStack() as es:
            dcl = alloc(es, "dcl", [128, DW], F32)
            B_dcl = Buf("dcl")
            DMA(dcl[:, :], c_dclamp[:, :], [], [B_dcl])
            tmpf = [alloc(es, f"tmpf{i}", [128, 512], F32) for i in range(4)]
            B_tmpf = [Buf(f"tmpf{i}") for i in range(4)]
            tri = alloc(es, "tri", [128, 128], BF16)
            tri2 = alloc(es, "tri2", [128, 128], BF16)
            B_tri = Buf("tri")
            DMA(tmpf[1][:, 0:128], c_tri[:, :], [], [B_tmpf[1]])
            CP('dve', tri[:, :], tmpf[1][:, 0:128], [B_tmpf[1]], [B_tri])
            TS('dve', tri2[:, :], tmpf[1][:, 0:128], -1.0, 1.0, ALU.mult, ALU.add, [B_tmpf[1]], [B_tri], acc=True)
            expb = alloc(es, "expb", [32, 2048], BF16)
            B_exp = Buf("exp")
            for q4 in range(4):
                DMA(tmpf[0][0:32, :], c_expand[:, q4 * 512:(q4 + 1) * 512], [], [B_tmpf[0]])
                CP('dve', expb[:, q4 * 512:(q4 + 1) * 512], tmpf[0][0:32, :], [B_tmpf[0]], [B_exp], acc=True)
            ctxb = alloc(es, "ctxb", [128, 1], F32)
            B_ctxb = Buf("ctxb")
            DMA(ctxb[:, :], p_ctxb[:, :], [], [B_ctxb])
            cmpmask = alloc(es, "cmpmask", [128, 8, 128], F32)
            B_cm = Buf("cmpmask")
            DMA(cmpmask[:, :, :], p_cmpmask.rearrange("p (i n) -> p i n", n=128), [], [B_cm])
            selA = alloc(es, "selA", [128, 8, 32], F32)
            selB = alloc(es, "selB", [128, 8, 32], F32)
            B_sAB = Buf("selAB")
            DMA(selA[:, :, :], p_selA.rearrange("p (i n) -> p i n", n=32), [], [B_sAB])
            DMA(selB[:, :, :], p_selB.rearrange("p (i n) -> p i n", n=32), [], [B_sAB], acc=True,
                dsem=T.newsem("selB"))
            gn = alloc(es, "gn", [128, 512], F32)
            gd = alloc(es, "gd", [128, 256], F32)
            B_g = Buf("gn")
            B_gd = Buf("gd")
            DMA(gd[:, :], gdif[:, :], [], [B_gd])
            lam_t = alloc(es, "lam_t", [128, 512], F32)
            B_lam = Buf("lam")
            lsc = alloc(es, "lsc", [128, 8], F32)
            DMA(lam_t[:, :], lamv[:, :], [], [B_lam])
            TT('dve', lam_t[:, 0:128], lam_t[:, 0:128], lam_t[:, 128:256], ALU.mult, [B_lam], [B_lam])
            TT('dve', lam_t[:, 256:384], lam_t[:, 256:384], lam_t[:, 384:512], ALU.mult, [B_lam], [B_lam])
            REDUCE('dve', lsc[:, 0:1], lam_t[:, 0:128], ALU.add, [B_lam], [B_lam])
            REDUCE('dve', lsc[:, 1:2], lam_t[:, 256:384], ALU.add, [B_lam], [B_lam])
            ACT(lsc[:, 2:4], lsc[:, 0:2], AF.Exp, [B_lam], [B_lam])
            TT('dve', lsc[:, 4:5], lsc[:, 3:4], lsc[:, 2:3], ALU.subtract, [B_lam], [B_lam])
            TS('dve', lsc[:, 5:6], lsc[:, 4:5], -0.2, None, ALU.add, None, [B_lam], [B_lam])
            neglam = lsc[:, 5:6]

            PT = [alloc(es, f"PT{i}", [128, 512], BF16) for i in range(5)]
            B_PT = [Buf(f"PT{i}") for i in range(5)]
            rg = {'t': 0, 'p': 0, 's': 0, 'm': 0}
            sm = alloc(es, "sm", [128, 64], F32)
            B_sm = Buf("sm")
            sqs = alloc(es, "sqs", [128, 256], F32)
            B_sqs = Buf("sqs")
            ob = alloc(es, "ob", [128, 256], BF16)
            B_ob = Buf("ob")
            of = alloc(es, "of", [128, 256], F32)
            B_of = Buf("of")

            def rms_and_store(src_ap, src_bufs, width, gain_ap, gain_bufs, extra, chunk0, i):
                TT('pool', sqs[:, 0:width], src_ap, src_ap, ALU.mult, src_bufs, [B_sqs])
                REDUCE('dve', sm[:, 0:1], sqs[:, 0:width], ALU.add, [B_sqs], [B_sm])
                TS('dve', sm[:, 0:1], sm[:, 0:1], 1.0 / width, RMS_EPS, ALU.mult, ALU.add, [B_sm], [B_sm])
                ACT(sm[:, 1:2], sm[:, 0:1], AF.Ln, [B_sm], [B_sm])
                ACT(sm[:, 2:3], sm[:, 1:2], AF.Exp, [B_sm], [B_sm], scale=-0.5)
                if extra != 1.0:
                    TS('dve', sm[:, 2:3], sm[:, 2:3], extra, None, ALU.mult, None, [B_sm], [B_sm])
                STT('dve', ob[:, 0:width], src_ap, sm[:, 2:3], gain_ap, ALU.mult, ALU.mult,
                    src_bufs + [B_sm] + gain_bufs, [B_ob])
                for hf in range(width // 128):
                    bi = 6 + (rg['m'] % 2)
                    rg['m'] += 1
                    MM(banks[bi][:, 0:128], ob[:, hf * 128:(hf + 1) * 128], identb[:, :], True, True,
                       [B_ob, B_ident], [BB[bi]])
                    CP('act', catT[:, chunk0 + hf, i * 128:(i + 1) * 128], banks[bi][:, 0:128], [BB[bi]],
                       [B_cat[chunk0 + hf]], acc=True)

            def load_V(VT, B_VT, Vtok, B_Vtok, ncol):
                for kt in range(16):
                    bi = 6 + (rg['m'] % 2)
                    rg['m'] += 1
                    for c in range(ncol):
                        MM(banks[bi][:, c * 128:(c + 1) * 128], VT[c][:, kt * 128:(kt + 1) * 128], identb[:, :], True, True,
                           [B_VT[c], B_ident], [BB[bi]], inc=(c == ncol - 1), acc=(c > 0))
                    CP('act' if kt % 2 == 0 else 'dve', Vtok[:, kt, 0:ncol * 128], banks[bi][:, 0:ncol * 128],
                       [BB[bi]], [B_Vtok], acc=True)

            def dense_attn(nm, QT, B_QT, KT, B_KT, Vtok, B_Vtok, dv, slope, CN, chunks, kt_lo_fn, pair_fn, MT, B_MT,
                           finalize):
                ntile = CN // 128
                for c in chunks:
                    qt0 = 8 + c * ntile
                    kts = list(range(kt_lo_fn(qt0), qt0 + ntile))
                    obank = {}
                    for m in range(nm):
                        for ip in range(ntile):
                            obank[(m, ip)] = 2 + m * ntile + ip
                    first = {}
                    sbank = {}

                    def emit_S(kt_):
                        sb_ = (0, 1, 6, 7)[rg['s'] % 4]
                        rg['s'] += 1
                        sbank[kt_] = sb_
                        for m in range(nm):
                            MM(banks[sb_][:, m * CN:(m + 1) * CN], KT[m][:, kt_ * 128:(kt_ + 1) * 128],
                               QT[m][:, c * CN:(c + 1) * CN], True, True, [B_KT[m], B_QT[m]], [BB[sb_]],
                               inc=(m == nm - 1), acc=(m > 0))
                    for k0 in range(min(3, len(kts))):
                        emit_S(kts[k0])
                    for kidx, kt in enumerate(kts):
                        if kidx + 3 < len(kts):
                            emit_S(kts[kidx + 3])
                        sbk = sbank[kt]
                        u0 = 1024 + c * CN - kt * 128 + DOFF
                        tf = rg['t'] % 4
                        rg['t'] += 1
                        if nm == 1:
                            STT('dve', tmpf[tf][:, 0:CN], dcl[:, u0:u0 + CN], -slope, banks[sbk][:, 0:CN],
                                ALU.mult, ALU.add, [B_dcl, BB[sbk]], [B_tmpf[tf]])
                        else:
                            STT('dve', tmpf[tf][:, 0:nm * CN].rearrange("p (m n) -> p m n", m=nm),
                                dcl[:, u0:u0 + CN].unsqueeze(1).broadcast_to([128, nm, CN]), -slope,
                                banks[sbk][:, 0:nm * CN].rearrange("p (m n) -> p m n", m=nm),
                                ALU.mult, ALU.add, [B_dcl, BB[sbk]], [B_tmpf[tf]])
                        pi = rg['p'] % 5
                        rg['p'] += 1
                        ACT(PT[pi][:, 0:nm * CN], tmpf[tf][:, 0:nm * CN], AF.Exp, [B_tmpf[tf], B_ctxb, B_zcol],
                            [B_PT[pi]], bias=(ctxb[:, 0:1] if kt < 8 else zcol[:, 0:1]))
                        if MT is not None:
                            TT('pool', PT[pi][:, 0:CN], PT[pi][:, 0:CN], MT[:, kt, :], ALU.mult,
                               [B_PT[pi], B_MT], [B_PT[pi]])
                        for ip in range(ntile):
                            qt = qt0 + ip
                            pm = pair_fn(kt, qt)
                            if pm is None or pm == 'full':
                                continue
                            msk = tri if pm == 'tri' else tri2
                            if nm == 1:
                                TT('pool', PT[pi][:, ip * 128:(ip + 1) * 128], PT[pi][:, ip * 128:(ip + 1) * 128],
                                   msk[:, :], ALU.mult, [B_PT[pi], B_tri], [B_PT[pi]])
                            else:
                                v = PT[pi][:, 0:nm * CN].rearrange("p (m n) -> p m n", m=nm)[:, :, ip * 128:(ip + 1) * 128]
                                TT('pool', v, v, msk[:, :].unsqueeze(1).broadcast_to([128, nm, 128]), ALU.mult,
                                   [B_PT[pi], B_tri], [B_PT[pi]])
                        avs = []
                        for m in range(nm):
                            for ip in range(ntile):
                                qt = qt0 + ip
                                if pair_fn(kt, qt) is None:
                                    continue
                                avs.append((m, ip))
                        for idx, (m, ip) in enumerate(avs):
                            qt = qt0 + ip
                            ob_ = obank[(m, ip)]
                            st_ = (m, ip) not in first
                            first[(m, ip)] = True
                            MM(banks[ob_][:, 0:dv + 1], PT[pi][:, m * CN + ip * 128:m * CN + (ip + 1) * 128],
                               Vtok[:, kt, 0:dv + 1], st_, (kt == qt), [B_PT[pi], B_Vtok], [BB[ob_]],
                               inc=(idx == len(avs) - 1), acc=(not st_))
                    finalize([(c * ntile + ip, [obank[(m, ip)] for m in range(nm)]) for ip in range(ntile)])

            with ExitStack() as es2:
                QT = [alloc(es2, f"QT{r}", [128, 1024], BF16) for r in range(4)]
                B_QT = [Buf(f"QT{r}") for r in range(4)]
                sK = alloc(es2, "sK", [128, 2048], BF16)
                wK = alloc(es2, "wK", [128, 2048], BF16)
                B_sK = Buf("sK")
                B_wK = Buf("wK")
                VTs = [alloc(es2, "VTs0", [128, 2048], BF16)] * 2
                B_VTs = [Buf("VTs0")] * 2
                sV = alloc(es2, "sV", [128, 16, 132], BF16)
                wV = alloc(es2, "wV", [128, 16, 132], BF16)
                B_sV = Buf("sV")
                B_wV = Buf("wV")
                MS('pool', sV[:, :, 128:129], 1.0, [B_sV])
                MS('pool', wV[:, :, 128:129], 1.0, [B_wV])
                acc = alloc(es2, "acc", [128, 8, 4, 128], F32)
                B_acc = [Buf(f"acc{i}") for i in range(8)]
                MT = alloc(es2, "MT", [128, 16, 512], BF16)
                B_MT = Buf("MT")
                NL = 2
                E4 = [alloc(es2, f"E4{j}", [128, 4, 128], F32) for j in range(NL)]
                B_E4 = [Buf(f"E4{j}") for j in range(NL)]
                pb4 = [alloc(es2, f"pb4{j}", [128, 4, 128], BF16) for j in range(NL)]
                B_pb4 = [Buf(f"pb4{j}") for j in range(NL)]
                pT4 = [alloc(es2, f"pT4{j}", [128, 4, 128], BF16) for j in range(NL)]
                B_pT4 = [Buf(f"pT4{j}") for j in range(NL)]
                rs4 = [alloc(es2, f"rs4{j}", [128, 8], F32) for j in range(NL)]
                B_rs4 = [Buf(f"rs4{j}") for j in range(NL)]
                Ppad = [alloc(es2, f"Ppad{j}", [128, 132], F32) for j in range(NL)]
                B_Pp = [Buf(f"Ppad{j}") for j in range(NL)]
                for j in range(NL):
                    MS('dve', Ppad[j][:, :], 0.0, [B_Pp[j]])
                impt = [alloc(es2, f"impt{j}", [128, 3, 32], F32) for j in range(NL)]
                B_imp = [Buf(f"imp{j}") for j in range(NL)]
                c3 = [alloc(es2, f"c3{j}", [128, 32, 32], F32) for j in range(NL)]
                B_c3 = [Buf(f"c3{j}") for j in range(NL)]
                selb = [alloc(es2, f"selb{j}", [128, 32], BF16) for j in range(NL)]
                B_selb = [Buf(f"selb{j}") for j in range(NL)]
                selT = alloc(es2, "selT", [32, 1024], BF16)
                B_selT = Buf("selT")
                rq4 = [alloc(es2, f"rq4{j}", [128, 12], F32) for j in range(4)]
                B_rq4 = [Buf(f"rq4{j}") for j in range(4)]
                fsm = [alloc(es2, f"fsm{j}", [128, 4], F32) for j in range(4)]
                B_fsm = [Buf(f"fsm{j}") for j in range(4)]

                def v4(ap):
                    return ap.rearrange("p (r n) -> p r n", r=4)

                def fin_add(items, r, h, br):
                    for ip, (i, obs) in enumerate(items):
                        TS('dve', fsm[ip][:, 0:1], banks[obs[0]][:, 128:129], 1e-30, None, ALU.add, None,
                           [BB[obs[0]]], [B_fsm[ip]])
                    for ip, (i, obs) in enumerate(items):
                        RECIP('dve', fsm[ip][:, 1:2], fsm[ip][:, 0:1], [B_fsm[ip]], [B_fsm[ip]])
                    for ip, (i, obs) in enumerate(items):
                        TT('dve', fsm[ip][:, 2:3], fsm[ip][:, 1:2], gates[:, i, h * 3 + br:h * 3 + br + 1], ALU.mult,
                           [B_fsm[ip], B_gates], [B_fsm[ip]])
                    for ip, (i, obs) in enumerate(items):
                        STT('dve', acc[:, i, r, :], banks[obs[0]][:, 0:128], fsm[ip][:, 2:3], acc[:, i, r, :],
                            ALU.mult, ALU.add, [BB[obs[0]], B_fsm[ip], B_acc[i]], [B_acc[i]])

                def rms_batch(g, c):
                    sq = [E4[0][:, :, :], E4[1][:, :, :], v4(tmpf[0][:, :]), v4(tmpf[1][:, :])]
                    B_sq = [B_E4[0], B_E4[1], B_tmpf[0], B_tmpf[1]]
                    obt = [pb4[0], pb4[1], pT4[0], pT4[1]]
                    B_obt = [B_pb4[0], B_pb4[1], B_pT4[0], B_pT4[1]]
                    bk = [6, 7, 0, 1]
                    its = [(ip, 4 * c + ip) for ip in range(4)]
                    for ip, i in its:
                        TT('pool', sq[ip], acc[:, i, :, :], acc[:, i, :, :], ALU.mult, [B_acc[i]], [B_sq[ip]])
                    for ip, i in its:
                        REDUCE('dve', rq4[ip][:, 0:4], sq[ip], ALU.add, [B_sq[ip]], [B_rq4[ip]])
                    for ip, i in its:
                        TS('dve', rq4[ip][:, 0:4], rq4[ip][:, 0:4], 1.0 / 128, RMS_EPS, ALU.mult, ALU.add,
                           [B_rq4[ip]], [B_rq4[ip]])
                    for ip, i in its:
                        ACT(rq4[ip][:, 4:8], rq4[ip][:, 0:4], AF.Ln, [B_rq4[ip]], [B_rq4[ip]])
                    for ip, i in its:
                        ACT(rq4[ip][:, 8:12], rq4[ip][:, 4:8], AF.Exp, [B_rq4[ip]], [B_rq4[ip]], scale=-0.5)
                    for ip, i in its:
                        TT('dve', sq[ip], acc[:, i, :, :], rq4[ip][:, 8:12].unsqueeze(2).broadcast_to([128, 4, 128]),
                           ALU.mult, [B_acc[i], B_rq4[ip]], [B_sq[ip]])
                    for ip, i in its:
                        TT('pool', obt[ip][:, :, :], sq[ip], v4(gn[:, :]), ALU.mult, [B_sq[ip], B_g], [B_obt[ip]])
                    for ip, i in its:
                        for r in range(4):
                            MM(banks[bk[ip]][:, r * 128:(r + 1) * 128], obt[ip][:, r, :], identb[:, :], True, True,
                               [B_obt[ip], B_ident], [BB[bk[ip]]], inc=(r == 3), acc=(r > 0))
                    for ip, i in its:
                        CP('act', catT[:, 4 * g:4 * g + 4, i * 128:(i + 1) * 128], v4(banks[bk[ip]][:, :]),
                           [BB[bk[ip]]], [B_cat[4 * g + r] for r in range(4)], acc=True)

                def pair_win(kt, qt):
                    if kt == qt:
                        return 'tri'
                    if kt == qt - 4:
                        return 'tri2'
                    if qt - 4 < kt < qt:
                        return 'full'
                    return None

                for g in range(4):
                    DMA(gn[:, :], gnsa[:, g * 512:(g + 1) * 512], [], [B_g])
                    for r in range(4):
                        DMA(QT[r][:, :], S_in[g * 4 + r][:, 1024:2048], [B_Sin[g * 4 + r]], [B_QT[r]])
                    DMA(sK[:, :], S_in[16 + 2 * 4 + g], [B_Sin[16 + 8 + g]], [B_sK])
                    DMA(wK[:, :], S_in[16 + 4 * 4 + g], [B_Sin[16 + 16 + g]], [B_wK])
                    DMA(VTs[0][:, :], S_in[16 + 3 * 4 + g], [B_Sin[16 + 12 + g]], [B_VTs[0]])
                    load_V([VTs[0]], [B_VTs[0]], sV, B_sV, 1)
                    DMA(VTs[1][:, :], S_in[16 + 5 * 4 + g], [B_Sin[16 + 20 + g]], [B_VTs[1]])
                    load_V([VTs[1]], [B_VTs[1]], wV, B_wV, 1)
                    for i0 in range(0, 8, NL):
                        ch = [(j, i0 + j) for j in range(NL)]
                        for j, i in ch:
                            A = 2 + 3 * j
                            for r in range(4):
                                MM(banks[A][:, r * 128:(r + 1) * 128], QT[r][:, i * 128:(i + 1) * 128], kcT_all[:, g, :],
                                   True, True, [B_QT[r], B_kc], [BB[A]], inc=(r == 3), acc=(r > 0))
                        for j, i in ch:
                            A = 2 + 3 * j
                            TT('dve', E4[j][:, :, :], v4(banks[A][:, :]),
                               cmpmask[:, i, :].unsqueeze(1).broadcast_to([128, 4, 128]), ALU.add,
                               [BB[A], B_cm], [B_E4[j]])
                        for j, i in ch:
                            ACT(E4[j][:, :, :], E4[j][:, :, :], AF.Exp, [B_E4[j]], [B_E4[j]])
                        for j, i in ch:
                            REDUCE('dve', rs4[j][:, 0:4], E4[j][:, :, :], ALU.add, [B_E4[j]], [B_rs4[j]])
                        for j, i in ch:
                            TS('dve', rs4[j][:, 0:4], rs4[j][:, 0:4], 1e-30, None, ALU.add, None, [B_rs4[j]], [B_rs4[j]])
                        for j, i in ch:
                            RECIP('dve', rs4[j][:, 4:8], rs4[j][:, 0:4], [B_rs4[j]], [B_rs4[j]])
                        for j, i in ch:
                            TT('dve', E4[j][:, :, :], E4[j][:, :, :],
                               rs4[j][:, 4:8].unsqueeze(2).broadcast_to([128, 4, 128]), ALU.mult,
                               [B_E4[j], B_rs4[j]], [B_E4[j]])
                        for j, i in ch:
                            REDUCE('dve', Ppad[j][:, 1:129], E4[j][:, :, :].rearrange("p r n -> p n r"), ALU.add,
                                   [B_E4[j]], [B_Pp[j]])
                        for j, i in ch:
                            CP('pool', pb4[j][:, :, :], E4[j][:, :, :], [B_E4[j]], [B_pb4[j]])
                        for j, i in ch:
                            Bk = 3 + 3 * j
                            for r in range(4):
                                MM(banks[Bk][:, r * 128:(r + 1) * 128], pb4[j][:, r, :], identb[:, :], True, True,
                                   [B_pb4[j], B_ident], [BB[Bk]], inc=(r == 3), acc=(r > 0))
                        for j, i in ch:
                            Bk = 3 + 3 * j
                            CP('act', pT4[j][:, :, :], v4(banks[Bk][:, :]), [BB[Bk]], [B_pT4[j]])
                        for j, i in ch:
                            Ck = 4 + 3 * j
                            for r in range(4):
                                MM(banks[Ck][:, r * 128:(r + 1) * 128], pT4[j][:, r, :], vc_all[:, g, :], True, True,
                                   [B_pT4[j], B_vc], [BB[Ck]], inc=(r == 3), acc=(r > 0))
                        for j, i in ch:
                            Ck = 4 + 3 * j
                            gv = gates[:, i, g * 12:(g + 1) * 12].rearrange("p (r b) -> p r b", b=3)[:, :, 0:1]
                            TT('dve', acc[:, i, :, :], v4(banks[Ck][:, :]), gv.broadcast_to([128, 4, 128]), ALU.mult,
                               [BB[Ck], B_gates], [B_acc[i]])
                        for j, i in ch:
                            Av = Ppad[j][:, 1:129].rearrange("p (j f) -> p j f", f=4)
                            TT('dve', impt[j][:, 0, :], Av[:, :, 0], Av[:, :, 1], ALU.add, [B_Pp[j]], [B_imp[j]])
                        for j, i in ch:
                            Av = Ppad[j][:, 1:129].rearrange("p (j f) -> p j f", f=4)
                            Bv = Ppad[j][:, 0:128].rearrange("p (j f) -> p j f", f=4)
                            TT('dve', impt[j][:, 1, :], Av[:, :, 3], Bv[:, :, 0], ALU.add, [B_Pp[j], B_imp[j]], [B_imp[j]])
                        for j, i in ch:
                            Av = Ppad[j][:, 1:129].rearrange("p (j f) -> p j f", f=4)
                            TT('dve', impt[j][:, 0, :], impt[j][:, 0, :], Av[:, :, 2], ALU.add, [B_Pp[j], B_imp[j]], [B_imp[j]])
                        for j, i in ch:
                            STT('dve', impt[j][:, 0, :], impt[j][:, 1, :], 0.5, impt[j][:, 0, :], ALU.mult, ALU.add,
                                [B_imp[j]], [B_imp[j]])
                        for j, i in ch:
                            TT('dve', impt[j][:, 0, :], impt[j][:, 0, :], selA[:, i, :], ALU.mult, [B_imp[j], B_sAB], [B_imp[j]])
                        for j, i in ch:
                            TT('dve', impt[j][:, 2, :], impt[j][:, 0, :], selB[:, i, :], ALU.add, [B_imp[j], B_sAB], [B_imp[j]])
                        for j, i in ch:
                            sc = impt[j][:, 2, :]
                            TT('dve', c3[j][:, :, :], sc.unsqueeze(1).broadcast_to([128, 32, 32]),
                               sc.unsqueeze(2).broadcast_to([128, 32, 32]), ALU.is_gt, [B_imp[j]], [B_c3[j]])
                        for j, i in ch:
                            REDUCE('dve', impt[j][:, 1, :], c3[j][:, :, :], ALU.add, [B_c3[j]], [B_imp[j]])
                        for j, i in ch:
                            TS('dve', selb[j][:, :], impt[j][:, 1, :], 15.5, None, ALU.is_lt, None, [B_imp[j]], [B_selb[j]])
                        for j, i in ch:
                            MM(banks[j][0:32, 0:128], selb[j][:, :], identb[:, :], True, True, [B_selb[j], B_ident], [BB[j]])
                        for j, i in ch:
                            CP('act', selT[:, i * 128:(i + 1) * 128], banks[j][0:32, 0:128], [BB[j]], [B_selT], acc=True)
                    for c in range(2):
                        qt0 = 8 + 4 * c
                        for kt in range(0, qt0 + 4):
                            bi = 6 + (rg['m'] % 2)
                            rg['m'] += 1
                            MM(banks[bi][:, :], expb[:, kt * 128:(kt + 1) * 128], selT[:, c * 512:(c + 1) * 512],
                               True, True, [B_exp, B_selT], [BB[bi]])
                            CP('act', MT[:, kt, :], banks[bi][:, :], [BB[bi]], [B_MT], acc=True)
                            if kt >= qt0:
                                ip = kt - qt0
                                TT('pool', MT[:, kt, ip * 128:(ip + 1) * 128], MT[:, kt, ip * 128:(ip + 1) * 128],
                                   tri[:, :], ALU.mult, [B_MT, B_tri], [B_MT])
                        for r in range(4):
                            h = g * 4 + r
                            dense_attn(1, [QT[r]], [B_QT[r]], [sK], [B_sK], sV, B_sV, 128, nsa_slope(h), 512, [c],
                                       lambda qt0_: 0, lambda kt, qt: ('full' if kt <= qt else None), MT, B_MT,
                                       lambda items, r=r, h=h: fin_add(items, r, h, 1))
                    for c in range(2):
                        for r in range(4):
                            h = g * 4 + r
                            dense_attn(1, [QT[r]], [B_QT[r]], [wK], [B_wK], wV, B_wV, 128, nsa_slope(h), 512, [c],
                                       lambda qt0_: qt0_ - 4, pair_win, None, None,
                                       lambda items, r=r, h=h: fin_add(items, r, h, 2))
                        rms_batch(g, c)
                T.barrier()
                T.emit()

            with ExitStack() as es2:
                QT = [alloc(es2, f"dQT{m}", [128, 1024], BF16) for m in range(2)]
                B_QT = [Buf(f"dQT{m}") for m in range(2)]
                KT = [alloc(es2, f"dKT{m}", [128, 2048], BF16) for m in range(2)]
                B_KT = [Buf(f"dKT{m}") for m in range(2)]
                VT = [alloc(es2, f"dVT{m}", [128, 2048], BF16) for m in range(2)]
                B_VT = [Buf(f"dVT{m}") for m in range(2)]
                dV = alloc(es2, "dV", [128, 16, 260], BF16)
                B_dV = Buf("dV")
                MS('pool', dV[:, :, 256:257], 1.0, [B_dV])
                of2 = [alloc(es2, f"of2{j}", [128, 256], F32) for j in range(2)]
                B_of2 = [Buf(f"of2{j}") for j in range(2)]
                sq2 = [alloc(es2, f"sq2{j}", [128, 256], F32) for j in range(2)]
                B_sq2 = [Buf(f"sq2{j}") for j in range(2)]
                ob2 = [alloc(es2, f"ob2{j}", [128, 256], BF16) for j in range(2)]
                B_ob2 = [Buf(f"ob2{j}") for j in range(2)]
                d8 = [alloc(es2, f"d8{j}", [128, 8], F32) for j in range(2)]
                B_d8 = [Buf(f"d8{j}") for j in range(2)]

                def fin_diff(items, h):
                    E = list(enumerate(items))
                    for ip, (i, obs) in E:
                        TS('dve', d8[ip][:, 0:1], banks[obs[0]][:, 256:257], 1e-30, None, ALU.add, None, [BB[obs[0]]], [B_d8[ip]])
                    for ip, (i, obs) in E:
                        TS('dve', d8[ip][:, 2:3], banks[obs[1]][:, 256:257], 1e-30, None, ALU.add, None, [BB[obs[1]]], [B_d8[ip]])
                    for ip, (i, obs) in E:
                        RECIP('dve', d8[ip][:, 1:2], d8[ip][:, 0:1], [B_d8[ip]], [B_d8[ip]])
                    for ip, (i, obs) in E:
                        RECIP('dve', d8[ip][:, 3:4], d8[ip][:, 2:3], [B_d8[ip]], [B_d8[ip]])
                    for ip, (i, obs) in E:
                        TT('dve', d8[ip][:, 4:5], d8[ip][:, 3:4], neglam, ALU.mult, [B_d8[ip], B_lam], [B_d8[ip]])
                    for ip, (i, obs) in E:
                        TS('dve', of2[ip][:, :], banks[obs[0]][:, 0:256], d8[ip][:, 1:2], None, ALU.mult, None,
                           [BB[obs[0]], B_d8[ip]], [B_of2[ip]])
                    for ip, (i, obs) in E:
                        STT('dve', of2[ip][:, :], banks[obs[1]][:, 0:256], d8[ip][:, 4:5], of2[ip][:, :], ALU.mult, ALU.add,
                            [BB[obs[1]], B_d8[ip], B_of2[ip]], [B_of2[ip]])
                    for ip, (i, obs) in E:
                        TT('pool', sq2[ip][:, :], of2[ip][:, :], of2[ip][:, :], ALU.mult, [B_of2[ip]], [B_sq2[ip]])
                    for ip, (i, obs) in E:
                        REDUCE('dve', d8[ip][:, 5:6], sq2[ip][:, :], ALU.add, [B_sq2[ip]], [B_d8[ip]])
                    for ip, (i, obs) in E:
                        TS('dve', d8[ip][:, 5:6], d8[ip][:, 5:6], 1.0 / 256, RMS_EPS, ALU.mult, ALU.add, [B_d8[ip]], [B_d8[ip]])
                    for ip, (i, obs) in E:
                        ACT(d8[ip][:, 6:7], d8[ip][:, 5:6], AF.Ln, [B_d8[ip]], [B_d8[ip]])
                    for ip, (i, obs) in E:
                        ACT(d8[ip][:, 7:8], d8[ip][:, 6:7], AF.Exp, [B_d8[ip]], [B_d8[ip]], scale=-0.5)
                    for ip, (i, obs) in E:
                        TS('dve', d8[ip][:, 7:8], d8[ip][:, 7:8], 0.8, None, ALU.mult, None, [B_d8[ip]], [B_d8[ip]])
                    for ip, (i, obs) in E:
                        STT('dve', ob2[ip][:, :], of2[ip][:, :], d8[ip][:, 7:8], gd[:, :], ALU.mult, ALU.mult,
                            [B_of2[ip], B_d8[ip], B_gd], [B_ob2[ip]])
                    for ip, (i, obs) in E:
                        bk = 6 + ip
                        for hf in range(2):
                            MM(banks[bk][:, hf * 128:(hf + 1) * 128], ob2[ip][:, hf * 128:(hf + 1) * 128], identb[:, :],
                               True, True, [B_ob2[ip], B_ident], [BB[bk]], inc=(hf == 1), acc=(hf > 0))
                    for ip, (i, obs) in E:
                        bk = 6 + ip
                        CP('act', catT[:, 16 + 2 * h:16 + 2 * h + 2, i * 128:(i + 1) * 128],
                           banks[bk][:, 0:256].rearrange("p (c q) -> p c q", c=2), [BB[bk]],
                           [B_cat[16 + 2 * h], B_cat[16 + 2 * h + 1]], acc=True)

                for h in range(8):
                    for m in range(2):
                        DMA(QT[m][:, :], S_in[40 + 2 * h + m][:, 1024:2048], [B_Sin[40 + 2 * h + m]], [B_QT[m]])
                        DMA(KT[m][:, :], S_in[56 + 2 * h + m], [B_Sin[56 + 2 * h + m]], [B_KT[m]])
                        DMA(VT[m][:, :], S_in[72 + 2 * h + m], [B_Sin[72 + 2 * h + m]], [B_VT[m]])
                    load_V(VT, B_VT, dV, B_dV, 2)
                    dense_attn(2, QT, B_QT, KT, B_KT, dV, B_dV, 256, diff_slope(h), 256, [0, 1, 2, 3],
                               lambda qt0_: 0, lambda kt, qt: ('tri' if kt == qt else ('full' if kt < qt else None)),
                               None, None, lambda items, h=h: fin_diff(items, h))
                if debug:
                    DMA(dbg_cat[:, :], catT[:, :, :].rearrange("p k n -> p (k n)"), B_cat, [Buf("dbgc")])
                T.barrier()
                T.emit()

        def ln_pass(es, src, B_src_fn, gi, bi_, emit_out):
            ys = [alloc(es, f"lny{i}", [128, 1024], F32) for i in range(2)]
            B_ys = [Buf(f"lny{i}") for i in range(2)]
            sq = [alloc(es, f"lnq{i}", [128, 1024], F32) for i in range(2)]
            B_sq = [Buf(f"lnq{i}") for i in range(2)]
            mean = alloc(es, "lnmean", [128, 1024], F32)
            rstd = alloc(es, "lnrstd", [128, 1024], F32)
            nmr = alloc(es, "lnnmr", [128, 1024], F32)
            B_st = Buf("lnstat")
            ones = alloc(es, "lnones", [128, 128], F32)
            B_ones = Buf("lnones")
            osts = [alloc(es, f"lno{i}", [128, 1024], F32) for i in range(2)]
            B_os = [Buf(f"lno{i}") for i in range(2)]
            for b in B_os:
                b.dsem = T.newsem("ln_" + b.name)
            MS('dve', ones[:, :], 1.0, [B_ones])
            for k in range(32):
                s = k % 2
                DMA(ys[s][:, :], src[k], B_src_fn(k), [B_ys[s]])
                ACT(sq[s][:, :], ys[s][:, :], AF.Square, [B_ys[s]], [B_sq[s]])
                for tg in range(2):
                    MM(banks[tg][:, :], ones[:, :], ys[s][:, tg * 512:(tg + 1) * 512], (k == 0), (k == 31),
                       [B_ones, B_ys[s]], [BB[tg]], acc=(k > 0))
                    MM(banks[2 + tg][:, :], ones[:, :], sq[s][:, tg * 512:(tg + 1) * 512], (k == 0), (k == 31),
                       [B_ones, B_sq[s]], [BB[2 + tg]], acc=(k > 0))
            for tg in range(2):
                sl = slice(tg * 512, (tg + 1) * 512)
                ACT(mean[:, sl], banks[tg][:, :], AF.Copy, [BB[tg]], [B_st], scale=1.0 / 4096, acc=True)
                ACT(rstd[:, sl], banks[2 + tg][:, :], AF.Copy, [BB[2 + tg]], [B_st], scale=1.0 / 4096, acc=True)
            TT('dve', nmr[:, :], mean[:, :], mean[:, :], ALU.mult, [B_st], [B_st])
            TT('dve', rstd[:, :], rstd[:, :], nmr[:, :], ALU.subtract, [B_st], [B_st])
            TS('dve', rstd[:, :], rstd[:, :], LN_EPS, None, ALU.add, None, [B_st], [B_st])
            ACT(rstd[:, :], rstd[:, :], AF.Ln, [B_st], [B_st])
            ACT(rstd[:, :], rstd[:, :], AF.Exp, [B_st], [B_st], scale=-0.5)
            STT('dve', nmr[:, :], mean[:, :], -1.0, rstd[:, :], ALU.mult, ALU.mult, [B_st], [B_st])
            for k in range(32):
                s = k % 2
                DMA(ys[s][:, :], src[k], B_src_fn(k), [B_ys[s]])
                TT('dve', ys[s][:, :], ys[s][:, :], rstd[:, :], ALU.mult, [B_ys[s], B_st], [B_ys[s]])
                TT('pool', ys[s][:, :], ys[s][:, :], nmr[:, :], ALU.add, [B_ys[s], B_st], [B_ys[s]])
                TS('dve' if k % 2 == 0 else 'pool', osts[s][:, :], ys[s][:, :], lnp_t[:, gi * 32 + k:gi * 32 + k + 1],
                   lnp_t[:, bi_ * 32 + k:bi_ * 32 + k + 1], ALU.mult, ALU.add, [B_ys[s], B_lnp], [B_os[s]])
                emit_out(k, osts[s], B_os[s])

        x1T = None
        with ExitStack() as es:
            P = make_pipe(es)
            xres = [alloc(es, f"xres{i}", [128, 1024], F32) for i in range(2)]
            B_xres = [Buf(f"xres{i}") for i in range(2)]
            for b in B_xres:
                b.dsem = T.newsem("xr_" + b.name)
                b.dsem2 = T.newsem("xs_" + b.name)
            stc = {'c': 0}

            def mk_pre(k):
                def pre():
                    s = k % 2
                    DMA(xres[s][:, :], xT[k * 128:(k + 1) * 128, 1024:2048], [], [B_xres[s]], dsem=B_xres[s].dsem)
                return pre

            def mk_ev(k):
                def ev(bset):
                    s = k % 2
                    for tg in range(2):
                        sl = slice(tg * 512, (tg + 1) * 512)
                        STT('dve', xres[s][:, sl], xres[s][:, sl], ALPHA, banks[bset + tg][:, :], ALU.mult, ALU.add,
                            [B_xres[s], BB[bset + tg]], [B_xres[s]])
                    DMA(Ys[k], xres[s][:, :], [B_xres[s]], [B_Ys[k]], dsem=B_xres[s].dsem2)
                return ev

            def rhs_cat(k, tg):
                return catT[:, k, tg * 512:(tg + 1) * 512], [B_cat[k]]
            jobs = [dict(src=w_out[k], KC=32, MC=1, rhs=rhs_cat, pre=mk_pre(k), evac=mk_ev(k)) for k in range(32)]
            gemm(P, jobs)
            T.barrier()
            T.emit()
        esBC.__exit__(None, None, None)
        x1T = alloc(top, "x1T", [128, 32, 1024], BF16)
        B_x1 = [Buf(f"x1_{k}") for k in range(32)]
        with ExitStack() as es:
            def out1(k, t, B_t):
                DMA(X1s[k], t[:, :], [B_t], [B_X1s[k]], dsem=B_t.dsem)
                CP('act', x1T[:, k, :], t[:, :], [B_t], [B_x1[k]])
            ln_pass(es, Ys, lambda k: [B_Ys[k]], 0, 1, out1)
            T.barrier()
            T.emit()

        with ExitStack() as es:
            P = make_pipe(es, nwb=2)
            hT = alloc(es, "hT", [128, FPR, 1024], BF16)
            B_h = [Buf(f"h{i}") for i in range(FPR)]
            rt = [alloc(es, f"rt{i}", [128, 512], F32) for i in range(2)]
            B_rt = [Buf(f"rt{i}") for i in range(2)]
            pst = [alloc(es, f"pst{i}", [128, 2, 1024], F32) for i in range(2)]
            B_pst = [Buf(f"pst{i}") for i in range(2)]
            for b in B_pst:
                b.dsem = T.newsem("pl_" + b.name)
                b.dsem2 = T.newsem("ps_" + b.name)
            std = {'r': 0}

            def rhs_x1(k, tg):
                return x1T[:, k, tg * 512:(tg + 1) * 512], [B_x1[k]]

            def mk_ev1(fcl):
                def ev(bset):
                    for tg in range(2):
                        s = std['r'] % 2
                        std['r'] += 1
                        ACT(rt[s][:, :], banks[bset + tg][:, :], AF.Relu, [BB[bset + tg]], [B_rt[s]])
                        TT('pool', hT[:, fcl, tg * 512:(tg + 1) * 512], rt[s][:, :], rt[s][:, :], ALU.mult,
                           [B_rt[s]], [B_h[fcl]], acc=(tg == 1))
                return ev

            def rhs_h(k, tg):
                return hT[:, k, tg * 512:(tg + 1) * 512], [B_h[k]]

            def mk_pre2(rq, db):
                def pre():
                    s = db % 2
                    if rq == 0:
                        srcv = X1s[2 * db:2 * db + 2].rearrange("c p n -> p c n")
                        rb = [B_X1s[2 * db], B_X1s[2 * db + 1]]
                    else:
                        srcv = Y2s[2 * db:2 * db + 2].rearrange("c p n -> p c n")
                        rb = [B_Y2s[db]]
                    DMA(pst[s][:, :, :], srcv, rb, [B_pst[s]], dsem=B_pst[s].dsem)
                return pre

            def mk_ev2(rq, db):
                def ev(bset):
                    s = db % 2
                    for mc in range(2):
                        for tg in range(2):
                            sl = slice(tg * 512, (tg + 1) * 512)
                            STT('dve', pst[s][:, mc, sl], pst[s][:, mc, sl], (ALPHA if rq == 0 else 1.0),
                                banks[bset + mc * 2 + tg][:, :], ALU.mult, ALU.add,
                                [B_pst[s], BB[bset + mc * 2 + tg]], [B_pst[s]])
                    DMA(Y2s[2 * db:2 * db + 2].rearrange("c p n -> p c n"), pst[s][:, :, :], [B_pst[s]], [B_Y2s[db]],
                        dsem=B_pst[s].dsem2)
                return ev
            for rq in range(NFQ):
                jobs = [dict(src=w_ff1[rq * FPR + f], KC=32, MC=1, rhs=rhs_x1, evac=mk_ev1(f)) for f in range(FPR)]
                gemm(P, jobs)
                jobs = [dict(src=w_ff2[db, rq], KC=FPR, MC=2, rhs=rhs_h, pre=mk_pre2(rq, db), evac=mk_ev2(rq, db))
                        for db in range(16)]
                gemm(P, jobs)
            T.barrier()
            T.emit()

        with ExitStack() as es:
            def out2(k, t, B_t):
                DMA(outT[k * 128:(k + 1) * 128, :], t[:, :], [B_t], [B_out], dsem=B_t.dsem, acc=True)
            ln_pass(es, Y2s, lambda k: [B_Y2s[k // 2]], 2, 3, out2)
            T.barrier()
            T.emit()
    return nc, T


def _blk(w, c0, n=128):
    t = np.zeros((4096, 128), np.float32)
    t[:, :n] = w[:, c0:c0 + n]
    return t.reshape(32, 128, 128).transpose(1, 0, 2).reshape(128, 4096)


def _prep_shared(inp):
    w_in = inp["w_in"][0]
    starts = []
    for h in range(16):
        starts.append(h * 128)
    for j in range(24):
        starts.append(2048 + j * 128)
    for j in range(16):
        starts.append(5168 + j * 128)
    for j in range(16):
        starts.append(7216 + j * 128)
    for j in range(16):
        starts.append(9264 + j * 128)
    w_in_r = np.empty((89, 128, 4096), np.float32)
    for b, c0 in enumerate(starts):
        w_in_r[b] = _blk(w_in, c0)
    w_in_r[88] = _blk(w_in, 5120, 48)
    w_out = inp["w_out"][0]
    w_out_r = np.ascontiguousarray(w_out.reshape(32, 128, 32, 128).transpose(2, 1, 0, 3)).reshape(32, 128, 4096)
    w1 = inp["w_ff1"][0]
    w_ff1_r = np.ascontiguousarray(w1.reshape(32, 128, 128, 128).transpose(2, 1, 0, 3)).reshape(128, 128, 4096)
    w2 = inp["w_ff2"][0]
    w_ff2_r = np.ascontiguousarray(
        w2.reshape(NFQ, FPR, 128, 16, 256).transpose(3, 0, 2, 1, 4)).reshape(16, NFQ, 128, FPR * 256)
    w1c = np.empty((2, 2, 128, 4096), np.float32)
    w2c = np.empty((2, 128, 256), np.float32)
    pec = np.empty((2, 128, 32), np.float32)
    for kv, nm in enumerate(["k", "v"]):
        a = inp["cmp_w1_" + nm][0].reshape(32, 128, 256).transpose(1, 0, 2)
        w1c[kv, 0] = a[:, 0:16, :].reshape(128, 4096)
        w1c[kv, 1] = a[:, 16:32, :].reshape(128, 4096)
        w2c[kv] = inp["cmp_w2_" + nm][0].reshape(2, 128, 128).transpose(1, 0, 2).reshape(128, 256)
        pec[kv] = inp["cmp_pe_" + nm][0].T
    gnsa = np.ascontiguousarray(np.broadcast_to(inp["nsa_out_g"][0].reshape(1, 2048), (128, 2048))).astype(np.float32)
    gdif = np.ascontiguousarray(np.broadcast_to(inp["diff_subln_g"][0].reshape(1, 256), (128, 256))).astype(np.float32)
    lv = np.concatenate([inp["lambda_q1"][0], inp["lambda_k1"][0], inp["lambda_q2"][0], inp["lambda_k2"][0]])
    lamv = np.ascontiguousarray(np.broadcast_to(lv.reshape(1, 512), (128, 512))).astype(np.float32)
    lnp = np.concatenate([inp[n][0].reshape(32, 128).T for n in ["ln1_g", "ln1_b", "ln2_g", "ln2_b"]], axis=1)
    j = np.arange(128)[:, None]
    i = np.arange(128)[None, :]
    u = np.arange(DW)[None, :]
    sh = dict(
        w_in=w_in_r, w_out=w_out_r, w_ff1=w_ff1_r, w_ff2=w_ff2_r, w1c=w1c, w2c=w2c, pec=pec,
        gnsa=gnsa, gdif=gdif, lamv=lamv, lnp=np.ascontiguousarray(lnp.astype(np.float32)),
        c_ident=np.eye(128, dtype=np.float32),
        c_tri=(j <= i).astype(np.float32),
        c_expand=(np.arange(2048)[None, :] // 64 == np.arange(32)[:, None]).astype(np.float32),
        c_dclamp=np.maximum(u - DOFF - j, 0).astype(np.float32),
    )
    return sh


def _prep_core(hh):
    q = np.arange(128)[:, None, None]
    it = np.arange(8)[None, :, None]
    tl = 1024 + 128 * it + q
    n = np.arange(128)[None, None, :]
    okn = (n <= 126) & (16 * n + 31 <= tl)
    if hh == 0:
        okn = okn & (n >= 64)
    cmpmask = np.where(okn, 0.0, NEGB).astype(np.float32).reshape(128, 1024)
    jb = np.arange(32)[None, None, :]
    valid = (64 * jb <= tl)
    if hh == 0:
        valid = valid & (jb >= 16)
    tb = tl // 64
    first = 0 if hh == 1 else 16
    forced = (jb == first) | (jb == tb) | (jb == tb - 1)
    A = np.where(valid & ~forced, 1.0, 0.0)
    Bm = np.where(~valid, -1.0, np.where(forced, 1e6, 0.0))
    ctxb = np.full((128, 1), 0.0 if hh == 1 else NEGB, np.float32)
    return dict(p_cmpmask=cmpmask, p_selA=A.astype(np.float32).reshape(128, 256),
                p_selB=Bm.astype(np.float32).reshape(128, 256), p_ctxb=ctxb)


_CACHE = {}


def kernel(**inputs):
    debug = bool(int(os.environ.get("MK_DEBUG", "0")))
    inp = {k: np.asarray(v) for k, v in inputs.items()}
    x = inp["x"]
    sh = _prep_shared(inp)
    in_maps = []
    for c in range(8):
        b, hh = c // 2, c % 2
        xb = x[b]
        xT = np.zeros((4096, 2048), np.float32)
        if hh == 1:
            xT[:, :] = xb.T
        else:
            xT[:, 1024:] = xb[:1024].T
        m = dict(sh)
        m.update(_prep_core(hh))
        m["xT"] = xT
        in_maps.append(m)
    if 'nc' not in _CACHE:
        _CACHE['nc'] = build(debug)[0]
    nc = _CACHE['nc']
    res = run_bass_kernel_spmd(nc, in_maps, core_ids=list(range(8)))
    out = np.empty((4, 2048, 4096), np.float32)
    for c in range(8):
        b, hh = c // 2, c % 2
        out[b, hh * 1024:(hh + 1) * 1024, :] = res.results[c]["outT"].T
    if debug:
        _CACHE['res'] = res
    return out
```
